# Optimizing a Trainium2 kernel written in Bass

```python
import jax, jax.numpy as jnp
from jax import lax
import numpy as np

D_MODEL = 1024
BATCH = 4
SEQ = 8192
DEPTH = 2

MIX_WIDTH = (3 * D_MODEL) // 4
MEM_WIDTH = D_MODEL // 4
N_MEM = 256
MEM_HEADS = 4
MEM_HEAD_DIM = MEM_WIDTH // MEM_HEADS
CHUNK = 128
SG_GROUPS = 8
SG_GROUP_DIM = MIX_WIDTH // SG_GROUPS
RET_CHUNK = 128
RET_HEADS = 4
RET_V_DIM = MIX_WIDTH // RET_HEADS
RET_QK_DIM = RET_V_DIM // 2
ROPE_BASE = 10000.0
D_FF = 2816
N_EXPERTS = 8
TOP_K = 2
D_FF_EXPERT = 3584
MOE_BLOCK = 128
N_A = (DEPTH + 1) // 2
N_B = DEPTH // 2
DEEPNORM_ALPHA = (2.0 * DEPTH) ** 0.25
DEEPNORM_BETA = (8.0 * DEPTH) ** -0.25
LN_EPS = 1e-5

kernel_name = 'hybrid_sgmlp_retention_moe_encoder'


def layer_norm(x, g, b):
    xf = x.astype(jnp.float32)
    mu = jnp.mean(xf, -1, keepdims=True)
    var = jnp.mean(jnp.square(xf - mu), -1, keepdims=True)
    return ((xf - mu) * lax.rsqrt(var + LN_EPS)).astype(x.dtype) * g + b


def head_norm(o):
    mu = jnp.mean(o, -1, keepdims=True)
    var = jnp.mean(jnp.square(o - mu), -1, keepdims=True)
    on = (o - mu) * lax.rsqrt(var + LN_EPS)
    return on.reshape(o.shape[0], o.shape[1], -1)


def rope_tables(seq, dim):
    inv = ROPE_BASE ** (-jnp.arange(0, dim, 2, dtype=jnp.float32) / dim)
    ang = jnp.arange(seq, dtype=jnp.float32)[:, None] * inv[None, :]
    return jnp.cos(ang), jnp.sin(ang)


def apply_rope(t, cos, sin):
    half = t.shape[-1] // 2
    t1, t2 = t[..., :half], t[..., half:]
    c = cos[None, :, None, :]
    s = sin[None, :, None, :]
    return jnp.concatenate([t1 * c - t2 * s, t2 * c + t1 * s], axis=-1)


def memory_attention(mq, mem_k, mem_v):
    b, s, _ = mq.shape
    q = mq.reshape(b, s, MEM_HEADS, MEM_HEAD_DIM)
    scores = jnp.einsum('bshd,bmhd->bhsm', q, mem_k).astype(jnp.float32) * (MEM_HEAD_DIM ** -0.5)
    probs = jax.nn.softmax(scores, axis=-1).astype(mem_v.dtype)
    return jnp.einsum('bhsm,bmhd->bshd', probs, mem_v).reshape(b, s, MEM_WIDTH)


def spatial_gating(u, v, ln_g, ln_b, w_s, b_s):
    b, s, _ = v.shape
    v = layer_norm(v, ln_g, ln_b).reshape(b, s // CHUNK, CHUNK, SG_GROUPS, SG_GROUP_DIM)
    mixed = jnp.einsum('gpq,bnqgd->bnpgd', w_s, v) + b_s.T[:, :, None]
    return u * mixed.reshape(b, s, MIX_WIDTH)


def chunk_scan(qd, kd, vc, chunk_decay, reverse):
    b, _, _, h, dk = qd.shape
    dv = vc.shape[-1]

    def step(state, xs):
        qn, kn, vn = xs
        out = jnp.einsum('bihd,bhde->bihe', qn, state)
        state = state * chunk_decay[None, :, None, None] + jnp.einsum('bjhd,bjhe->bhde', kn, vn)
        return state, out

    xs = (jnp.moveaxis(qd, 1, 0), jnp.moveaxis(kd, 1, 0), jnp.moveaxis(vc, 1, 0))
    init = jnp.zeros((b, h, dk, dv), jnp.float32)
    _, out = lax.scan(step, init, xs, reverse=reverse)
    return jnp.moveaxis(out, 0, 1)


def bidirectional_retention(q, k, v, log_gf, log_gb):
    b, s, h, dk = q.shape
    dv = v.shape[-1]
    nc = s // RET_CHUNK
    pos = jnp.arange(RET_CHUNK, dtype=jnp.float32)
    qc = (q * dk ** -0.5).reshape(b, nc, RET_CHUNK, h, dk)
    kc = k.reshape(b, nc, RET_CHUNK, h, dk)
    vc = v.reshape(b, nc, RET_CHUNK, h, dv)
    diff = pos[:, None] - pos[None, :]
    expo = jnp.where(diff[None] >= 0, diff[None] * log_gf[:, None, None], -diff[None] * log_gb[:, None, None])
    decay_mask = jnp.exp(expo)
    scores = jnp.einsum('bnihd,bnjhd->bnhij', qc, kc) * decay_mask
    o_inner = jnp.einsum('bnhij,bnjhe->bnihe', scores, vc)
    q_f = qc * jnp.exp(pos[:, None] * log_gf)[:, :, None]
    k_f = kc * jnp.exp((RET_CHUNK - pos)[:, None] * log_gf)[:, :, None]
    o_f = chunk_scan(q_f, k_f, vc, jnp.exp(RET_CHUNK * log_gf), reverse=False)
    q_b = qc * jnp.exp((RET_CHUNK - 1 - pos)[:, None] * log_gb)[:, :, None]
    k_b = kc * jnp.exp((pos + 1)[:, None] * log_gb)[:, :, None]
    o_b = chunk_scan(q_b, k_b, vc, jnp.exp(RET_CHUNK * log_gb), reverse=True)
    return (o_inner + o_f + o_b).reshape(b, s, h, dv)


def dense_swiglu(x, wg, wu, wd):
    return (jax.nn.silu(x @ wg) * (x @ wu)) @ wd


def moe_swiglu(x, w_router, b_router, w_gate, w_up, w_down):
    b, s, d = x.shape
    xt = x.reshape(-1, d)
    n_tok = xt.shape[0]
    n_asg = n_tok * TOP_K
    logits = (xt @ w_router).astype(jnp.float32) + b_router.astype(jnp.float32)
    top_logit, top_idx = lax.top_k(logits, TOP_K)
    gate = jax.nn.softmax(top_logit, axis=-1)
    exp_id = top_idx.reshape(-1)
    tok_id = jnp.arange(n_asg) // TOP_K
    order = jnp.argsort(exp_id)
    sorted_exp = exp_id[order]
    counts = jnp.bincount(exp_id, length=N_EXPERTS)
    padded = (counts + MOE_BLOCK - 1) // MOE_BLOCK * MOE_BLOCK
    pad_end = jnp.cumsum(padded)
    pad_start = pad_end - padded
    grp_start = jnp.cumsum(counts) - counts
    dest = pad_start[sorted_exp] + (jnp.arange(n_asg) - grp_start[sorted_exp])
    n_rows = n_asg + N_EXPERTS * MOE_BLOCK
    n_blocks = n_rows // MOE_BLOCK
    sorted_tok = tok_id[order]
    buf = jnp.zeros((n_rows, d), x.dtype).at[dest].set(xt[sorted_tok])
    blk_exp = jnp.minimum(jnp.searchsorted(pad_end, jnp.arange(n_blocks) * MOE_BLOCK, side='right'), N_EXPERTS - 1)

    def expert_block(args):
        xb, e = args
        return (jax.nn.silu(xb @ w_gate[e]) * (xb @ w_up[e])) @ w_down[e]

    yb = lax.map(expert_block, (buf.reshape(n_blocks, MOE_BLOCK, d), blk_exp))
    y_rows = yb.reshape(n_rows, d)[dest]
    w = gate.reshape(-1)[order].astype(x.dtype)
    y = jax.ops.segment_sum(y_rows * w[:, None], sorted_tok, num_segments=n_tok)
    return y.reshape(b, s, d)


def setup_inputs(seed: int = 0) -> dict:
    key = jax.random.key(seed)
    ks = iter(jax.random.split(key, 32))
    f32 = jnp.float32

    def dense(shape, fan_in, scale=1.0):
        return jax.random.normal(next(ks), shape, f32) * (scale * fan_in ** -0.5)

    def gain(shape):
        return 1.0 + 0.02 * jax.random.normal(next(ks), shape, f32)

    def small(shape, scale=0.02):
        return scale * jax.random.normal(next(ks), shape, f32)

    gam = 1.0 - 2.0 ** (-5.0 - jnp.arange(RET_HEADS, dtype=f32))
    decay_base = jnp.log(gam) - jnp.log1p(-gam)
    n_in_b = 2 * RET_HEADS * RET_QK_DIM + 2 * MIX_WIDTH + MEM_WIDTH
    return {
        'x': jax.random.normal(next(ks), (BATCH, SEQ, D_MODEL), f32),
        'mem': jax.random.normal(next(ks), (BATCH, N_MEM, D_MODEL), f32),
        'w_mem_kv': dense((D_MODEL, 2 * MEM_WIDTH), D_MODEL),
        'w_in_a': dense((N_A, D_MODEL, 2 * MIX_WIDTH + MEM_WIDTH), D_MODEL),
        'sg_ln_g': gain((N_A, MIX_WIDTH)),
        'sg_ln_b': small((N_A, MIX_WIDTH)),
        'sg_w': dense((N_A, SG_GROUPS, CHUNK, CHUNK), CHUNK, 0.5),
        'sg_b': gain((N_A, SG_GROUPS, CHUNK)),
        'w_in_b': dense((N_B, D_MODEL, n_in_b), D_MODEL),
        'decay_logit_f': decay_base + small((N_B, RET_HEADS), 0.1),
        'decay_logit_b': decay_base + small((N_B, RET_HEADS), 0.1),
        'ret_gn_g': gain((N_B, MIX_WIDTH)),
        'ret_gn_b': small((N_B, MIX_WIDTH)),
        'w_out': dense((DEPTH, D_MODEL, D_MODEL), D_MODEL, DEEPNORM_BETA),
        'ln_g': gain((DEPTH, 2, D_MODEL)),
        'ln_b': small((DEPTH, 2, D_MODEL)),
        'w_ff_gate': dense((N_A, D_MODEL, D_FF), D_MODEL),
        'w_ff_up': dense((N_A, D_MODEL, D_FF), D_MODEL),
        'w_ff_down': dense((N_A, D_FF, D_MODEL), D_FF, DEEPNORM_BETA),
        'w_router': dense((N_B, D_MODEL, N_EXPERTS), D_MODEL),
        'b_router': small((N_B, N_EXPERTS), 0.01),
        'w_e_gate': dense((N_B, N_EXPERTS, D_MODEL, D_FF_EXPERT), D_MODEL),
        'w_e_up': dense((N_B, N_EXPERTS, D_MODEL, D_FF_EXPERT), D_MODEL),
        'w_e_down': dense((N_B, N_EXPERTS, D_FF_EXPERT, D_MODEL), D_FF_EXPERT, DEEPNORM_BETA),
    }


def reference(x, mem, w_mem_kv, w_in_a, sg_ln_g, sg_ln_b, sg_w, sg_b, w_in_b, decay_logit_f, decay_logit_b, ret_gn_g, ret_gn_b, w_out, ln_g, ln_b, w_ff_gate, w_ff_up, w_ff_down, w_router, b_router, w_e_gate, w_e_up, w_e_down):
    b, s, _ = x.shape
    f32 = jnp.float32
    mem_k, mem_v = jnp.split(mem @ w_mem_kv, 2, axis=-1)
    mem_k = mem_k.reshape(b, N_MEM, MEM_HEADS, MEM_HEAD_DIM)
    mem_v = mem_v.reshape(b, N_MEM, MEM_HEADS, MEM_HEAD_DIM)
    cos, sin = rope_tables(s, RET_QK_DIM)
    qk_w = RET_HEADS * RET_QK_DIM
    for i in range(DEPTH):
        j = i // 2
        if i % 2 == 0:
            h = x @ w_in_a[j]
            u, v, mq = jnp.split(h, [MIX_WIDTH, 2 * MIX_WIDTH], axis=-1)
            mix = spatial_gating(jax.nn.gelu(u, approximate=False), jax.nn.gelu(v, approximate=False),
                                 sg_ln_g[j], sg_ln_b[j], sg_w[j], sg_b[j])
        else:
            h = x @ w_in_b[j]
            q, k, v, g, mq = jnp.split(h, [qk_w, 2 * qk_w, 2 * qk_w + MIX_WIDTH, 2 * qk_w + 2 * MIX_WIDTH], axis=-1)
            q = apply_rope(q.reshape(b, s, RET_HEADS, RET_QK_DIM).astype(f32), cos, sin)
            k = apply_rope(k.reshape(b, s, RET_HEADS, RET_QK_DIM).astype(f32), cos, sin)
            v = v.reshape(b, s, RET_HEADS, RET_V_DIM).astype(f32)
            log_gf = jax.nn.log_sigmoid(decay_logit_f[j].astype(f32))
            log_gb = jax.nn.log_sigmoid(decay_logit_b[j].astype(f32))
            o = bidirectional_retention(q, k, v, log_gf, log_gb)
            o = head_norm(o).astype(x.dtype) * ret_gn_g[j] + ret_gn_b[j]
            mix = jax.nn.silu(g) * o
        y = jnp.concatenate([mix, memory_attention(mq, mem_k, mem_v)], axis=-1) @ w_out[i]
        x = layer_norm(DEEPNORM_ALPHA * x + y, ln_g[i, 0], ln_b[i, 0])
        if i % 2 == 0:
            f = dense_swiglu(x, w_ff_gate[j], w_ff_up[j], w_ff_down[j])
        else:
            f = moe_swiglu(x, w_router[j], b_router[j], w_e_gate[j], w_e_up[j], w_e_down[j])
        x = layer_norm(DEEPNORM_ALPHA * x + f, ln_g[i, 1], ln_b[i, 1])
    return x
```

```python
import numpy as np
import concourse.bass as bass
import concourse.mybir as mybir

F32 = mybir.dt.float32
BF16 = mybir.dt.bfloat16
I32 = mybir.dt.int32
AF = mybir.ActivationFunctionType
OP = mybir.AluOpType
AX = mybir.AxisListType

NDS = 40


class S:
    def __init__(self, nc):
        self.nc = nc
        self.eng = dict(pe=nc.tensor, act=nc.scalar, dve=nc.vector, pool=nc.gpsimd, sp=nc.sync)
        self.sem = {k: nc.alloc_semaphore("s_" + k) for k in ("pe", "act", "dve", "pool")}
        self.cnt = {k: 0 for k in self.sem}
        self.dsem = [nc.alloc_semaphore("d%d" % i) for i in range(NDS)]
        self.dcnt = [0] * NDS
        self.dnext = 0
        self.waited = {}
        self.w = {}
        self.r = {}
        self.nins = 0

    def _semof(self, sk):
        return self.sem[sk] if isinstance(sk, str) else self.dsem[sk[1]]

    def _wait(self, e, toks):
        best = {}
        for sk, v in toks:
            if sk == e and e == "pe":
                continue
            if best.get(sk, 0) < v:
                best[sk] = v
        for sk, v in best.items():
            if self.waited.get((e, sk), 0) >= v:
                continue
            self.eng[e].wait_ge(self._semof(sk), v)
            self.waited[(e, sk)] = v
            self.nins += 1

    def _deps(self, rd, wr):
        toks = []
        for res in rd:
            if res in self.w:
                toks.append(self.w[res])
        for res in wr:
            if res in self.w:
                toks.append(self.w[res])
            toks.extend(self.r.get(res, ()))
        return toks

    def _record(self, tok, rd, wr):
        for res in wr:
            self.w[res] = tok
            self.r[res] = []
        for res in rd:
            lst = self.r.setdefault(res, [])
            lst[:] = [t for t in lst if t[0] != tok[0]]
            lst.append(tok)

    def op(self, e, fn, rd=(), wr=()):
        self._wait(e, self._deps(rd, wr))
        ins = fn(self.eng[e])
        self.cnt[e] += 1
        ins.then_inc(self.sem[e], 1)
        self.nins += 1
        tok = (e, self.cnt[e])
        self._record(tok, rd, wr)
        return tok

    def mm(self, out, wr, pairs, rd, start=True, stop=True):
        e = "pe"
        self._wait(e, self._deps(rd, [wr]))
        n = len(pairs)
        ins = None
        for i, (a, b) in enumerate(pairs):
            ins = self.nc.tensor.matmul(out, a, b, start=(start and i == 0), stop=(stop and i == n - 1))
            self.nins += 1
        self.cnt[e] += 1
        ins.then_inc(self.sem[e], 1)
        tok = (e, self.cnt[e])
        self._record(tok, rd, [wr])
        return tok

    def tr(self, outs_ins, wr, rd, ident):
        e = "pe"
        self._wait(e, self._deps(list(rd) + ["ident"], [wr]))
        ins = None
        for o, i in outs_ins:
            ins = self.nc.tensor.transpose(o, i, ident)
            self.nins += 1
        self.cnt[e] += 1
        ins.then_inc(self.sem[e], 1)
        tok = (e, self.cnt[e])
        self._record(tok, list(rd) + ["ident"], [wr])
        return tok

    def dma(self, q, out, in_, rd=(), wr=(), **kw):
        i = self.dnext
        self.dnext = (self.dnext + 1) % NDS
        toks = self._deps(rd, wr)
        if self.dcnt[i] > 0:
            toks.append((("d", i), self.dcnt[i]))
        self._wait(q, toks)
        ins = self.eng[q].dma_start(out, in_, **kw)
        self.dcnt[i] += 16
        ins.then_inc(self.dsem[i], 16)
        self.nins += 1
        tok = (("d", i), self.dcnt[i])
        self._record(tok, rd, wr)
        return tok

    def wait_all(self, e):
        toks = [(k, v) for k, v in self.cnt.items() if v > 0]
        toks += [(("d", i), v) for i, v in enumerate(self.dcnt) if v > 0]
        self._wait(e, toks)


from concourse.bass_utils import run_bass_kernel_spmd

ALPHA = float((2.0 * 2) ** 0.25)
EPS = 1e-5
NTOK = 4096


import os
from contextlib import contextmanager, ExitStack


class Stop(Exception):
    pass


class K:
    def ck(self, name):
        if os.environ.get("STOP") == name:
            raise Stop()

    @contextmanager
    def scope(self):
        st = ExitStack()
        self.stacks.append(st)
        try:
            yield
        finally:
            self.barrier()
            self.stacks.pop()
            st.close()

    def barrier(self):
        for e in ("pe", "act", "dve", "pool", "sp"):
            self.s.wait_all(e)

    def __init__(self):
        nc = bass.Bass("TRN2", target_bir_lowering=False)
        self.nc = nc
        self.stacks = [ExitStack()]
        self.s = S(nc)
        self.ps = nc.alloc_psum_tensor("ps", [128, 8, 512], F32).ap()
        self.banks = [0, 1, 2, 3]
        self.bi = 0
        self.ev = 0
        self.uid = 0
        self.ident = self.sb("ident", [128, 128], F32)
        s = self.s
        s.op("pool", lambda e: e.memset(self.ident, 0.0), wr=["ident"])
        s.op("pool", lambda e: e.affine_select(out=self.ident, in_=self.ident, compare_op=OP.not_equal, fill=1.0,
                                               base=0, pattern=[[-1, 128]], channel_multiplier=1),
             rd=["ident"], wr=["ident"])
        self.small = []
        for i in range(4):
            self.small.append((self.sb("st%d" % i, [128, 2, 6], F32), self.sb("mv%d" % i, [128, 2], F32),
                               self.sb("rs%d" % i, [128, 1], F32), "sm%d" % i))
        self.smi = 0

    def din(self, name, shape, dt=F32):
        return self.nc.dram_tensor(name, list(shape), dt, kind="ExternalInput").ap()

    def dout(self, name, shape, dt=F32):
        return self.nc.dram_tensor(name, list(shape), dt, kind="ExternalOutput").ap()

    def dscr(self, name, shape, dt=F32):
        return self.nc.dram_tensor(name, list(shape), dt).ap()

    def sb(self, name, shape, dt):
        return self.stacks[-1].enter_context(self.nc.sbuf_tensor(name, list(shape), dt)).ap()

    def bank(self):
        b = self.banks[self.bi % len(self.banks)]
        self.bi += 1
        return b

    def evac(self, out, in_, rd, wr, eng=None):
        if eng is None:
            eng = ("dve", "act")[self.ev % 2]
            self.ev += 1
        if eng == "act":
            return self.s.op("act", lambda e: e.copy(out, in_), rd=rd, wr=wr)
        return self.s.op(eng, lambda e: e.tensor_copy(out, in_), rd=rd, wr=wr)

    def cast_dram(self, dst, src, rows_per=4096):
        R = src.shape[0]
        for r0 in range(0, R, rows_per):
            r1 = min(R, r0 + rows_per)
            self.s.dma("pool", dst[r0:r1, :], src[r0:r1, :], rd=[], wr=[dst.name])

    def cast_jobs(self, dst, src, rows_per=2048):
        R = src.shape[0]
        jobs = []
        for r0 in range(0, R, rows_per):
            r1 = min(R, r0 + rows_per)
            jobs.append(lambda rd=(), r0=r0, r1=r1: self.s.dma("pool", dst[r0:r1, :], src[r0:r1, :], rd=list(rd), wr=[dst.name + "_%d" % r0]))
        return jobs

    def transpose_tok(self, src, src_res, dstT, dst_res, col0, nk=8):
        s = self.s
        for h in range(0, nk, 4):
            b = self.bank()
            n = min(4, nk - h)
            s.tr([(self.ps[:, b, i * 128:(i + 1) * 128], src[:, (h + i) * 128:(h + i + 1) * 128]) for i in range(n)],
                 "ps%d" % b, [src_res], self.ident)
            self.evac(dstT[:, h:h + n, col0:col0 + 128],
                      self.ps[:, b, 0:n * 128].rearrange("p (k t) -> p k t", k=n), rd=["ps%d" % b], wr=[dst_res], eng="act")

    def ln_inplace(self, X, res, gt, bt, out=None, out_res=None):
        s = self.s
        st, mv, rs, sm = self.small[self.smi % 4]
        self.smi += 1
        W = X.shape[-1]
        nch = (W + 511) // 512
        cw = W // nch
        for c in range(nch):
            s.op("dve", lambda e, c=c: e.bn_stats(st[:, c, :], X[:, c * cw:(c + 1) * cw]), rd=[res], wr=[sm])
        s.op("dve", lambda e: e.bn_aggr(mv, st[:, 0:nch, :].rearrange("p a b -> p (a b)")), rd=[sm], wr=[sm])
        s.op("act", lambda e: e.activation(rs, mv[:, 1:2], AF.Ln, bias=EPS), rd=[sm], wr=[sm])
        s.op("act", lambda e: e.activation(rs, rs, AF.Exp, scale=-0.5), rd=[sm], wr=[sm])
        if gt is None:
            s.op("dve", lambda e: e.tensor_scalar(X, X, mv[:, 0:1], rs, OP.subtract, OP.mult), rd=[res, sm], wr=[res])
        else:
            s.op("dve", lambda e: e.scalar_tensor_tensor(X, X, mv[:, 0:1], gt, OP.subtract, OP.mult), rd=[res, sm, "lnc"], wr=[res])
            s.op("dve", lambda e: e.scalar_tensor_tensor(out if out is not None else X, X, rs, bt, OP.mult, OP.add),
                 rd=[res, sm, "lnc"], wr=[out_res if out is not None else res])


def setup_mem(k, mem, wkv_bf):
    s, ps = k.s, k.ps
    memkT = k.sb("memkT", [128, 2, 256], BF16)
    vmem = k.sb("vmem", [128, 2, 256], BF16)
    with k.scope():
        _setup_mem(k, mem, wkv_bf, memkT, vmem)
    return memkT, vmem


def _setup_mem(k, mem, wkv_bf, memkT, vmem):
    s, ps = k.s, k.ps
    memX = k.sb("memX", [128, 2, 1024], F32)
    memT = k.sb("memT", [128, 8, 256], BF16)
    Wkv = k.sb("Wkv", [128, 8, 512], BF16)
    s.dma("sp", memX, mem.rearrange("(t p) d -> p t d", p=128), wr=["memX"])
    s.dma("sp", Wkv, wkv_bf.rearrange("(k p) n -> p k n", p=128), rd=[wkv_bf.name], wr=["Wkv"])
    for t in range(2):
        k.transpose_tok(memX[:, t, :], "memX", memT, "memT", t * 128)
    for hp in range(2):
        b = k.bank()
        s.mm(ps[:, b, 0:256], "ps%d" % b, [(Wkv[:, kk, hp * 128:(hp + 1) * 128], memT[:, kk, :]) for kk in range(8)],
             ["Wkv", "memT"])
        s.op("act", lambda e, b=b, hp=hp: e.mul(memkT[:, hp, :], ps[:, b, 0:256], 0.125), rd=["ps%d" % b], wr=["memkT"])
    for mt in range(2):
        b = k.bank()
        s.mm(ps[:, b, 0:256], "ps%d" % b,
             [(memT[:, kk, mt * 128:(mt + 1) * 128], Wkv[:, kk, 256:512]) for kk in range(8)], ["Wkv", "memT"])
        k.evac(vmem[:, mt, :], ps[:, b, 0:256], rd=["ps%d" % b], wr=["vmem"])
    k.ck("mem")
    return memkT, vmem


def mem_attn_a(k, mqT, mq_res, memkT, vmem, memoT, memo_res, s_, bufs):
    s, ps = k.s, k.ps
    probs, probsT, rmax, rsum = bufs
    c0 = s_ * 128
    for h2 in range(2):
        b = k.bank()
        for hp in range(2):
            s.mm(ps[:, b, hp * 256:(hp + 1) * 256], "ps%d" % b,
                 [(mqT[h2 * 64:(h2 + 1) * 64, hp, c0:c0 + 128], memkT[h2 * 64:(h2 + 1) * 64, hp, :])],
                 [mq_res, "memkT"])
        s.op("dve", lambda e, b=b, h2=h2: e.tensor_reduce(rmax[:, h2 * 2:(h2 + 1) * 2],
                                                         ps[:, b, :].rearrange("p (h m) -> p h m", h=2),
                                                         AX.X, OP.max, negate=True),
             rd=["ps%d" % b], wr=["rmax"])
        for hp in range(2):
            h = hp * 2 + h2
            s.op("act", lambda e, b=b, h=h, hp=hp, h2=h2: e.activation(probs[:, h, :], ps[:, b, hp * 256:(hp + 1) * 256], AF.Exp,
                                                               bias=rmax[:, h2 * 2 + hp:h2 * 2 + hp + 1], scale=1.0,
                                                               accum_out=rsum[:, h:h + 1]),
                 rd=["ps%d" % b, "rmax"], wr=["probs", "rsum"])
    s.op("dve", lambda e: e.reciprocal(rsum, rsum), rd=["rsum"], wr=["rsum"])
    for h in range(4):
        s.op("act", lambda e, h=h: e.mul(probs[:, h, :], probs[:, h, :], rsum[:, h:h + 1]),
             rd=["probs", "rsum"], wr=["probs"])


def mem_attn_b(k, mqT, mq_res, memkT, vmem, memoT, memo_res, s_, bufs):
    s, ps = k.s, k.ps
    probs, probsT, rmax, rsum = bufs
    c0 = s_ * 128
    for hp in range(2):
        b = k.bank()
        s.tr([(ps[:, b, (h2 * 2 + mt) * 128:(h2 * 2 + mt + 1) * 128], probs[:, hp * 2 + h2, mt * 128:(mt + 1) * 128])
              for h2 in range(2) for mt in range(2)], "ps%d" % b, ["probs"], k.ident)
        k.evac(probsT[:, hp * 2:(hp + 1) * 2, :, :].rearrange("p h m t -> p (h m t)"), ps[:, b, :],
               rd=["ps%d" % b], wr=["probsT"])
    b = k.bank()
    for h in range(4):
        hp, h2 = h // 2, h % 2
        s.mm(ps[h2 * 64:(h2 + 1) * 64, b, hp * 128:(hp + 1) * 128], "ps%d" % b,
             [(vmem[:, mt, h * 64:(h + 1) * 64], probsT[:, h, mt, :]) for mt in range(2)], ["vmem", "probsT"])
    k.evac(memoT[:, :, c0:c0 + 128], ps[:, b, 0:256].rearrange("p (t c) -> p t c", t=2), rd=["ps%d" % b], wr=[memo_res])


def mem_attn(k, mqT, mq_res, memkT, vmem, memoT, memo_res, s_, bufs):
    mem_attn_a(k, mqT, mq_res, memkT, vmem, memoT, memo_res, s_, bufs)
    mem_attn_b(k, mqT, mq_res, memkT, vmem, memoT, memo_res, s_, bufs)


def phase_l0(k, jobs, memkT, vmem, w_in_bf, w_out_bf, wg_bf, wu_bf, wd_bf, sg_w, sg_b, sg_ln_g, sg_ln_b, ln_g, ln_b, ntt=16, bg=None):
    s, ps = k.s, k.ps
    T = 256
    Win = k.sb("Win", [128, 8, 1792], BF16)
    Wom = k.sb("Wom", [96, 8, 1024], BF16)
    Woq = k.sb("Woq", [128, 2, 1024], BF16)
    s.dma("sp", Win, w_in_bf.rearrange("(k p) n -> p k n", p=128), rd=[w_in_bf.name], wr=["Win"])
    s.dma("sp", Wom, w_out_bf[0:768, :].rearrange("(g p) n -> p g n", p=96), rd=[w_out_bf.name], wr=["Wom"])
    s.dma("sp", Woq, w_out_bf[768:1024, :].rearrange("(t p) n -> p t n", p=128), rd=[w_out_bf.name], wr=["Woq"])
    wsX = k.sb("wsX", [128, 8, 128], F32)
    wsT = k.sb("wsT", [128, 8, 128], BF16)
    s.dma("sp", wsX, sg_w.rearrange("g p q -> p g q"), wr=["wsX"])
    for h in range(2):
        b = k.bank()
        s.tr([(ps[:, b, i * 128:(i + 1) * 128], wsX[:, h * 4 + i, :]) for i in range(4)], "ps%d" % b, ["wsX"], k.ident)
        k.evac(wsT[:, h * 4:(h + 1) * 4, :].rearrange("p g q -> p (g q)"), ps[:, b, :], rd=["ps%d" % b], wr=["wsT"])
    bsb = k.sb("bsb", [96, 8, 128], F32)
    s.dma("sp", bsb.rearrange("p g q -> p (g q)"), sg_b.rearrange("g q -> (g q)").partition_broadcast(96), wr=["bsb"])
    lng = k.sb("lng", [128, 768], F32)
    lnb = k.sb("lnb", [128, 768], F32)
    s.dma("sp", lng, sg_ln_g.partition_broadcast(128), wr=["lnc"])
    s.dma("sp", lnb, sg_ln_b.partition_broadcast(128), wr=["lnc"])
    G = [[k.sb("lg%d%d" % (i, j), [128, 1024], F32) for j in range(2)] for i in range(2)]
    for i in range(2):
        s.dma("sp", G[i][0], ln_g[i].partition_broadcast(128), wr=["lnc"])
        s.dma("sp", G[i][1], ln_b[i].partition_broadcast(128), wr=["lnc"])

    XA = [k.sb("XA%d" % i, [128, 2, 1024], F32) for i in range(3)]
    X0T = k.sb("X0T", [128, 8, T], BF16)
    X1T = [k.sb("X1T%d" % i, [128, 8, T], BF16) for i in range(2)]
    uT = k.sb("uT", [96, 8, T], BF16)
    mqT = k.sb("mqT", [128, 2, T], BF16)
    memoT = k.sb("memoT", [128, 2, T], BF16)
    Vf = [k.sb("Vf%d" % i, [128, 768], F32) for i in range(2)]
    vn = [k.sb("vn%d" % i, [128, 768], BF16) for i in range(2)]
    tmp = k.sb("tmpmix", [96, 4, 128], F32)
    probs = k.sb("probs", [128, 4, 256], F32)
    probsT = k.sb("probsT", [128, 4, 2, 128], BF16)
    rmax = k.sb("rmax", [128, 4], F32)
    rsum = k.sb("rsum", [128, 4], F32)
    NW = 3
    WG = [k.sb("WG%d" % i, [128, 8, 256], BF16) for i in range(NW)]
    WU = [k.sb("WU%d" % i, [128, 8, 256], BF16) for i in range(NW)]
    WD = [k.sb("WD%d" % i, [128, 2, 1024], BF16) for i in range(NW)]
    sg = [k.sb("sg%d" % i, [128, T], BF16) for i in range(2)]
    hT = [k.sb("hT%d" % i, [128, 2, T], BF16) for i in range(2)]
    NT = len(jobs) * ntt
    NBLK = 11

    def wload(g):
        if g >= NT * NBLK:
            return
        j = g % NBLK
        wb = g % NW
        s.dma("sp", WG[wb], wg_bf[:, j * 256:(j + 1) * 256].rearrange("(k p) n -> p k n", p=128), rd=[wg_bf.name], wr=["WG%d" % wb])
        s.dma("sp", WU[wb], wu_bf[:, j * 256:(j + 1) * 256].rearrange("(k p) n -> p k n", p=128), rd=[wu_bf.name], wr=["WU%d" % wb])
        s.dma("sp", WD[wb], wd_bf[j * 256:(j + 1) * 256, :].rearrange("(t p) n -> p t n", p=128), rd=[wd_bf.name], wr=["WD%d" % wb])

    def xload(jt):
        if jt >= NT:
            return
        x, X1, pfx = jobs[jt // ntt]
        r0 = (jt % ntt) * T
        s.dma("sp", XA[jt % 3], x[r0:r0 + T, :].rearrange("(s p) d -> p s d", p=128), wr=["XA%d_%d" % (jt % 3, i) for i in range(2)])

    def mixer(jt):
        p = jt % 2
        x, X1, pfx = jobs[jt // ntt]
        tt = jt % ntt
        r0 = tt * T
        xa = XA[jt % 3]
        xrn = ["XA%d_%d" % (jt % 3, i) for i in range(2)]
        for s_ in range(2):
            k.transpose_tok(xa[:, s_, :], xrn[s_], X0T, "X0T", s_ * 128)
        yield
        for s_ in range(2):
            c0 = s_ * 128
            for h2 in range(2):
                b = k.bank()
                s.mm(ps[:, b, 0:384], "ps%d" % b,
                     [(X0T[:, kk, c0:c0 + 128], Win[:, kk, 768 + h2 * 384:768 + (h2 + 1) * 384]) for kk in range(8)],
                     ["Win", "X0T"])
                s.op("act", lambda e, b=b, h2=h2, s_=s_: e.activation(Vf[s_][:, h2 * 384:(h2 + 1) * 384], ps[:, b, 0:384], AF.Gelu),
                     rd=["ps%d" % b], wr=["Vf%d" % s_])
            k.ln_inplace(Vf[s_], "Vf%d" % s_, lng, lnb, out=vn[s_], out_res="vn%d" % s_)
        yield
        for g in range(8):
            b = k.bank()
            s.mm(ps[0:96, b, 0:T], "ps%d" % b, [(Win[:, kk, g * 96:(g + 1) * 96], X0T[:, kk, :]) for kk in range(8)],
                 ["Win", "X0T"])
            s.op("act", lambda e, b=b, g=g: e.activation(uT[:, g, :], ps[0:96, b, 0:T], AF.Gelu), rd=["ps%d" % b], wr=["uT"])
        for t in range(2):
            b = k.bank()
            s.mm(ps[:, b, 0:T], "ps%d" % b,
                 [(Win[:, kk, 1536 + t * 128:1536 + (t + 1) * 128], X0T[:, kk, :]) for kk in range(8)], ["Win", "X0T"])
            k.evac(mqT[:, t, :], ps[:, b, 0:T], rd=["ps%d" % b], wr=["mqT"])
        yield
        for s_ in range(2):
            c0 = s_ * 128
            for gh in range(2):
                b = k.bank()
                for g4 in range(4):
                    g = gh * 4 + g4
                    s.mm(ps[0:96, b, g4 * 128:(g4 + 1) * 128], "ps%d" % b, [(vn[s_][:, g * 96:(g + 1) * 96], wsT[:, g, :])],
                         ["vn%d" % s_, "wsT"])
                s.op("dve", lambda e, b=b, gh=gh: e.tensor_tensor(tmp, ps[0:96, b, :].rearrange("p (g q) -> p g q", g=4),
                                                                 bsb[:, gh * 4:(gh + 1) * 4, :], OP.add),
                     rd=["ps%d" % b, "bsb"], wr=["tmpmix"])
                s.op("dve", lambda e, gh=gh, c0=c0: e.tensor_tensor(uT[:, gh * 4:(gh + 1) * 4, c0:c0 + 128], tmp,
                                                                   uT[:, gh * 4:(gh + 1) * 4, c0:c0 + 128], OP.mult),
                     rd=["tmpmix", "uT"], wr=["uT"])
        yield
        for s_ in range(2):
            mem_attn_a(k, mqT, "mqT", memkT, vmem, memoT, "memoT", s_, (probs, probsT, rmax, rsum))
            yield
            mem_attn_b(k, mqT, "mqT", memkT, vmem, memoT, "memoT", s_, (probs, probsT, rmax, rsum))
            yield
        for s_ in range(2):
            c0 = s_ * 128
            for nh in range(2):
                b = k.bank()
                s.mm(ps[:, b, :], "ps%d" % b,
                     [(uT[:, g, c0:c0 + 128], Wom[:, g, nh * 512:(nh + 1) * 512]) for g in range(8)] +
                     [(memoT[:, t, c0:c0 + 128], Woq[:, t, nh * 512:(nh + 1) * 512]) for t in range(2)],
                     ["uT", "memoT", "Wom", "Woq"])
                s.op("dve", lambda e, b=b, nh=nh, s_=s_: e.scalar_tensor_tensor(
                    xa[:, s_, nh * 512:(nh + 1) * 512], xa[:, s_, nh * 512:(nh + 1) * 512], ALPHA, ps[:, b, :],
                    OP.mult, OP.add), rd=["ps%d" % b, xrn[s_]], wr=[xrn[s_]])
            k.ln_inplace(xa[:, s_, :], xrn[s_], G[0][0], G[0][1])
            yield
        for s_ in range(2):
            k.transpose_tok(xa[:, s_, :], xrn[s_], X1T[p], "X1T%d" % p, s_ * 128)
        yield

    def ffn(jt):
        p = jt % 2
        x, X1, pfx = jobs[jt // ntt]
        tt = jt % ntt
        r0 = tt * T
        xa = XA[jt % 3]
        xt = X1T[p]
        xtn = "X1T%d" % p
        xrn = ["XA%d_%d" % (jt % 3, i) for i in range(2)]

        def down(j):
            g = jt * NBLK + j
            wb = g % NW
            hb = g % 2
            for s_ in range(2):
                for nh in range(2):
                    ab = 4 + s_ * 2 + nh
                    s.mm(ps[:, ab, :], "ps%d" % ab,
                         [(hT[hb][:, t2, s_ * 128:(s_ + 1) * 128], WD[wb][:, t2, nh * 512:(nh + 1) * 512]) for t2 in range(2)],
                         ["hT%d" % hb, "WD%d" % wb], start=(j == 0), stop=(j == NBLK - 1))

        for j in range(NBLK):
            g = jt * NBLK + j
            wb = g % NW
            hb = g % 2
            if g == 0:
                wload(0)
            wload(g + 1)
            for t2 in range(2):
                bg_ = k.bank()
                bu = k.bank()
                s.mm(ps[:, bg_, 0:T], "ps%d" % bg_, [(WG[wb][:, kk, t2 * 128:(t2 + 1) * 128], xt[:, kk, :]) for kk in range(8)],
                     ["WG%d" % wb, xtn])
                s.mm(ps[:, bu, 0:T], "ps%d" % bu, [(WU[wb][:, kk, t2 * 128:(t2 + 1) * 128], xt[:, kk, :]) for kk in range(8)],
                     ["WU%d" % wb, xtn])
                s.op("act", lambda e, bg_=bg_, t2=t2: e.activation(sg[t2], ps[:, bg_, 0:T], AF.Silu), rd=["ps%d" % bg_],
                     wr=["sg%d" % t2])
                s.op("dve", lambda e, bu=bu, t2=t2, hb=hb: e.tensor_tensor(hT[hb][:, t2, :], sg[t2], ps[:, bu, 0:T], OP.mult),
                     rd=["ps%d" % bu, "sg%d" % t2], wr=["hT%d" % hb])
            if j > 0:
                down(j - 1)
            yield
        down(NBLK - 1)
        for s_ in range(2):
            for nh in range(2):
                ab = 4 + s_ * 2 + nh
                s.op("dve", lambda e, ab=ab, nh=nh, s_=s_: e.scalar_tensor_tensor(
                    xa[:, s_, nh * 512:(nh + 1) * 512], xa[:, s_, nh * 512:(nh + 1) * 512], ALPHA, ps[:, ab, :],
                    OP.mult, OP.add), rd=["ps%d" % ab, xrn[s_]], wr=[xrn[s_]])
            k.ln_inplace(xa[:, s_, :], xrn[s_], G[1][0], G[1][1])
            s.dma("sp", X1[r0 + s_ * 128:r0 + (s_ + 1) * 128, :], xa[:, s_, :], rd=[xrn[s_]], wr=[pfx + "_%d" % (tt * 2 + s_)])
        yield

    bg = list(bg) if bg else []
    xload(0)
    xload(1)
    for _ in mixer(0):
        pass
    sched = [1] * 11 + [0]
    for jt in range(NT):
        m = mixer(jt + 1) if jt + 1 < NT else None
        xload(jt + 2)
        for bi, _ in enumerate(ffn(jt)):
            for _r in range(sched[bi] if bi < len(sched) else 1):
                if m is not None:
                    try:
                        next(m)
                    except StopIteration:
                        m = None
        while m is not None:
            try:
                next(m)
            except StopIteration:
                m = None
        if bg:
            bg.pop(0)()
    while bg:
        bg.pop(0)()


def bc(ap, dims):
    pstep = ap.ap[0]
    return bass.AP(ap.tensor, ap.offset, [list(pstep)] + [list(d) for d in dims])


DK = 96
DV = 192
QS = float(DK ** -0.5)
TWO_PI = float(2 * np.pi)


def setup_l1(k, dlf, dlb, posb):
    c = {}
    c["lg"] = k.sb("lg", [128, 8], F32)
    c["fac"] = k.sb("fac", [128, 4, 4], F32)
    c["g128"] = k.sb("g128", [128, 8], F32)
    c["pw"] = k.sb("pw", [128, 8, 32], F32)
    c["maskT"] = k.sb("maskT", [128, 4, 128], F32)
    c["cos"] = k.sb("cos", [128, 32, 48], F32)
    c["sin"] = k.sb("sin", [128, 32, 48], F32)
    c["A"] = k.sb("ropeA", [128, 768], F32)
    c["B"] = k.sb("ropeB", [128, 768], F32)
    with k.scope():
        _setup_l1(k, c, dlf, dlb, posb)
    return c


def _setup_l1(k, c, dlf, dlb, posb):
    s = k.s
    lg, fac, g128, pw, maskT, cos, sin = c["lg"], c["fac"], c["g128"], c["pw"], c["maskT"], c["cos"], c["sin"]
    s.dma("sp", lg[:, 0:4], dlf.partition_broadcast(128), wr=["lg"])
    s.dma("sp", lg[:, 4:8], dlb.partition_broadcast(128), wr=["lg"])
    s.op("act", lambda e: e.activation(lg, lg, AF.Exp, scale=-1.0), rd=["lg"], wr=["lg"])
    s.op("act", lambda e: e.activation(lg, lg, AF.Ln, bias=1.0), rd=["lg"], wr=["lg"])
    s.op("dve", lambda e: e.tensor_scalar(lg, lg, -1.0, None, OP.mult), rd=["lg"], wr=["lg"])
    pidx_i = k.sb("pidx_i", [128, 1], I32)
    pidx = k.sb("pidx", [128, 1], F32)
    s.op("pool", lambda e: e.iota(pidx_i, [[0, 1]], base=0, channel_multiplier=1), wr=["pidx_i"])
    s.op("dve", lambda e: e.tensor_copy(pidx, pidx_i), rd=["pidx_i"], wr=["pidx"])
    q = k.sb("qtmp", [128, 1], F32)
    specs = [(0, 1.0, 0.0, QS, 0), (1, -1.0, 127.0, QS, 4), (2, -1.0, 128.0, 1.0, 0), (3, 1.0, 1.0, 1.0, 4)]
    for idx, a, b, m, lo in specs:
        s.op("dve", lambda e, a=a, b=b: e.tensor_scalar(q, pidx, a, b, OP.mult, OP.add), rd=["pidx"], wr=["qtmp"])
        s.op("dve", lambda e, idx=idx, lo=lo: e.tensor_scalar(fac[:, idx, :], lg[:, lo:lo + 4], q, None, OP.mult),
             rd=["qtmp", "lg"], wr=["fac"])
        s.op("act", lambda e, idx=idx: e.activation(fac[:, idx, :], fac[:, idx, :], AF.Exp), rd=["fac"], wr=["fac"])
        if m != 1.0:
            s.op("dve", lambda e, idx=idx, m=m: e.tensor_scalar(fac[:, idx, :], fac[:, idx, :], m, None, OP.mult),
                 rd=["fac"], wr=["fac"])
    s.op("act", lambda e: e.activation(g128, lg, AF.Exp, scale=128.0), rd=["lg"], wr=["g128"])
    nidx_i = k.sb("nidx_i", [128, 32], I32)
    nidx = k.sb("nidx", [128, 32], F32)
    s.op("pool", lambda e: e.iota(nidx_i, [[-128, 32]], base=128 * 31, channel_multiplier=0), wr=["nidx_i"])
    s.op("dve", lambda e: e.tensor_copy(nidx, nidx_i), rd=["nidx_i"], wr=["nidx"])
    for j in range(8):
        s.op("dve", lambda e, j=j: e.tensor_scalar(pw[:, j, :], nidx, lg[:, j:j + 1], None, OP.mult), rd=["nidx", "lg"], wr=["pw"])
    s.op("act", lambda e: e.activation(pw, pw, AF.Exp), rd=["pw"], wr=["pw"])
    D_i = k.sb("D_i", [128, 128], I32)
    Dp = k.sb("Dp", [128, 128], F32)
    Dn = k.sb("Dn", [128, 128], F32)
    E1 = k.sb("E1", [128, 128], F32)
    s.op("pool", lambda e: e.iota(D_i, [[1, 128]], base=0, channel_multiplier=-1), wr=["D_i"])
    s.op("dve", lambda e: e.tensor_copy(Dn, D_i), rd=["D_i"], wr=["Dn"])
    s.op("dve", lambda e: e.tensor_scalar(Dp, Dn, 0.0, None, OP.max), rd=["Dn"], wr=["Dp"])
    s.op("dve", lambda e: e.tensor_tensor(Dn, Dp, Dn, OP.subtract), rd=["Dp", "Dn"], wr=["Dn"])
    for h in range(4):
        s.op("dve", lambda e, h=h: e.tensor_scalar(E1, Dp, lg[:, h:h + 1], None, OP.mult), rd=["Dp", "lg"], wr=["E1"])
        s.op("dve", lambda e, h=h: e.scalar_tensor_tensor(E1, Dn, lg[:, 4 + h:5 + h], E1, OP.mult, OP.add),
             rd=["Dn", "lg", "E1"], wr=["E1"])
        s.op("act", lambda e, h=h: e.activation(maskT[:, h, :], E1, AF.Exp), rd=["E1"], wr=["maskT"])
    s.op("dve", lambda e: e.tensor_scalar(maskT, maskT, QS, None, OP.mult), rd=["maskT"], wr=["maskT"])


def setup_rope(k, c, posb):
    with k.scope():
        _setup_rope(k, c, posb)


def _setup_rope(k, c, posb):
    s = k.s
    cos, sin = c["cos"], c["sin"]
    k.uid += 1
    u = "_%d" % k.uid
    pb = k.sb("pb" + u, [128, 1], F32)
    s.dma("sp", pb, posb.partition_broadcast(128), wr=["pb"])
    ii = k.sb("ii" + u, [128, 48], I32)
    inv = k.sb("inv" + u, [128, 48], F32)
    s.op("pool", lambda e: e.iota(ii, [[1, 48]], base=0, channel_multiplier=0), wr=["ii"])
    s.op("dve", lambda e: e.tensor_copy(inv, ii), rd=["ii"], wr=["inv"])
    s.op("act", lambda e: e.activation(inv, inv, AF.Exp, scale=float(-2.0 * np.log(10000.0) / 96.0)), rd=["inv"], wr=["inv"])
    pos_i = k.sb("pos_i" + u, [128, 32], I32)
    pos = k.sb("pos" + u, [128, 32], F32)
    s.op("pool", lambda e: e.iota(pos_i, [[128, 32]], base=0, channel_multiplier=1), wr=["pos_i"])
    s.op("dve", lambda e: e.tensor_copy(pos, pos_i), rd=["pos_i"], wr=["pos"])
    s.op("dve", lambda e: e.tensor_scalar(pos, pos, pb, None, OP.add), rd=["pos", "pb"], wr=["pos"])
    ang = k.sb("ang" + u, [128, 32, 48], F32)
    ri = k.sb("ri" + u, [128, 32, 48], I32)
    rf = k.sb("rf" + u, [128, 32, 48], F32)
    for n in range(32):
        s.op("dve", lambda e, n=n: e.tensor_scalar(ang[:, n, :], inv, pos[:, n:n + 1], None, OP.mult), rd=["inv", "pos"], wr=["ang"])
    for (dst, name, off) in ((sin, "sin", 0.0), (cos, "cos", 0.25)):
        s.op("dve", lambda e, dst=dst, off=off: e.tensor_scalar(dst, ang, float(1.0 / TWO_PI), off, OP.mult, OP.add), rd=["ang"], wr=[name])
        s.op("dve", lambda e, dst=dst: e.tensor_copy(ri, dst), rd=[name], wr=["ri"])
        s.op("dve", lambda e: e.tensor_copy(rf, ri), rd=["ri"], wr=["rf"])
        s.op("dve", lambda e, dst=dst: e.tensor_tensor(dst, dst, rf, OP.subtract), rd=[name, "rf"], wr=[name])
        s.op("dve", lambda e, dst=dst: e.tensor_scalar(rf, dst, 0.5, None, OP.is_gt), rd=[name], wr=["rf"])
        s.op("dve", lambda e, dst=dst: e.tensor_tensor(dst, dst, rf, OP.subtract), rd=[name, "rf"], wr=[name])
        s.op("dve", lambda e, dst=dst: e.tensor_scalar(rf, dst, -0.5, None, OP.is_lt), rd=[name], wr=["rf"])
        s.op("dve", lambda e, dst=dst: e.tensor_tensor(dst, dst, rf, OP.add), rd=[name, "rf"], wr=[name])
        s.op("act", lambda e, dst=dst: e.activation(dst, dst, AF.Sin, scale=TWO_PI), rd=[name], wr=[name])


def rope(k, c, n, src_ps, nh, A, B, dst, rd, wr):
    s = k.s
    cosb = bc(c["cos"][:, n, :], [[0, nh], [0, 2], [1, 48]])
    sinb = bc(c["sin"][:, n, :], [[0, nh], [0, 2], [1, 48]])
    v4 = lambda ap: ap.rearrange("p (h t d) -> p h t d", h=nh, t=2)
    s.op("dve", lambda e: e.tensor_tensor(v4(A), v4(src_ps), cosb, OP.mult), rd=rd + ["cos"], wr=["ropeA"])
    s.op("dve", lambda e: e.tensor_tensor(v4(B), v4(src_ps), sinb, OP.mult), rd=rd + ["sin"], wr=["ropeB"])
    s.op("dve", lambda e: e.tensor_tensor(v4(dst)[:, :, 0, :], v4(A)[:, :, 0, :], v4(B)[:, :, 1, :], OP.subtract),
         rd=["ropeA", "ropeB"], wr=[wr])
    s.op("dve", lambda e: e.tensor_tensor(v4(dst)[:, :, 1, :], v4(A)[:, :, 1, :], v4(B)[:, :, 0, :], OP.add),
         rd=["ropeA", "ropeB"], wr=[wr])


def load_l1_weights(k, w_in_b_bf, w_out1_bf):
    s = k.s
    Wb = k.sb("Wb", [128, 8, 2560], BF16)
    for kk in range(8):
        s.dma("sp", Wb[:, kk, :], w_in_b_bf[kk * 128:(kk + 1) * 128, :], rd=[w_in_b_bf.name], wr=["Wb"])
    return Wb


def phase_scan(k, c, X1, Wb, Floc_out, Sbend_out, SbAll, pfx="X1", u="", need_f=True):
    s, ps = k.s, k.ps
    k.banks = [0, 1, 2, 3, 4, 5, 6, 7]
    X = k.sb("Xs" + u, [128, 1024], F32)
    xT = k.sb("xTs" + u, [128, 8, 128], BF16)
    A, B = c["A"], c["B"]
    kr = k.sb("kr" + u, [128, 384], F32)
    kb = k.sb("kb" + u, [128, 4, 96], BF16)
    kf = k.sb("kf" + u, [128, 4, 96], BF16)
    vb = k.sb("vb" + u, [128, 768], BF16)
    Sb = k.sb("Sb" + u, [96, 4, 192], F32)
    Fl = k.sb("Fl" + u, [96, 4, 192], F32)
    s.op("pool", lambda e: e.memset(Sb, 0.0), wr=["Sb"])
    s.op("pool", lambda e: e.memset(Fl, 0.0), wr=["Fl"])
    fac, g128, pw = c["fac"], c["g128"], c["pw"]
    for n in range(31, -1, -1):
        s.dma("sp", X, X1[n * 128:(n + 1) * 128, :], rd=[pfx + "_%d" % n], wr=["Xs"])
        k.transpose_tok(X, "Xs", xT, "xTs", 0)
        bk = k.bank()
        s.mm(ps[:, bk, 0:384], "ps%d" % bk, [(xT[:, kk, :], Wb[:, kk, 384:768]) for kk in range(8)], ["xTs", "Wb"])
        rope(k, c, n, ps[:, bk, 0:384], 4, A[:, 0:384], B[:, 0:384], kr, ["ps%d" % bk], "kr")
        for h2 in range(2):
            b = k.bank()
            s.mm(ps[:, b, 0:384], "ps%d" % b, [(xT[:, kk, :], Wb[:, kk, 768 + h2 * 384:768 + (h2 + 1) * 384]) for kk in range(8)],
                 ["xTs", "Wb"])
            s.op("act", lambda e, b=b, h2=h2: e.copy(vb[:, h2 * 384:(h2 + 1) * 384], ps[:, b, 0:384]), rd=["ps%d" % b], wr=["vb"])
        kr3 = kr.rearrange("p (h d) -> p h d", h=4)
        s.op("dve", lambda e: e.tensor_tensor(kb, kr3, bc(fac[:, 3, :], [[1, 4], [0, 96]]), OP.mult), rd=["kr", "fac"], wr=["kb"])
        if need_f:
            s.op("dve", lambda e: e.tensor_tensor(kf, kr3, bc(fac[:, 2, :], [[1, 4], [0, 96]]), OP.mult), rd=["kr", "fac"], wr=["kf"])
        s.op("act", lambda e, n=n: e.copy(SbAll[:, n, :], Sb.rearrange("p h d -> p (h d)")), rd=["Sb"], wr=["SbAll"])
        for (kx, kres, dirn) in (((kb, "kb", 1), (kf, "kf", 0)) if need_f else ((kb, "kb", 1),)):
            for hp in range(2):
                b = k.bank()
                for h2 in range(2):
                    h = hp * 2 + h2
                    s.mm(ps[0:96, b, h2 * 192:(h2 + 1) * 192], "ps%d" % b, [(kx[:, h, :], vb[:, h * 192:(h + 1) * 192])], [kres, "vb"])
                for h2 in range(2):
                    h = hp * 2 + h2
                    if dirn == 1:
                        s.op("dve", lambda e, b=b, h=h, h2=h2: e.scalar_tensor_tensor(
                            Sb[:, h, :], Sb[:, h, :], g128[0:96, 4 + h:5 + h], ps[0:96, b, h2 * 192:(h2 + 1) * 192], OP.mult, OP.add),
                            rd=["ps%d" % b, "Sb", "g128"], wr=["Sb"])
                    else:
                        s.op("dve", lambda e, b=b, h=h, h2=h2, n=n: e.scalar_tensor_tensor(
                            Fl[:, h, :], ps[0:96, b, h2 * 192:(h2 + 1) * 192], pw[0:96, h, n:n + 1], Fl[:, h, :], OP.mult, OP.add),
                            rd=["ps%d" % b, "Fl", "pw"], wr=["Fl"])
    if need_f:
        s.dma("sp", Floc_out, Fl.rearrange("p h d -> p (h d)"), rd=["Fl"], wr=["Floc_out" + u])
    s.dma("sp", Sbend_out, Sb.rearrange("p h d -> p (h d)"), rd=["Sb"], wr=["Sbend_out" + u])


def phase_l1(k, c, X1, Wb, memkT, vmem, w_out1_bf, Fp, Bp, SbAll, gn_g, gn_b, ln_g, ln_b, w_router, b_router, X1N, X1NT, GT, nch=32, masks=None, st_rd=()):
    s, ps = k.s, k.ps
    k.banks = [0, 1, 2, 3, 4, 5, 6, 7]
    fac, g128, pw, maskT = c["fac"], c["g128"], c["pw"], c["maskT"]
    Wo = k.sb("Wo1", [128, 8, 1024], BF16)
    s.dma("sp", Wo, w_out1_bf.rearrange("(k p) n -> p k n", p=128), rd=[w_out1_bf.name], wr=["Wo1"])
    gng = k.sb("gng", [128, 768], F32)
    gnb = k.sb("gnb", [128, 768], F32)
    s.dma("sp", gng, gn_g.partition_broadcast(128), wr=["lnc"])
    s.dma("sp", gnb, gn_b.partition_broadcast(128), wr=["lnc"])
    Lg = k.sb("L1g", [128, 1024], F32)
    Lb = k.sb("L1b", [128, 1024], F32)
    s.dma("sp", Lg, ln_g.partition_broadcast(128), wr=["lnc"])
    s.dma("sp", Lb, ln_b.partition_broadcast(128), wr=["lnc"])
    Wr = k.sb("Wr", [128, 8, 8], F32)
    s.dma("sp", Wr, w_router.rearrange("(k p) e -> p k e", p=128), wr=["Wr"])
    brt = k.sb("brt", [128, 8], F32)
    s.dma("sp", brt, b_router.partition_broadcast(128), wr=["brt"])
    Sf = k.sb("Sf", [96, 4, 192], F32)
    BkP = k.sb("BkP", [96, 4, 192], F32)
    s.dma("sp", Sf.rearrange("p h d -> p (h d)"), Fp, rd=list(st_rd), wr=["Sf"])
    s.dma("sp", BkP.rearrange("p h d -> p (h d)"), Bp, rd=list(st_rd), wr=["BkP"])
    if masks is not None:
        mk = k.sb("mk", [96, 2], F32)
        s.dma("sp", mk, masks.partition_broadcast(96), wr=["mk"])
        s.op("dve", lambda e: e.tensor_scalar(Sf.rearrange("p h d -> p (h d)"), Sf.rearrange("p h d -> p (h d)"), mk[:, 0:1], None, OP.mult),
             rd=["Sf", "mk"], wr=["Sf"])
        s.op("dve", lambda e: e.tensor_scalar(BkP.rearrange("p h d -> p (h d)"), BkP.rearrange("p h d -> p (h d)"), mk[:, 1:2], None, OP.mult),
             rd=["BkP", "mk"], wr=["BkP"])
    XX = [k.sb("Xf%d" % i, [128, 1024], F32) for i in range(2)]
    xT = k.sb("xTf", [128, 8, 128], BF16)
    xT32 = k.sb("xT32", [128, 8, 128], F32)
    A, B = c["A"], c["B"]
    qk = k.sb("qk", [128, 768], F32)
    KF = [k.sb("kf1_%d" % i, [128, 4, 96], BF16) for i in range(2)]
    VB = [k.sb("vb1_%d" % i, [128, 768], BF16) for i in range(2)]
    SG = [k.sb("sgl%d" % i, [128, 768], F32) for i in range(2)]
    QT = [k.sb("qkT%d" % i, [96, 8, 128], BF16) for i in range(2)]
    STT = [k.sb("ST%d" % i, [128, 4, 128], BF16) for i in range(2)]
    Sfb = k.sb("Sfb", [96, 4, 192], BF16)
    SbT = k.sb("SbT", [96, 4, 192], BF16)
    o = k.sb("o", [128, 4, 192], F32)
    hst = k.sb("hst", [128, 4, 6], F32)
    hmv = k.sb("hmv", [128, 4, 2], F32)
    hrs = k.sb("hrs", [128, 4], F32)
    mixT = k.sb("mixT1", [128, 6, 128], BF16)
    MQ = [k.sb("mqT1_%d" % i, [128, 2, 128], BF16) for i in range(2)]
    xTo = k.sb("xTo", [128, 8, 128], BF16)
    memoT = k.sb("memoT1", [128, 2, 128], BF16)
    probs = k.sb("probs1", [128, 4, 256], F32)
    probsT = k.sb("probsT1", [128, 4, 2, 128], BF16)
    rmax = k.sb("rmax1", [128, 4], F32)
    rsum = k.sb("rsum1", [128, 4], F32)
    lgt = k.sb("lgt", [128, 8], F32)
    l2 = k.sb("l2", [128, 8], F32)
    eq1 = k.sb("eq1", [128, 8], F32)
    eq2 = k.sb("eq2", [128, 8], F32)
    m12 = k.sb("m12", [128, 4], F32)
    def stA(n):
        p = n % 2
        X, vb, sgl, qkT, ST, kf, mqT = XX[p], VB[p], SG[p], QT[p], STT[p], KF[p], MQ[p]
        rX, rvb, rsgl, rqkT, rST, rkf, rmq = "Xf%d" % p, "vb1_%d" % p, "sgl%d" % p, "qkT%d" % p, "ST%d" % p, "kf1_%d" % p, "mqT1_%d" % p
        s.dma("sp", X, X1[n * 128:(n + 1) * 128, :], rd=["X1_%d" % n], wr=[rX])
        yield
        k.transpose_tok(X, rX, xT, "xTf", 0)
        yield
        for h2 in range(2):
            b = k.bank()
            s.mm(ps[:, b, 0:384], "ps%d" % b, [(xT[:, kk, :], Wb[:, kk, h2 * 384:(h2 + 1) * 384]) for kk in range(8)], ["xTf", "Wb"])
            rope(k, c, n, ps[:, b, 0:384], 4, A[:, 0:384], B[:, 0:384], qk[:, h2 * 384:(h2 + 1) * 384], ["ps%d" % b], "qk")
        yield
        for h2 in range(2):
            b = k.bank()
            s.mm(ps[:, b, 0:384], "ps%d" % b, [(xT[:, kk, :], Wb[:, kk, 768 + h2 * 384:768 + (h2 + 1) * 384]) for kk in range(8)],
                 ["xTf", "Wb"])
            s.op("act", lambda e, b=b, h2=h2: e.copy(vb[:, h2 * 384:(h2 + 1) * 384], ps[:, b, 0:384]), rd=["ps%d" % b], wr=[rvb])
        for h2 in range(2):
            b = k.bank()
            s.mm(ps[:, b, 0:384], "ps%d" % b, [(xT[:, kk, :], Wb[:, kk, 1536 + h2 * 384:1536 + (h2 + 1) * 384]) for kk in range(8)],
                 ["xTf", "Wb"])
            s.op("act", lambda e, b=b, h2=h2: e.activation(sgl[:, h2 * 384:(h2 + 1) * 384], ps[:, b, 0:384], AF.Silu),
                 rd=["ps%d" % b], wr=[rsgl])
        for t in range(2):
            b = k.bank()
            s.mm(ps[:, b, 0:128], "ps%d" % b, [(Wb[:, kk, 2304 + t * 128:2304 + (t + 1) * 128], xT[:, kk, :]) for kk in range(8)],
                 ["xTf", "Wb"])
            k.evac(mqT[:, t, :], ps[:, b, 0:128], rd=["ps%d" % b], wr=[rmq])
        for hh in range(2):
            b = k.bank()
            s.tr([(ps[0:96, b, i * 128:(i + 1) * 128], qk[:, (hh * 4 + i) * 96:(hh * 4 + i + 1) * 96]) for i in range(4)],
                 "ps%d" % b, ["qk"], k.ident)
            k.evac(qkT[:, hh * 4:(hh + 1) * 4, :].rearrange("p h t -> p (h t)"), ps[0:96, b, :], rd=["ps%d" % b], wr=[rqkT])
        yield
        k3 = qk[:, 384:768].rearrange("p (h d) -> p h d", h=4)
        s.op("dve", lambda e: e.tensor_tensor(kf, k3, bc(fac[:, 2, :], [[1, 4], [0, 96]]), OP.mult), rd=["qk", "fac"], wr=[rkf])
        b = k.bank()
        for h in range(4):
            s.mm(ps[:, b, h * 128:(h + 1) * 128], "ps%d" % b, [(qkT[:, 4 + h, :], qkT[:, h, :])], [rqkT])
        s.op("dve", lambda e, b=b: e.tensor_tensor(ST.rearrange("p h i -> p (h i)"), ps[:, b, :], maskT.rearrange("p h i -> p (h i)"), OP.mult),
             rd=["ps%d" % b, "maskT"], wr=[rST])
        s.op("act", lambda e: e.copy(Sfb.rearrange("p h d -> p (h d)"), Sf.rearrange("p h d -> p (h d)")), rd=["Sf"], wr=["Sfb"])
        for h in range(4):
            s.op("dve", lambda e, h=h, n=n: e.scalar_tensor_tensor(SbT[:, h, :], BkP[:, h, :], pw[0:96, 4 + h, n:n + 1],
                                                                  SbAll[:, n, h * 192:(h + 1) * 192], OP.mult, OP.add),
                 rd=["BkP", "pw", "SbAll"], wr=["SbT"])

    def stB(n):
        p = n % 2
        X, vb, sgl, qkT, ST, kf, mqT = XX[p], VB[p], SG[p], QT[p], STT[p], KF[p], MQ[p]
        rX, rvb, rsgl, rqkT, rST, rkf, rmq = "Xf%d" % p, "vb1_%d" % p, "sgl%d" % p, "qkT%d" % p, "ST%d" % p, "kf1_%d" % p, "mqT1_%d" % p
        for hp in range(2):
            bi_, bf_, bb_ = k.bank(), k.bank(), k.bank()
            for h2 in range(2):
                h = hp * 2 + h2
                cs = slice(h2 * 192, (h2 + 1) * 192)
                s.mm(ps[:, bi_, cs], "ps%d" % bi_, [(ST[:, h, :], vb[:, h * 192:(h + 1) * 192])], [rST, rvb])
                s.mm(ps[:, bf_, cs], "ps%d" % bf_, [(qkT[:, h, :], Sfb[:, h, :])], [rqkT, "Sfb"])
                s.mm(ps[:, bb_, cs], "ps%d" % bb_, [(qkT[:, h, :], SbT[:, h, :])], [rqkT, "SbT"])
            s.op("act", lambda e, hp=hp, bi_=bi_: e.copy(o[:, hp * 2:(hp + 1) * 2, :].rearrange("p h d -> p (h d)"), ps[:, bi_, 0:384]),
                 rd=["ps%d" % bi_], wr=["o"])
            for h2 in range(2):
                h = hp * 2 + h2
                cs = slice(h2 * 192, (h2 + 1) * 192)
                s.op("dve", lambda e, h=h, cs=cs, bf_=bf_: e.scalar_tensor_tensor(o[:, h, :], ps[:, bf_, cs], fac[:, 0, h:h + 1], o[:, h, :],
                                                                              OP.mult, OP.add), rd=["ps%d" % bf_, "o", "fac"], wr=["o"])
                s.op("dve", lambda e, h=h, cs=cs, bb_=bb_: e.scalar_tensor_tensor(o[:, h, :], ps[:, bb_, cs], fac[:, 1, h:h + 1], o[:, h, :],
                                                                              OP.mult, OP.add), rd=["ps%d" % bb_, "o", "fac"], wr=["o"])
        yield
        for hp in range(2):
            b = k.bank()
            for h2 in range(2):
                h = hp * 2 + h2
                s.mm(ps[0:96, b, h2 * 192:(h2 + 1) * 192], "ps%d" % b, [(kf[:, h, :], vb[:, h * 192:(h + 1) * 192])], [rkf, rvb])
            for h2 in range(2):
                h = hp * 2 + h2
                s.op("dve", lambda e, b=b, h=h, h2=h2: e.scalar_tensor_tensor(
                    Sf[:, h, :], Sf[:, h, :], g128[0:96, h:h + 1], ps[0:96, b, h2 * 192:(h2 + 1) * 192], OP.mult, OP.add),
                    rd=["ps%d" % b, "Sf", "g128", "Sfb"], wr=["Sf"])
        for h in range(4):
            s.op("dve", lambda e, h=h: e.bn_stats(hst[:, h, :], o[:, h, :]), rd=["o"], wr=["hst"])
        for h in range(4):
            s.op("dve", lambda e, h=h: e.bn_aggr(hmv[:, h, :], hst[:, h, :]), rd=["hst"], wr=["hmv"])
        s.op("act", lambda e: e.activation(hrs, hmv[:, :, 1], AF.Ln, bias=EPS), rd=["hmv"], wr=["hrs"])
        s.op("act", lambda e: e.activation(hrs, hrs, AF.Exp, scale=-0.5), rd=["hrs"], wr=["hrs"])
        for h in range(4):
            s.op("dve", lambda e, h=h: e.tensor_scalar(o[:, h, :], o[:, h, :], hmv[:, h, 0:1], hrs[:, h:h + 1], OP.subtract, OP.mult),
                 rd=["o", "hmv", "hrs"], wr=["o"])
        of = o.rearrange("p h d -> p (h d)")
        s.op("dve", lambda e: e.tensor_tensor(of, of, gng, OP.mult), rd=["o", "lnc"], wr=["o"])
        s.op("dve", lambda e: e.tensor_tensor(of, of, gnb, OP.add), rd=["o", "lnc"], wr=["o"])
        s.op("dve", lambda e: e.tensor_tensor(of, of, sgl, OP.mult), rd=["o", rsgl], wr=["o"])
        yield
        k.transpose_tok(of, "o", mixT, "mixT1", 0, nk=6)
        mem_attn_a(k, mqT, rmq, memkT, vmem, memoT, "memoT1", 0, (probs, probsT, rmax, rsum))
        yield
        mem_attn_b(k, mqT, rmq, memkT, vmem, memoT, "memoT1", 0, (probs, probsT, rmax, rsum))
        yield
        for nh in range(2):
            b = k.bank()
            s.mm(ps[:, b, :], "ps%d" % b,
                 [(mixT[:, g, :], Wo[:, g, nh * 512:(nh + 1) * 512]) for g in range(6)] +
                 [(memoT[:, t, :], Wo[:, 6 + t, nh * 512:(nh + 1) * 512]) for t in range(2)],
                 ["mixT1", "memoT1", "Wo1"])
            s.op("dve", lambda e, b=b, nh=nh: e.scalar_tensor_tensor(
                X[:, nh * 512:(nh + 1) * 512], X[:, nh * 512:(nh + 1) * 512], ALPHA, ps[:, b, :], OP.mult, OP.add),
                rd=["ps%d" % b, rX], wr=[rX])
        k.ln_inplace(X, rX, Lg, Lb)
        s.dma("sp", X1N[n * 128:(n + 1) * 128, :], X, rd=[rX], wr=["X1N_%d" % n])
        yield
        for hh in range(2):
            b = k.bank()
            s.tr([(ps[:, b, i * 128:(i + 1) * 128], X[:, (hh * 4 + i) * 128:(hh * 4 + i + 1) * 128]) for i in range(4)],
                 "ps%d" % b, [rX], k.ident)
            s.op("dve", lambda e, b=b, hh=hh: e.tensor_copy(xT32[:, hh * 4:(hh + 1) * 4, :].rearrange("p k t -> p (k t)"), ps[:, b, :]),
                 rd=["ps%d" % b], wr=["xT32"])
            s.op("act", lambda e, hh=hh: e.copy(xTo[:, hh * 4:(hh + 1) * 4, :].rearrange("p k t -> p (k t)"),
                                               xT32[:, hh * 4:(hh + 1) * 4, :].rearrange("p k t -> p (k t)")),
                 rd=["xT32"], wr=["xTo"])
        s.dma("sp", X1NT[n], xTo.rearrange("p k t -> p (k t)"), rd=["xTo"], wr=["X1NT_%d" % n])
        yield
        b = k.bank()
        s.mm(ps[:, b, 0:8], "ps%d" % b, [(xT32[:, kk, :], Wr[:, kk, :]) for kk in range(8)], ["xT32", "Wr"])
        s.op("dve", lambda e, b=b: e.tensor_tensor(lgt, ps[:, b, 0:8], brt, OP.add), rd=["ps%d" % b, "brt"], wr=["lgt"])
        s.op("dve", lambda e: e.tensor_reduce(m12[:, 0:1], lgt, AX.X, OP.max), rd=["lgt"], wr=["m12"])
        s.op("dve", lambda e: e.tensor_scalar(eq1, lgt, m12[:, 0:1], None, OP.is_equal), rd=["lgt", "m12"], wr=["eq1"])
        s.op("dve", lambda e: e.scalar_tensor_tensor(l2, eq1, -1e30, lgt, OP.mult, OP.add), rd=["eq1", "lgt"], wr=["l2"])
        s.op("dve", lambda e: e.tensor_reduce(m12[:, 1:2], l2, AX.X, OP.max), rd=["l2"], wr=["m12"])
        s.op("dve", lambda e: e.tensor_scalar(eq2, l2, m12[:, 1:2], None, OP.is_equal), rd=["l2", "m12"], wr=["eq2"])
        s.op("dve", lambda e: e.tensor_tensor(m12[:, 2:3], m12[:, 1:2], m12[:, 0:1], OP.subtract), rd=["m12"], wr=["m12"])
        s.op("act", lambda e: e.activation(m12[:, 2:3], m12[:, 2:3], AF.Exp), rd=["m12"], wr=["m12"])
        s.op("dve", lambda e: e.tensor_scalar(m12[:, 3:4], m12[:, 2:3], 1.0, None, OP.add), rd=["m12"], wr=["m12"])
        s.op("dve", lambda e: e.reciprocal(m12[:, 3:4], m12[:, 3:4]), rd=["m12"], wr=["m12"])
        s.op("dve", lambda e: e.tensor_tensor(m12[:, 2:3], m12[:, 2:3], m12[:, 3:4], OP.mult), rd=["m12"], wr=["m12"])
        s.op("dve", lambda e: e.tensor_scalar(eq1, eq1, m12[:, 3:4], None, OP.mult), rd=["eq1", "m12"], wr=["eq1"])
        s.op("dve", lambda e, n=n: e.scalar_tensor_tensor(GT[:, n, :], eq2, m12[:, 2:3], eq1, OP.mult, OP.add),
             rd=["eq1", "eq2", "m12"], wr=["GT"])


    for _ in stA(0):
        pass
    for n in range(nch):
        gens = {"A": stA(n + 1) if n + 1 < nch else None, "B": stB(n)}
        for ch in "ABABABABBABB":
            g = gens[ch]
            if g is not None:
                try:
                    next(g)
                except StopIteration:
                    gens[ch] = None
        for ch in "BA":
            g = gens[ch]
            while g is not None:
                try:
                    next(g)
                except StopIteration:
                    g = None


def phase_moe(k, X1N, X1NT, GT, weg_bf, weu_bf, wed_bf, ln_g, ln_b, out, nexp=8, ecast=None):
    s, ps = k.s, k.ps
    k.banks = [0, 1, 2, 3, 4, 5, 6, 7]
    XNT = k.sb("XNT", [128, 16, 8, 128], BF16)
    facc = k.sb("facc", [128, 16, 1024], F32)
    WG = [k.sb("EG%d" % i, [128, 8, 512], BF16) for i in range(2)]
    WU = [k.sb("EU%d" % i, [128, 8, 512], BF16) for i in range(2)]
    WD = [k.sb("ED%d" % i, [128, 4, 1024], BF16) for i in range(2)]
    sgt = [k.sb("esg%d" % i, [128, 512], BF16) for i in range(2)]
    hT = [k.sb("ehT%d" % i, [128, 4, 512], BF16) for i in range(2)]
    Lg = k.sb("L2g", [128, 1024], F32)
    Lb = k.sb("L2b", [128, 1024], F32)
    s.dma("sp", Lg, ln_g.partition_broadcast(128), wr=["lnc"])
    s.dma("sp", Lb, ln_b.partition_broadcast(128), wr=["lnc"])
    Xr = [k.sb("Xr%d" % i, [128, 1024], F32) for i in range(2)]
    xrc = [0]

    def epilogue(hh, sub):
        gsub = hh * 16 + sub
        xr = Xr[xrc[0] % 2]
        rn = "Xr%d" % (xrc[0] % 2)
        xrc[0] += 1
        s.dma("sp", xr, X1N[gsub * 128:(gsub + 1) * 128, :], rd=["X1N_%d" % gsub], wr=[rn])
        s.op("dve", lambda e: e.scalar_tensor_tensor(xr, xr, ALPHA, facc[:, sub, :], OP.mult, OP.add),
             rd=[rn, "facc%d" % sub], wr=[rn])
        k.ln_inplace(xr, rn, Lg, Lb)
        s.dma("sp", out[gsub * 128:(gsub + 1) * 128, :], xr, rd=[rn], wr=["out_%d" % gsub])

    def down(hh, ex, blk, tt, jb, hb):
        first = (ex == 0 and blk == 0)
        if first and hh == 1:
            for s4 in range(4):
                epilogue(0, tt * 4 + s4)
        for s4 in range(4):
            sub = tt * 4 + s4
            gsub = hh * 16 + sub
            for nh in range(2):
                b = k.bank()
                s.mm(ps[:, b, :], "ps%d" % b,
                     [(hT[hb][:, t4, s4 * 128:(s4 + 1) * 128], WD[jb][:, t4, nh * 512:(nh + 1) * 512]) for t4 in range(4)],
                     ["ehT%d" % hb, "ED%d" % jb])
                if first:
                    s.op("dve", lambda e, b=b, sub=sub, gsub=gsub, nh=nh, ex=ex: e.tensor_scalar(
                        facc[:, sub, nh * 512:(nh + 1) * 512], ps[:, b, :], GT[:, gsub, ex:ex + 1], None, OP.mult),
                        rd=["ps%d" % b, "GT", "facc%d" % sub], wr=["facc%d" % sub])
                else:
                    s.op("dve", lambda e, b=b, sub=sub, gsub=gsub, nh=nh, ex=ex: e.scalar_tensor_tensor(
                        facc[:, sub, nh * 512:(nh + 1) * 512], ps[:, b, :], GT[:, gsub, ex:ex + 1],
                        facc[:, sub, nh * 512:(nh + 1) * 512], OP.mult, OP.add),
                        rd=["ps%d" % b, "GT", "facc%d" % sub], wr=["facc%d" % sub])
        if hh == 1 and ex == nexp - 1 and blk == 6:
            for s4 in range(4):
                epilogue(1, tt * 4 + s4)

    wc = 0
    hc = 0
    pend = None
    for hh in range(2):
        for cc in range(16):
            s.dma("sp", XNT[:, cc, :, :].rearrange("p k t -> p (k t)"), X1NT[hh * 16 + cc],
                  rd=["X1NT_%d" % (hh * 16 + cc)], wr=["XNT%d" % (cc // 4)])
        for ex in range(nexp):
            nxt = list(ecast[ex + 1]) if (ecast is not None and hh == 0 and ex + 1 < nexp) else []
            for blk in range(7):
                jb = wc % 2
                wc += 1
                s.dma("sp", WG[jb], weg_bf[ex * 1024:(ex + 1) * 1024, blk * 512:(blk + 1) * 512].rearrange("(k p) n -> p k n", p=128),
                      rd=[weg_bf.name + "_%d" % (ex * 2048 + i * 512) for i in range(4)], wr=["EG%d" % jb])
                s.dma("sp", WU[jb], weu_bf[ex * 1024:(ex + 1) * 1024, blk * 512:(blk + 1) * 512].rearrange("(k p) n -> p k n", p=128),
                      rd=[weu_bf.name + "_%d" % (ex * 2048 + i * 512) for i in range(4)], wr=["EU%d" % jb])
                s.dma("sp", WD[jb], wed_bf[ex * 3584 + blk * 512:ex * 3584 + (blk + 1) * 512, :].rearrange("(t p) n -> p t n", p=128),
                      rd=[wed_bf.name + "_%d" % (ex * 3584 + blk * 512)], wr=["ED%d" % jb])
                for tt in range(4):
                    hb = hc % 2
                    hc += 1
                    for t4 in range(4):
                        bg, bu = k.bank(), k.bank()
                        xs = lambda kk: XNT[:, tt * 4:(tt + 1) * 4, kk, :]
                        s.mm(ps[:, bg, :].rearrange("p (c t) -> p c t", c=4), "ps%d" % bg,
                             [(WG[jb][:, kk, t4 * 128:(t4 + 1) * 128], xs(kk)) for kk in range(8)], ["EG%d" % jb, "XNT%d" % tt])
                        s.mm(ps[:, bu, :].rearrange("p (c t) -> p c t", c=4), "ps%d" % bu,
                             [(WU[jb][:, kk, t4 * 128:(t4 + 1) * 128], xs(kk)) for kk in range(8)], ["EU%d" % jb, "XNT%d" % tt])
                        s.op("act", lambda e, bg=bg, t4=t4: e.activation(sgt[t4 % 2], ps[:, bg, :], AF.Silu), rd=["ps%d" % bg], wr=["esg%d" % (t4 % 2)])
                        s.op("dve", lambda e, bu=bu, t4=t4, hb=hb: e.tensor_tensor(hT[hb][:, t4, :], sgt[t4 % 2], ps[:, bu, :], OP.mult),
                             rd=["ps%d" % bu, "esg%d" % (t4 % 2)], wr=["ehT%d" % hb])
                    if pend is not None:
                        down(*pend)
                    pend = (hh, ex, blk, tt, jb, hb)
                    if tt == 1:
                        for _ in range((2, 2, 2, 2, 3, 2, 2)[blk]):
                            if nxt:
                                nxt.pop(0)(rd=["ehT%d" % hb])
    down(*pend)


def build_F():
    k = K()
    x = k.din("x", [4096, 1024]); xo = k.din("xo", [4096, 1024])
    mem = k.din("mem", [256, 1024]); wkv = k.din("w_mem_kv", [1024, 512])
    w_in_a = k.din("w_in_a", [1024, 1792]); sg_ln_g = k.din("sg_ln_g", [768]); sg_ln_b = k.din("sg_ln_b", [768])
    sg_w = k.din("sg_w", [8, 128, 128]); sg_b = k.din("sg_b", [8, 128])
    w_out = k.din("w_out", [2, 1024, 1024]); ln_g = k.din("ln_g", [2, 2, 1024]); ln_b = k.din("ln_b", [2, 2, 1024])
    wg = k.din("w_ff_gate", [1024, 2816]); wu = k.din("w_ff_up", [1024, 2816]); wd = k.din("w_ff_down", [2816, 1024])
    w_in_b = k.din("w_in_b", [1024, 2560]); dlf = k.din("decay_logit_f", [4]); dlb = k.din("decay_logit_b", [4])
    gn_g = k.din("ret_gn_g", [768]); gn_b = k.din("ret_gn_b", [768])
    w_router = k.din("w_router", [1024, 8]); b_router = k.din("b_router", [8])
    weg = k.din("w_e_gate", [8, 1024, 3584]); weu = k.din("w_e_up", [8, 1024, 3584]); wed = k.din("w_e_down", [8, 3584, 1024])
    posb = k.din("posb", [1]); posbo = k.din("posbo", [1]); masks = k.din("masks", [2])
    out = k.dout("out", [4096, 1024])
    X1 = k.dscr("X1", [4096, 1024], F32); X1o = k.dscr("X1o", [4096, 1024], F32)
    st = k.dscr("st_oth", [192, 768], F32); st2 = k.dscr("st_own", [192, 768], F32)
    wkv_bf = k.dscr("wkv_bf", [1024, 512], BF16); w_in_bf = k.dscr("w_in_a_bf", [1024, 1792], BF16)
    w_out_bf = k.dscr("w_out_bf", [2048, 1024], BF16)
    wg_bf = k.dscr("wg_bf", [1024, 2816], BF16); wu_bf = k.dscr("wu_bf", [1024, 2816], BF16); wd_bf = k.dscr("wd_bf", [2816, 1024], BF16)
    w_in_b_bf = k.dscr("w_in_b_bf", [1024, 2560], BF16)
    weg_bf = k.dscr("weg_bf", [8 * 1024, 3584], BF16); weu_bf = k.dscr("weu_bf", [8 * 1024, 3584], BF16)
    wed_bf = k.dscr("wed_bf", [8 * 3584, 1024], BF16)
    X1N = k.dscr("X1N", [4096, 1024], F32); X1NT = k.dscr("X1NT", [32, 128, 1024], BF16)
    k.cast_dram(w_in_bf, w_in_a); k.cast_dram(wkv_bf, wkv)
    k.cast_dram(w_out_bf, w_out.rearrange("a k n -> (a k) n"))
    k.cast_dram(wg_bf.rearrange("k (a n) -> (k a) n", a=2), wg.rearrange("k (a n) -> (k a) n", a=2))
    k.cast_dram(wu_bf.rearrange("k (a n) -> (k a) n", a=2), wu.rearrange("k (a n) -> (k a) n", a=2))
    k.cast_dram(wd_bf, wd)
    late = [lambda: k.cast_dram(w_in_b_bf.rearrange("k (a n) -> (k a) n", a=2), w_in_b.rearrange("k (a n) -> (k a) n", a=2))]
    memkT, vmem = setup_mem(k, mem, wkv_bf)
    GT = k.sb("GT", [128, 32, 8], F32)
    bgj = []
    g1 = k.cast_jobs(weg_bf.rearrange("k (a n) -> (k a) n", a=2), weg.rearrange("e k (a n) -> (e k a) n", a=2), rows_per=512)
    g2 = k.cast_jobs(weu_bf.rearrange("k (a n) -> (k a) n", a=2), weu.rearrange("e k (a n) -> (e k a) n", a=2), rows_per=512)
    g3 = k.cast_jobs(wed_bf, wed.rearrange("e k n -> (e k) n"), rows_per=512)
    ecast = []
    for i in range(8):
        pcs = []
        for q in range(4):
            pcs += [g1[i * 4 + q], g2[i * 4 + q]]
        pcs += g3[i * 7:(i + 1) * 7]
        ecast.append(pcs)
    c = setup_l1(k, dlf, dlb, posb)
    setup_rope(k, c, posbo)
    with k.scope():
        phase_l0(k, [(xo, X1o, "X1o"), (x, X1, "X1")], memkT, vmem, w_in_bf, w_out_bf[0:1024, :], wg_bf, wu_bf, wd_bf,
                 sg_w, sg_b, sg_ln_g, sg_ln_b, ln_g[0], ln_b[0], bg=late)
    with k.scope():
        Wb = load_l1_weights(k, w_in_b_bf, None)
        SbAll = k.sb("SbAll", [96, 32, 768], BF16)
        for j in ecast[0]:
            j()
        with k.scope():
            phase_scan(k, c, X1o, Wb, st[0:96, :], st[96:192, :], SbAll, pfx="X1o", u="_o")
        setup_rope(k, c, posb)
        with k.scope():
            phase_scan(k, c, X1, Wb, st2[0:96, :], st2[96:192, :], SbAll, pfx="X1", u="", need_f=False)
        with k.scope():
            phase_l1(k, c, X1, Wb, memkT, vmem, w_out_bf[1024:2048, :], st[0:96, :], st[96:192, :], SbAll, gn_g, gn_b,
                     ln_g[1, 0], ln_b[1, 0], w_router, b_router, X1N, X1NT, GT, masks=masks, st_rd=["Floc_out_o", "Sbend_out_o"])
    with k.scope():
        phase_moe(k, X1N, X1NT, GT, weg_bf, weu_bf, wed_bf, ln_g[1, 1], ln_b[1, 1], out, ecast=ecast)
    k.s.wait_all("sp")
    return k.nc


def kernel(x, mem, w_mem_kv, w_in_a, sg_ln_g, sg_ln_b, sg_w, sg_b, w_in_b, decay_logit_f, decay_logit_b, ret_gn_g, ret_gn_b,
           w_out, ln_g, ln_b, w_ff_gate, w_ff_up, w_ff_down, w_router, b_router, w_e_gate, w_e_up, w_e_down):
    f = lambda a: np.ascontiguousarray(np.asarray(a, dtype=np.float32))
    x = f(x); mem = f(mem)
    n = 8
    common = dict(w_mem_kv=f(w_mem_kv), w_in_a=f(w_in_a)[0], sg_ln_g=f(sg_ln_g)[0], sg_ln_b=f(sg_ln_b)[0], sg_w=f(sg_w)[0],
                  sg_b=f(sg_b)[0], w_out=f(w_out), ln_g=f(ln_g), ln_b=f(ln_b), w_ff_gate=f(w_ff_gate)[0], w_ff_up=f(w_ff_up)[0],
                  w_ff_down=f(w_ff_down)[0], w_in_b=f(w_in_b)[0], decay_logit_f=f(decay_logit_f)[0], decay_logit_b=f(decay_logit_b)[0],
                  ret_gn_g=f(ret_gn_g)[0], ret_gn_b=f(ret_gn_b)[0], w_router=f(w_router)[0], b_router=f(b_router)[0],
                  w_e_gate=f(w_e_gate)[0], w_e_up=f(w_e_up)[0], w_e_down=f(w_e_down)[0])
    in_maps = []
    for c in range(n):
        b, hf = c // 2, c % 2
        in_maps.append(dict(common, x=np.ascontiguousarray(x[b, hf * 4096:(hf + 1) * 4096]),
                            xo=np.ascontiguousarray(x[b, (1 - hf) * 4096:(2 - hf) * 4096]),
                            mem=np.ascontiguousarray(mem[b]), posb=np.full([1], float(hf * 4096), np.float32),
                            posbo=np.full([1], float((1 - hf) * 4096), np.float32),
                            masks=np.array([float(hf == 1), float(hf == 0)], np.float32)))
    nc = build_F()
    res = run_bass_kernel_spmd(nc, in_maps, core_ids=list(range(n))).results
    out = np.zeros([4, 8192, 1024], np.float32)
    for c in range(n):
        out[c // 2, (c % 2) * 4096:(c % 2 + 1) * 4096] = res[c]["out"]
    return out
```

```python
import numpy as np
import concourse.bass as bass
import concourse.mybir as mybir

F32 = mybir.dt.float32
BF16 = mybir.dt.bfloat16
I32 = mybir.dt.int32
AF = mybir.ActivationFunctionType
OP = mybir.AluOpType
AX = mybir.AxisListType

NDS = 40
NPS = 8


class S:
    def __init__(self, nc):
        self.nc = nc
        self.eng = dict(pe=nc.tensor, act=nc.scalar, dve=nc.vector, pool=nc.gpsimd, sp=nc.sync)
        self.sem = {k: nc.alloc_semaphore("s_" + k) for k in ("pe", "act", "dve", "pool")}
        self.cnt = {k: 0 for k in self.sem}
        self.dsem = [nc.alloc_semaphore("d%d" % i) for i in range(NDS + NPS)]
        self.dcnt = [0] * (NDS + NPS)
        self.dnext = 0
        self.pnext = 0
        self.waited = {}
        self.w = {}
        self.r = {}
        self.nins = 0

    def _semof(self, sk):
        return self.sem[sk] if isinstance(sk, str) else self.dsem[sk[1]]

    def _wait(self, e, toks):
        best = {}
        for sk, v in toks:
            if sk == e and e == "pe":
                continue
            if best.get(sk, 0) < v:
                best[sk] = v
        for sk, v in best.items():
            if self.waited.get((e, sk), 0) >= v:
                continue
            self.eng[e].wait_ge(self._semof(sk), v)
            self.waited[(e, sk)] = v
            self.nins += 1

    def _deps(self, rd, wr):
        toks = []
        for res in rd:
            if res in self.w:
                toks.append(self.w[res])
        for res in wr:
            if res in self.w:
                toks.append(self.w[res])
            toks.extend(self.r.get(res, ()))
        return toks

    def _record(self, tok, rd, wr):
        for res in wr:
            self.w[res] = tok
            self.r[res] = []
        for res in rd:
            lst = self.r.setdefault(res, [])
            lst[:] = [t for t in lst if t[0] != tok[0]]
            lst.append(tok)

    def op(self, e, fn, rd=(), wr=()):
        self._wait(e, self._deps(rd, wr))
        ins = fn(self.eng[e])
        self.cnt[e] += 1
        ins.then_inc(self.sem[e], 1)
        self.nins += 1
        tok = (e, self.cnt[e])
        self._record(tok, rd, wr)
        return tok

    def mm(self, out, wr, pairs, rd, start=True, stop=True):
        e = "pe"
        self._wait(e, self._deps(rd, [wr]))
        n = len(pairs)
        ins = None
        for i, (a, b) in enumerate(pairs):
            ins = self.nc.tensor.matmul(out, a, b, start=(start and i == 0), stop=(stop and i == n - 1))
            self.nins += 1
        self.cnt[e] += 1
        ins.then_inc(self.sem[e], 1)
        tok = (e, self.cnt[e])
        self._record(tok, rd, [wr])
        return tok

    def tr(self, outs_ins, wr, rd, ident):
        e = "pe"
        self._wait(e, self._deps(list(rd) + ["ident"], [wr]))
        ins = None
        for o, i in outs_ins:
            ins = self.nc.tensor.transpose(o, i, ident)
            self.nins += 1
        self.cnt[e] += 1
        ins.then_inc(self.sem[e], 1)
        tok = (e, self.cnt[e])
        self._record(tok, list(rd) + ["ident"], [wr])
        return tok

    def dma(self, q, out, in_, rd=(), wr=(), **kw):
        if q == "pool":
            i = NDS + self.pnext
            self.pnext = (self.pnext + 1) % NPS
        else:
            i = self.dnext
            self.dnext = (self.dnext + 1) % NDS
        toks = self._deps(rd, wr)
        if self.dcnt[i] > 0:
            toks.append((("d", i), self.dcnt[i]))
        self._wait(q, toks)
        ins = self.eng[q].dma_start(out, in_, **kw)
        self.dcnt[i] += 16
        ins.then_inc(self.dsem[i], 16)
        self.nins += 1
        tok = (("d", i), self.dcnt[i])
        self._record(tok, rd, wr)
        return tok

    def wait_all(self, e):
        toks = [(k, v) for k, v in self.cnt.items() if v > 0]
        toks += [(("d", i), v) for i, v in enumerate(self.dcnt) if v > 0]
        self._wait(e, toks)


from concourse.bass_utils import run_bass_kernel_spmd

ALPHA = float((2.0 * 2) ** 0.25)
EPS = 1e-5
NTOK = 4096


import os
from contextlib import contextmanager, ExitStack


class Stop(Exception):
    pass


class K:
    def ck(self, name):
        if os.environ.get("STOP") == name:
            raise Stop()

    @contextmanager
    def scope(self):
        st = ExitStack()
        self.stacks.append(st)
        try:
            yield
        finally:
            self.barrier()
            self.stacks.pop()
            st.close()

    def barrier(self):
        for e in ("pe", "act", "dve", "pool", "sp"):
            self.s.wait_all(e)

    def __init__(self):
        nc = bass.Bass("TRN2", target_bir_lowering=False)
        self.nc = nc
        self.stacks = [ExitStack()]
        self.s = S(nc)
        self.ps = nc.alloc_psum_tensor("ps", [128, 8, 512], F32).ap()
        self.banks = [0, 1, 2, 3]
        self.bi = 0
        self.ev = 0
        self.uid = 0
        self.ident = self.sb("ident", [128, 128], F32)
        s = self.s
        s.op("pool", lambda e: e.memset(self.ident, 0.0), wr=["ident"])
        s.op("pool", lambda e: e.affine_select(out=self.ident, in_=self.ident, compare_op=OP.not_equal, fill=1.0,
                                               base=0, pattern=[[-1, 128]], channel_multiplier=1),
             rd=["ident"], wr=["ident"])
        self.small = []
        for i in range(4):
            self.small.append((self.sb("st%d" % i, [128, 2, 6], F32), self.sb("mv%d" % i, [128, 2], F32),
                               self.sb("rs%d" % i, [128, 1], F32), "sm%d" % i))
        self.smi = 0

    def din(self, name, shape, dt=F32):
        return self.nc.dram_tensor(name, list(shape), dt, kind="ExternalInput").ap()

    def dout(self, name, shape, dt=F32):
        return self.nc.dram_tensor(name, list(shape), dt, kind="ExternalOutput").ap()

    def dscr(self, name, shape, dt=F32):
        return self.nc.dram_tensor(name, list(shape), dt).ap()

    def sb(self, name, shape, dt):
        return self.stacks[-1].enter_context(self.nc.sbuf_tensor(name, list(shape), dt)).ap()

    def bank(self):
        b = self.banks[self.bi % len(self.banks)]
        self.bi += 1
        return b

    def evac(self, out, in_, rd, wr, eng=None):
        if eng is None:
            eng = ("dve", "act")[self.ev % 2]
            self.ev += 1
        if eng == "act":
            return self.s.op("act", lambda e: e.copy(out, in_), rd=rd, wr=wr)
        return self.s.op(eng, lambda e: e.tensor_copy(out, in_), rd=rd, wr=wr)

    def cast_dram(self, dst, src, rows_per=4096):
        R = src.shape[0]
        for r0 in range(0, R, rows_per):
            r1 = min(R, r0 + rows_per)
            self.s.dma("pool", dst[r0:r1, :], src[r0:r1, :], rd=[], wr=[dst.name])

    def cast_jobs(self, dst, src, rows_per=2048):
        R = src.shape[0]
        jobs = []
        for r0 in range(0, R, rows_per):
            r1 = min(R, r0 + rows_per)
            jobs.append(lambda rd=(), r0=r0, r1=r1: self.s.dma("pool", dst[r0:r1, :], src[r0:r1, :], rd=list(rd), wr=[dst.name + "_%d" % r0]))
        return jobs

    def transpose_tok(self, src, src_res, dstT, dst_res, col0, nk=8):
        s = self.s
        for h in range(0, nk, 4):
            b = self.bank()
            n = min(4, nk - h)
            s.tr([(self.ps[:, b, i * 128:(i + 1) * 128], src[:, (h + i) * 128:(h + i + 1) * 128]) for i in range(n)],
                 "ps%d" % b, [src_res], self.ident)
            self.evac(dstT[:, h:h + n, col0:col0 + 128],
                      self.ps[:, b, 0:n * 128].rearrange("p (k t) -> p k t", k=n), rd=["ps%d" % b], wr=[dst_res], eng="act")

    def ln_inplace(self, X, res, gt, bt, out=None, out_res=None):
        s = self.s
        st, mv, rs, sm = self.small[self.smi % 4]
        self.smi += 1
        W = X.shape[-1]
        nch = (W + 511) // 512
        cw = W // nch
        for c in range(nch):
            s.op("dve", lambda e, c=c: e.bn_stats(st[:, c, :], X[:, c * cw:(c + 1) * cw]), rd=[res], wr=[sm])
        s.op("dve", lambda e: e.bn_aggr(mv, st[:, 0:nch, :].rearrange("p a b -> p (a b)")), rd=[sm], wr=[sm])
        s.op("act", lambda e: e.activation(rs, mv[:, 1:2], AF.Ln, bias=EPS), rd=[sm], wr=[sm])
        s.op("act", lambda e: e.activation(rs, rs, AF.Exp, scale=-0.5), rd=[sm], wr=[sm])
        if gt is None:
            s.op("dve", lambda e: e.tensor_scalar(X, X, mv[:, 0:1], rs, OP.subtract, OP.mult), rd=[res, sm], wr=[res])
        else:
            s.op("dve", lambda e: e.scalar_tensor_tensor(X, X, mv[:, 0:1], gt, OP.subtract, OP.mult), rd=[res, sm, "lnc"], wr=[res])
            s.op("dve", lambda e: e.scalar_tensor_tensor(out if out is not None else X, X, rs, bt, OP.mult, OP.add),
                 rd=[res, sm, "lnc"], wr=[out_res if out is not None else res])


def setup_mem(k, mem, wkv_bf):
    s, ps = k.s, k.ps
    memkT = k.sb("memkT", [128, 2, 256], BF16)
    vmem = k.sb("vmem", [128, 2, 256], BF16)
    with k.scope():
        _setup_mem(k, mem, wkv_bf, memkT, vmem)
    return memkT, vmem


def _setup_mem(k, mem, wkv_bf, memkT, vmem):
    s, ps = k.s, k.ps
    memX = k.sb("memX", [128, 2, 1024], F32)
    memT = k.sb("memT", [128, 8, 256], BF16)
    Wkv = k.sb("Wkv", [128, 8, 512], BF16)
    s.dma("sp", memX, mem.rearrange("(t p) d -> p t d", p=128), wr=["memX"])
    s.dma("sp", Wkv, wkv_bf.rearrange("(k p) n -> p k n", p=128), rd=[wkv_bf.name], wr=["Wkv"])
    for t in range(2):
        k.transpose_tok(memX[:, t, :], "memX", memT, "memT", t * 128)
    for hp in range(2):
        b = k.bank()
        s.mm(ps[:, b, 0:256], "ps%d" % b, [(Wkv[:, kk, hp * 128:(hp + 1) * 128], memT[:, kk, :]) for kk in range(8)],
             ["Wkv", "memT"])
        s.op("act", lambda e, b=b, hp=hp: e.mul(memkT[:, hp, :], ps[:, b, 0:256], 0.125), rd=["ps%d" % b], wr=["memkT"])
    for mt in range(2):
        b = k.bank()
        s.mm(ps[:, b, 0:256], "ps%d" % b,
             [(memT[:, kk, mt * 128:(mt + 1) * 128], Wkv[:, kk, 256:512]) for kk in range(8)], ["Wkv", "memT"])
        k.evac(vmem[:, mt, :], ps[:, b, 0:256], rd=["ps%d" % b], wr=["vmem"])
    k.ck("mem")
    return memkT, vmem


def mem_attn_a(k, mqT, mq_res, memkT, vmem, memoT, memo_res, s_, bufs):
    s, ps = k.s, k.ps
    probs, probsT, rmax, rsum = bufs
    c0 = s_ * 128
    for h2 in range(2):
        b = k.bank()
        for hp in range(2):
            s.mm(ps[:, b, hp * 256:(hp + 1) * 256], "ps%d" % b,
                 [(mqT[h2 * 64:(h2 + 1) * 64, hp, c0:c0 + 128], memkT[h2 * 64:(h2 + 1) * 64, hp, :])],
                 [mq_res, "memkT"])
        s.op("dve", lambda e, b=b, h2=h2: e.tensor_reduce(rmax[:, h2 * 2:(h2 + 1) * 2],
                                                         ps[:, b, :].rearrange("p (h m) -> p h m", h=2),
                                                         AX.X, OP.max, negate=True),
             rd=["ps%d" % b], wr=["rmax"])
        for hp in range(2):
            h = hp * 2 + h2
            s.op("act", lambda e, b=b, h=h, hp=hp, h2=h2: e.activation(probs[:, h, :], ps[:, b, hp * 256:(hp + 1) * 256], AF.Exp,
                                                               bias=rmax[:, h2 * 2 + hp:h2 * 2 + hp + 1], scale=1.0,
                                                               accum_out=rsum[:, h:h + 1]),
                 rd=["ps%d" % b, "rmax"], wr=["probs", "rsum"])
    s.op("dve", lambda e: e.reciprocal(rsum, rsum), rd=["rsum"], wr=["rsum"])
    for h in range(4):
        s.op("act", lambda e, h=h: e.mul(probs[:, h, :], probs[:, h, :], rsum[:, h:h + 1]),
             rd=["probs", "rsum"], wr=["probs"])


def mem_attn_b(k, mqT, mq_res, memkT, vmem, memoT, memo_res, s_, bufs):
    s, ps = k.s, k.ps
    probs, probsT, rmax, rsum = bufs
    c0 = s_ * 128
    for hp in range(2):
        b = k.bank()
        s.tr([(ps[:, b, (h2 * 2 + mt) * 128:(h2 * 2 + mt + 1) * 128], probs[:, hp * 2 + h2, mt * 128:(mt + 1) * 128])
              for h2 in range(2) for mt in range(2)], "ps%d" % b, ["probs"], k.ident)
        k.evac(probsT[:, hp * 2:(hp + 1) * 2, :, :].rearrange("p h m t -> p (h m t)"), ps[:, b, :],
               rd=["ps%d" % b], wr=["probsT"])
    b = k.bank()
    for h in range(4):
        hp, h2 = h // 2, h % 2
        s.mm(ps[h2 * 64:(h2 + 1) * 64, b, hp * 128:(hp + 1) * 128], "ps%d" % b,
             [(vmem[:, mt, h * 64:(h + 1) * 64], probsT[:, h, mt, :]) for mt in range(2)], ["vmem", "probsT"])
    k.evac(memoT[:, :, c0:c0 + 128], ps[:, b, 0:256].rearrange("p (t c) -> p t c", t=2), rd=["ps%d" % b], wr=[memo_res])


def mem_attn(k, mqT, mq_res, memkT, vmem, memoT, memo_res, s_, bufs):
    mem_attn_a(k, mqT, mq_res, memkT, vmem, memoT, memo_res, s_, bufs)
    mem_attn_b(k, mqT, mq_res, memkT, vmem, memoT, memo_res, s_, bufs)


def phase_l0(k, jobs, memkT, vmem, w_in_bf, w_out_bf, wg_bf, wu_bf, wd_bf, sg_w, sg_b, sg_ln_g, sg_ln_b, ln_g, ln_b, ntt=16, bg=None):
    s, ps = k.s, k.ps
    T = 256
    Win = k.sb("Win", [128, 8, 1792], BF16)
    Wom = k.sb("Wom", [96, 8, 1024], BF16)
    Woq = k.sb("Woq", [128, 2, 1024], BF16)
    s.dma("sp", Win, w_in_bf.rearrange("(k p) n -> p k n", p=128), rd=[w_in_bf.name], wr=["Win"])
    s.dma("sp", Wom, w_out_bf[0:768, :].rearrange("(g p) n -> p g n", p=96), rd=[w_out_bf.name], wr=["Wom"])
    s.dma("sp", Woq, w_out_bf[768:1024, :].rearrange("(t p) n -> p t n", p=128), rd=[w_out_bf.name], wr=["Woq"])
    wsX = k.sb("wsX", [128, 8, 128], F32)
    wsT = k.sb("wsT", [128, 8, 128], BF16)
    s.dma("sp", wsX, sg_w.rearrange("g p q -> p g q"), wr=["wsX"])
    for h in range(2):
        b = k.bank()
        s.tr([(ps[:, b, i * 128:(i + 1) * 128], wsX[:, h * 4 + i, :]) for i in range(4)], "ps%d" % b, ["wsX"], k.ident)
        k.evac(wsT[:, h * 4:(h + 1) * 4, :].rearrange("p g q -> p (g q)"), ps[:, b, :], rd=["ps%d" % b], wr=["wsT"])
    bsb = k.sb("bsb", [96, 8, 128], F32)
    s.dma("sp", bsb.rearrange("p g q -> p (g q)"), sg_b.rearrange("g q -> (g q)").partition_broadcast(96), wr=["bsb"])
    lng = k.sb("lng", [128, 768], F32)
    lnb = k.sb("lnb", [128, 768], F32)
    s.dma("sp", lng, sg_ln_g.partition_broadcast(128), wr=["lnc"])
    s.dma("sp", lnb, sg_ln_b.partition_broadcast(128), wr=["lnc"])
    G = [[k.sb("lg%d%d" % (i, j), [128, 1024], F32) for j in range(2)] for i in range(2)]
    for i in range(2):
        s.dma("sp", G[i][0], ln_g[i].partition_broadcast(128), wr=["lnc"])
        s.dma("sp", G[i][1], ln_b[i].partition_broadcast(128), wr=["lnc"])

    XA = [k.sb("XA%d" % i, [128, 2, 1024], F32) for i in range(3)]
    X0T = k.sb("X0T", [128, 8, T], BF16)
    X1T = [k.sb("X1T%d" % i, [128, 8, T], BF16) for i in range(2)]
    uT = k.sb("uT", [96, 8, T], BF16)
    mqT = k.sb("mqT", [128, 2, T], BF16)
    memoT = k.sb("memoT", [128, 2, T], BF16)
    Vf = [k.sb("Vf%d" % i, [128, 768], F32) for i in range(2)]
    vn = [k.sb("vn%d" % i, [128, 768], BF16) for i in range(2)]
    tmp = k.sb("tmpmix", [96, 4, 128], F32)
    probs = k.sb("probs", [128, 4, 256], F32)
    probsT = k.sb("probsT", [128, 4, 2, 128], BF16)
    rmax = k.sb("rmax", [128, 4], F32)
    rsum = k.sb("rsum", [128, 4], F32)
    NW = 3
    WG = [k.sb("WG%d" % i, [128, 8, 256], BF16) for i in range(NW)]
    WU = [k.sb("WU%d" % i, [128, 8, 256], BF16) for i in range(NW)]
    WD = [k.sb("WD%d" % i, [128, 2, 1024], BF16) for i in range(NW)]
    sg = [k.sb("sg%d" % i, [128, T], BF16) for i in range(2)]
    hT = [k.sb("hT%d" % i, [128, 2, T], BF16) for i in range(2)]
    NT = len(jobs) * ntt
    NBLK = 11

    def wload(g):
        if g >= NT * NBLK:
            return
        j = g % NBLK
        wb = g % NW
        s.dma("sp", WG[wb], wg_bf[:, j * 256:(j + 1) * 256].rearrange("(k p) n -> p k n", p=128), rd=[wg_bf.name], wr=["WG%d" % wb])
        s.dma("sp", WU[wb], wu_bf[:, j * 256:(j + 1) * 256].rearrange("(k p) n -> p k n", p=128), rd=[wu_bf.name], wr=["WU%d" % wb])
        s.dma("sp", WD[wb], wd_bf[j * 256:(j + 1) * 256, :].rearrange("(t p) n -> p t n", p=128), rd=[wd_bf.name], wr=["WD%d" % wb])

    def xload(jt):
        if jt >= NT:
            return
        x, X1, pfx = jobs[jt // ntt]
        r0 = (jt % ntt) * T
        s.dma("sp", XA[jt % 3], x[r0:r0 + T, :].rearrange("(s p) d -> p s d", p=128), wr=["XA%d_%d" % (jt % 3, i) for i in range(2)])

    def mixer(jt):
        p = jt % 2
        x, X1, pfx = jobs[jt // ntt]
        tt = jt % ntt
        r0 = tt * T
        xa = XA[jt % 3]
        xrn = ["XA%d_%d" % (jt % 3, i) for i in range(2)]
        for s_ in range(2):
            k.transpose_tok(xa[:, s_, :], xrn[s_], X0T, "X0T", s_ * 128)
        yield
        for s_ in range(2):
            c0 = s_ * 128
            for h2 in range(2):
                b = k.bank()
                s.mm(ps[:, b, 0:384], "ps%d" % b,
                     [(X0T[:, kk, c0:c0 + 128], Win[:, kk, 768 + h2 * 384:768 + (h2 + 1) * 384]) for kk in range(8)],
                     ["Win", "X0T"])
                s.op("act", lambda e, b=b, h2=h2, s_=s_: e.activation(Vf[s_][:, h2 * 384:(h2 + 1) * 384], ps[:, b, 0:384], AF.Gelu),
                     rd=["ps%d" % b], wr=["Vf%d" % s_])
            k.ln_inplace(Vf[s_], "Vf%d" % s_, lng, lnb, out=vn[s_], out_res="vn%d" % s_)
        yield
        for g in range(8):
            b = k.bank()
            s.mm(ps[0:96, b, 0:T], "ps%d" % b, [(Win[:, kk, g * 96:(g + 1) * 96], X0T[:, kk, :]) for kk in range(8)],
                 ["Win", "X0T"])
            s.op("act", lambda e, b=b, g=g: e.activation(uT[:, g, :], ps[0:96, b, 0:T], AF.Gelu), rd=["ps%d" % b], wr=["uT"])
        for t in range(2):
            b = k.bank()
            s.mm(ps[:, b, 0:T], "ps%d" % b,
                 [(Win[:, kk, 1536 + t * 128:1536 + (t + 1) * 128], X0T[:, kk, :]) for kk in range(8)], ["Win", "X0T"])
            k.evac(mqT[:, t, :], ps[:, b, 0:T], rd=["ps%d" % b], wr=["mqT"])
        yield
        for s_ in range(2):
            c0 = s_ * 128
            for gh in range(2):
                b = k.bank()
                for g4 in range(4):
                    g = gh * 4 + g4
                    s.mm(ps[0:96, b, g4 * 128:(g4 + 1) * 128], "ps%d" % b, [(vn[s_][:, g * 96:(g + 1) * 96], wsT[:, g, :])],
                         ["vn%d" % s_, "wsT"])
                s.op("dve", lambda e, b=b, gh=gh: e.tensor_tensor(tmp, ps[0:96, b, :].rearrange("p (g q) -> p g q", g=4),
                                                                 bsb[:, gh * 4:(gh + 1) * 4, :], OP.add),
                     rd=["ps%d" % b, "bsb"], wr=["tmpmix"])
                s.op("dve", lambda e, gh=gh, c0=c0: e.tensor_tensor(uT[:, gh * 4:(gh + 1) * 4, c0:c0 + 128], tmp,
                                                                   uT[:, gh * 4:(gh + 1) * 4, c0:c0 + 128], OP.mult),
                     rd=["tmpmix", "uT"], wr=["uT"])
        yield
        for s_ in range(2):
            mem_attn_a(k, mqT, "mqT", memkT, vmem, memoT, "memoT", s_, (probs, probsT, rmax, rsum))
            yield
            mem_attn_b(k, mqT, "mqT", memkT, vmem, memoT, "memoT", s_, (probs, probsT, rmax, rsum))
            yield
        for s_ in range(2):
            c0 = s_ * 128
            for nh in range(2):
                b = k.bank()
                s.mm(ps[:, b, :], "ps%d" % b,
                     [(uT[:, g, c0:c0 + 128], Wom[:, g, nh * 512:(nh + 1) * 512]) for g in range(8)] +
                     [(memoT[:, t, c0:c0 + 128], Woq[:, t, nh * 512:(nh + 1) * 512]) for t in range(2)],
                     ["uT", "memoT", "Wom", "Woq"])
                s.op("dve", lambda e, b=b, nh=nh, s_=s_: e.scalar_tensor_tensor(
                    xa[:, s_, nh * 512:(nh + 1) * 512], xa[:, s_, nh * 512:(nh + 1) * 512], ALPHA, ps[:, b, :],
                    OP.mult, OP.add), rd=["ps%d" % b, xrn[s_]], wr=[xrn[s_]])
            k.ln_inplace(xa[:, s_, :], xrn[s_], G[0][0], G[0][1])
            yield
        for s_ in range(2):
            k.transpose_tok(xa[:, s_, :], xrn[s_], X1T[p], "X1T%d" % p, s_ * 128)
        yield

    def ffn(jt):
        p = jt % 2
        x, X1, pfx = jobs[jt // ntt]
        tt = jt % ntt
        r0 = tt * T
        xa = XA[jt % 3]
        xt = X1T[p]
        xtn = "X1T%d" % p
        xrn = ["XA%d_%d" % (jt % 3, i) for i in range(2)]

        def down(j):
            g = jt * NBLK + j
            wb = g % NW
            hb = g % 2
            for s_ in range(2):
                for nh in range(2):
                    ab = 4 + s_ * 2 + nh
                    s.mm(ps[:, ab, :], "ps%d" % ab,
                         [(hT[hb][:, t2, s_ * 128:(s_ + 1) * 128], WD[wb][:, t2, nh * 512:(nh + 1) * 512]) for t2 in range(2)],
                         ["hT%d" % hb, "WD%d" % wb], start=(j == 0), stop=(j == NBLK - 1))

        for j in range(NBLK):
            g = jt * NBLK + j
            wb = g % NW
            hb = g % 2
            if g == 0:
                wload(0)
            wload(g + 1)
            for t2 in range(2):
                bg_ = k.bank()
                bu = k.bank()
                s.mm(ps[:, bg_, 0:T], "ps%d" % bg_, [(WG[wb][:, kk, t2 * 128:(t2 + 1) * 128], xt[:, kk, :]) for kk in range(8)],
                     ["WG%d" % wb, xtn])
                s.mm(ps[:, bu, 0:T], "ps%d" % bu, [(WU[wb][:, kk, t2 * 128:(t2 + 1) * 128], xt[:, kk, :]) for kk in range(8)],
                     ["WU%d" % wb, xtn])
                s.op("act", lambda e, bg_=bg_, t2=t2: e.activation(sg[t2], ps[:, bg_, 0:T], AF.Silu), rd=["ps%d" % bg_],
                     wr=["sg%d" % t2])
                s.op("dve", lambda e, bu=bu, t2=t2, hb=hb: e.tensor_tensor(hT[hb][:, t2, :], sg[t2], ps[:, bu, 0:T], OP.mult),
                     rd=["ps%d" % bu, "sg%d" % t2], wr=["hT%d" % hb])
            if j > 0:
                down(j - 1)
            yield
        down(NBLK - 1)
        for s_ in range(2):
            for nh in range(2):
                ab = 4 + s_ * 2 + nh
                s.op("dve", lambda e, ab=ab, nh=nh, s_=s_: e.scalar_tensor_tensor(
                    xa[:, s_, nh * 512:(nh + 1) * 512], xa[:, s_, nh * 512:(nh + 1) * 512], ALPHA, ps[:, ab, :],
                    OP.mult, OP.add), rd=["ps%d" % ab, xrn[s_]], wr=[xrn[s_]])
            k.ln_inplace(xa[:, s_, :], xrn[s_], G[1][0], G[1][1])
            s.dma("sp", X1[r0 + s_ * 128:r0 + (s_ + 1) * 128, :], xa[:, s_, :], rd=[xrn[s_]], wr=[pfx + "_%d" % (tt * 2 + s_)])
        yield

    bg = list(bg) if bg else []
    xload(0)
    xload(1)
    for _ in mixer(0):
        pass
    sched = [1] * 11 + [0]
    for jt in range(NT):
        m = mixer(jt + 1) if jt + 1 < NT else None
        xload(jt + 2)
        for bi, _ in enumerate(ffn(jt)):
            for _r in range(sched[bi] if bi < len(sched) else 1):
                if m is not None:
                    try:
                        next(m)
                    except StopIteration:
                        m = None
        while m is not None:
            try:
                next(m)
            except StopIteration:
                m = None
        if bg:
            bg.pop(0)()
    while bg:
        bg.pop(0)()


def bc(ap, dims):
    pstep = ap.ap[0]
    return bass.AP(ap.tensor, ap.offset, [list(pstep)] + [list(d) for d in dims])


DK = 96
DV = 192
QS = float(DK ** -0.5)
TWO_PI = float(2 * np.pi)


def setup_l1(k, dlf, dlb, posb):
    c = {}
    c["lg"] = k.sb("lg", [128, 8], F32)
    c["fac"] = k.sb("fac", [128, 4, 4], F32)
    c["g128"] = k.sb("g128", [128, 8], F32)
    c["pw"] = k.sb("pw", [128, 8, 32], F32)
    c["maskT"] = k.sb("maskT", [128, 4, 128], F32)
    c["cos"] = k.sb("cos", [128, 32, 48], F32)
    c["sin"] = k.sb("sin", [128, 32, 48], F32)
    c["A"] = k.sb("ropeA", [128, 768], F32)
    c["B"] = k.sb("ropeB", [128, 768], F32)
    with k.scope():
        _setup_l1(k, c, dlf, dlb, posb)
    return c


def _setup_l1(k, c, dlf, dlb, posb):
    s = k.s
    lg, fac, g128, pw, maskT, cos, sin = c["lg"], c["fac"], c["g128"], c["pw"], c["maskT"], c["cos"], c["sin"]
    s.dma("sp", lg[:, 0:4], dlf.partition_broadcast(128), wr=["lg"])
    s.dma("sp", lg[:, 4:8], dlb.partition_broadcast(128), wr=["lg"])
    s.op("act", lambda e: e.activation(lg, lg, AF.Exp, scale=-1.0), rd=["lg"], wr=["lg"])
    s.op("act", lambda e: e.activation(lg, lg, AF.Ln, bias=1.0), rd=["lg"], wr=["lg"])
    s.op("dve", lambda e: e.tensor_scalar(lg, lg, -1.0, None, OP.mult), rd=["lg"], wr=["lg"])
    pidx_i = k.sb("pidx_i", [128, 1], I32)
    pidx = k.sb("pidx", [128, 1], F32)
    s.op("pool", lambda e: e.iota(pidx_i, [[0, 1]], base=0, channel_multiplier=1), wr=["pidx_i"])
    s.op("dve", lambda e: e.tensor_copy(pidx, pidx_i), rd=["pidx_i"], wr=["pidx"])
    q = k.sb("qtmp", [128, 1], F32)
    specs = [(0, 1.0, 0.0, QS, 0), (1, -1.0, 127.0, QS, 4), (2, -1.0, 128.0, 1.0, 0), (3, 1.0, 1.0, 1.0, 4)]
    for idx, a, b, m, lo in specs:
        s.op("dve", lambda e, a=a, b=b: e.tensor_scalar(q, pidx, a, b, OP.mult, OP.add), rd=["pidx"], wr=["qtmp"])
        s.op("dve", lambda e, idx=idx, lo=lo: e.tensor_scalar(fac[:, idx, :], lg[:, lo:lo + 4], q, None, OP.mult),
             rd=["qtmp", "lg"], wr=["fac"])
        s.op("act", lambda e, idx=idx: e.activation(fac[:, idx, :], fac[:, idx, :], AF.Exp), rd=["fac"], wr=["fac"])
        if m != 1.0:
            s.op("dve", lambda e, idx=idx, m=m: e.tensor_scalar(fac[:, idx, :], fac[:, idx, :], m, None, OP.mult),
                 rd=["fac"], wr=["fac"])
    s.op("act", lambda e: e.activation(g128, lg, AF.Exp, scale=128.0), rd=["lg"], wr=["g128"])
    nidx_i = k.sb("nidx_i", [128, 32], I32)
    nidx = k.sb("nidx", [128, 32], F32)
    s.op("pool", lambda e: e.iota(nidx_i, [[-128, 32]], base=128 * 31, channel_multiplier=0), wr=["nidx_i"])
    s.op("dve", lambda e: e.tensor_copy(nidx, nidx_i), rd=["nidx_i"], wr=["nidx"])
    for j in range(8):
        s.op("dve", lambda e, j=j: e.tensor_scalar(pw[:, j, :], nidx, lg[:, j:j + 1], None, OP.mult), rd=["nidx", "lg"], wr=["pw"])
    s.op("act", lambda e: e.activation(pw, pw, AF.Exp), rd=["pw"], wr=["pw"])
    D_i = k.sb("D_i", [128, 128], I32)
    Dp = k.sb("Dp", [128, 128], F32)
    Dn = k.sb("Dn", [128, 128], F32)
    E1 = k.sb("E1", [128, 128], F32)
    s.op("pool", lambda e: e.iota(D_i, [[1, 128]], base=0, channel_multiplier=-1), wr=["D_i"])
    s.op("dve", lambda e: e.tensor_copy(Dn, D_i), rd=["D_i"], wr=["Dn"])
    s.op("dve", lambda e: e.tensor_scalar(Dp, Dn, 0.0, None, OP.max), rd=["Dn"], wr=["Dp"])
    s.op("dve", lambda e: e.tensor_tensor(Dn, Dp, Dn, OP.subtract), rd=["Dp", "Dn"], wr=["Dn"])
    for h in range(4):
        s.op("dve", lambda e, h=h: e.tensor_scalar(E1, Dp, lg[:, h:h + 1], None, OP.mult), rd=["Dp", "lg"], wr=["E1"])
        s.op("dve", lambda e, h=h: e.scalar_tensor_tensor(E1, Dn, lg[:, 4 + h:5 + h], E1, OP.mult, OP.add),
             rd=["Dn", "lg", "E1"], wr=["E1"])
        s.op("act", lambda e, h=h: e.activation(maskT[:, h, :], E1, AF.Exp), rd=["E1"], wr=["maskT"])
    s.op("dve", lambda e: e.tensor_scalar(maskT, maskT, QS, None, OP.mult), rd=["maskT"], wr=["maskT"])


def setup_rope(k, c, posb):
    with k.scope():
        _setup_rope(k, c, posb)


def _setup_rope(k, c, posb):
    s = k.s
    cos, sin = c["cos"], c["sin"]
    k.uid += 1
    u = "_%d" % k.uid
    pb = k.sb("pb" + u, [128, 1], F32)
    s.dma("sp", pb, posb.partition_broadcast(128), wr=["pb"])
    ii = k.sb("ii" + u, [128, 48], I32)
    inv = k.sb("inv" + u, [128, 48], F32)
    s.op("pool", lambda e: e.iota(ii, [[1, 48]], base=0, channel_multiplier=0), wr=["ii"])
    s.op("dve", lambda e: e.tensor_copy(inv, ii), rd=["ii"], wr=["inv"])
    s.op("act", lambda e: e.activation(inv, inv, AF.Exp, scale=float(-2.0 * np.log(10000.0) / 96.0)), rd=["inv"], wr=["inv"])
    pos_i = k.sb("pos_i" + u, [128, 32], I32)
    pos = k.sb("pos" + u, [128, 32], F32)
    s.op("pool", lambda e: e.iota(pos_i, [[128, 32]], base=0, channel_multiplier=1), wr=["pos_i"])
    s.op("dve", lambda e: e.tensor_copy(pos, pos_i), rd=["pos_i"], wr=["pos"])
    s.op("dve", lambda e: e.tensor_scalar(pos, pos, pb, None, OP.add), rd=["pos", "pb"], wr=["pos"])
    ang = k.sb("ang" + u, [128, 32, 48], F32)
    ri = k.sb("ri" + u, [128, 32, 48], I32)
    rf = k.sb("rf" + u, [128, 32, 48], F32)
    for n in range(32):
        s.op("dve", lambda e, n=n: e.tensor_scalar(ang[:, n, :], inv, pos[:, n:n + 1], None, OP.mult), rd=["inv", "pos"], wr=["ang"])
    for (dst, name, off) in ((sin, "sin", 0.0), (cos, "cos", 0.25)):
        s.op("dve", lambda e, dst=dst, off=off: e.tensor_scalar(dst, ang, float(1.0 / TWO_PI), off, OP.mult, OP.add), rd=["ang"], wr=[name])
        s.op("dve", lambda e, dst=dst: e.tensor_copy(ri, dst), rd=[name], wr=["ri"])
        s.op("dve", lambda e: e.tensor_copy(rf, ri), rd=["ri"], wr=["rf"])
        s.op("dve", lambda e, dst=dst: e.tensor_tensor(dst, dst, rf, OP.subtract), rd=[name, "rf"], wr=[name])
        s.op("dve", lambda e, dst=dst: e.tensor_scalar(rf, dst, 0.5, None, OP.is_gt), rd=[name], wr=["rf"])
        s.op("dve", lambda e, dst=dst: e.tensor_tensor(dst, dst, rf, OP.subtract), rd=[name, "rf"], wr=[name])
        s.op("dve", lambda e, dst=dst: e.tensor_scalar(rf, dst, -0.5, None, OP.is_lt), rd=[name], wr=["rf"])
        s.op("dve", lambda e, dst=dst: e.tensor_tensor(dst, dst, rf, OP.add), rd=[name, "rf"], wr=[name])
        s.op("act", lambda e, dst=dst: e.activation(dst, dst, AF.Sin, scale=TWO_PI), rd=[name], wr=[name])


def rope(k, c, n, src_ps, nh, A, B, dst, rd, wr):
    s = k.s
    cosb = bc(c["cos"][:, n, :], [[0, nh], [0, 2], [1, 48]])
    sinb = bc(c["sin"][:, n, :], [[0, nh], [0, 2], [1, 48]])
    v4 = lambda ap: ap.rearrange("p (h t d) -> p h t d", h=nh, t=2)
    s.op("dve", lambda e: e.tensor_tensor(v4(A), v4(src_ps), cosb, OP.mult), rd=rd + ["cos"], wr=["ropeA"])
    s.op("dve", lambda e: e.tensor_tensor(v4(B), v4(src_ps), sinb, OP.mult), rd=rd + ["sin"], wr=["ropeB"])
    s.op("dve", lambda e: e.tensor_tensor(v4(dst)[:, :, 0, :], v4(A)[:, :, 0, :], v4(B)[:, :, 1, :], OP.subtract),
         rd=["ropeA", "ropeB"], wr=[wr])
    s.op("dve", lambda e: e.tensor_tensor(v4(dst)[:, :, 1, :], v4(A)[:, :, 1, :], v4(B)[:, :, 0, :], OP.add),
         rd=["ropeA", "ropeB"], wr=[wr])


def load_l1_weights(k, w_in_b_bf, w_out1_bf):
    s = k.s
    Wb = k.sb("Wb", [128, 8, 2560], BF16)
    for kk in range(8):
        s.dma("sp", Wb[:, kk, :], w_in_b_bf[kk * 128:(kk + 1) * 128, :], rd=[w_in_b_bf.name], wr=["Wb"])
    return Wb


def phase_scan(k, c, X1, Wb, Floc_out, Sbend_out, SbAll, pfx="X1", u="", need_f=True):
    s, ps = k.s, k.ps
    k.banks = [0, 1, 2, 3, 4, 5, 6, 7]
    X = k.sb("Xs" + u, [128, 1024], F32)
    xT = k.sb("xTs" + u, [128, 8, 128], BF16)
    A, B = c["A"], c["B"]
    kr = k.sb("kr" + u, [128, 384], F32)
    kb = k.sb("kb" + u, [128, 4, 96], BF16)
    kf = k.sb("kf" + u, [128, 4, 96], BF16)
    vb = k.sb("vb" + u, [128, 768], BF16)
    Sb = k.sb("Sb" + u, [96, 4, 192], F32)
    Fl = k.sb("Fl" + u, [96, 4, 192], F32)
    s.op("pool", lambda e: e.memset(Sb, 0.0), wr=["Sb"])
    s.op("pool", lambda e: e.memset(Fl, 0.0), wr=["Fl"])
    fac, g128, pw = c["fac"], c["g128"], c["pw"]
    for n in range(31, -1, -1):
        s.dma("sp", X, X1[n * 128:(n + 1) * 128, :], rd=[pfx + "_%d" % n], wr=["Xs"])
        k.transpose_tok(X, "Xs", xT, "xTs", 0)
        bk = k.bank()
        s.mm(ps[:, bk, 0:384], "ps%d" % bk, [(xT[:, kk, :], Wb[:, kk, 384:768]) for kk in range(8)], ["xTs", "Wb"])
        rope(k, c, n, ps[:, bk, 0:384], 4, A[:, 0:384], B[:, 0:384], kr, ["ps%d" % bk], "kr")
        for h2 in range(2):
            b = k.bank()
            s.mm(ps[:, b, 0:384], "ps%d" % b, [(xT[:, kk, :], Wb[:, kk, 768 + h2 * 384:768 + (h2 + 1) * 384]) for kk in range(8)],
                 ["xTs", "Wb"])
            s.op("act", lambda e, b=b, h2=h2: e.copy(vb[:, h2 * 384:(h2 + 1) * 384], ps[:, b, 0:384]), rd=["ps%d" % b], wr=["vb"])
        kr3 = kr.rearrange("p (h d) -> p h d", h=4)
        s.op("dve", lambda e: e.tensor_tensor(kb, kr3, bc(fac[:, 3, :], [[1, 4], [0, 96]]), OP.mult), rd=["kr", "fac"], wr=["kb"])
        if need_f:
            s.op("dve", lambda e: e.tensor_tensor(kf, kr3, bc(fac[:, 2, :], [[1, 4], [0, 96]]), OP.mult), rd=["kr", "fac"], wr=["kf"])
        s.op("act", lambda e, n=n: e.copy(SbAll[:, n, :], Sb.rearrange("p h d -> p (h d)")), rd=["Sb"], wr=["SbAll"])
        for (kx, kres, dirn) in (((kb, "kb", 1), (kf, "kf", 0)) if need_f else ((kb, "kb", 1),)):
            for hp in range(2):
                b = k.bank()
                for h2 in range(2):
                    h = hp * 2 + h2
                    s.mm(ps[0:96, b, h2 * 192:(h2 + 1) * 192], "ps%d" % b, [(kx[:, h, :], vb[:, h * 192:(h + 1) * 192])], [kres, "vb"])
                for h2 in range(2):
                    h = hp * 2 + h2
                    if dirn == 1:
                        s.op("dve", lambda e, b=b, h=h, h2=h2: e.scalar_tensor_tensor(
                            Sb[:, h, :], Sb[:, h, :], g128[0:96, 4 + h:5 + h], ps[0:96, b, h2 * 192:(h2 + 1) * 192], OP.mult, OP.add),
                            rd=["ps%d" % b, "Sb", "g128"], wr=["Sb"])
                    else:
                        s.op("dve", lambda e, b=b, h=h, h2=h2, n=n: e.scalar_tensor_tensor(
                            Fl[:, h, :], ps[0:96, b, h2 * 192:(h2 + 1) * 192], pw[0:96, h, n:n + 1], Fl[:, h, :], OP.mult, OP.add),
                            rd=["ps%d" % b, "Fl", "pw"], wr=["Fl"])
    if need_f:
        s.dma("sp", Floc_out, Fl.rearrange("p h d -> p (h d)"), rd=["Fl"], wr=["Floc_out" + u])
    s.dma("sp", Sbend_out, Sb.rearrange("p h d -> p (h d)"), rd=["Sb"], wr=["Sbend_out" + u])


def phase_l1(k, c, X1, Wb, memkT, vmem, w_out1_bf, Fp, Bp, SbAll, gn_g, gn_b, ln_g, ln_b, w_router, b_router, X1N, X1NT, GT, nch=32, masks=None, st_rd=()):
    s, ps = k.s, k.ps
    k.banks = [0, 1, 2, 3, 4, 5, 6, 7]
    fac, g128, pw, maskT = c["fac"], c["g128"], c["pw"], c["maskT"]
    Wo = k.sb("Wo1", [128, 8, 1024], BF16)
    s.dma("sp", Wo, w_out1_bf.rearrange("(k p) n -> p k n", p=128), rd=[w_out1_bf.name], wr=["Wo1"])
    gng = k.sb("gng", [128, 768], F32)
    gnb = k.sb("gnb", [128, 768], F32)
    s.dma("sp", gng, gn_g.partition_broadcast(128), wr=["lnc"])
    s.dma("sp", gnb, gn_b.partition_broadcast(128), wr=["lnc"])
    Lg = k.sb("L1g", [128, 1024], F32)
    Lb = k.sb("L1b", [128, 1024], F32)
    s.dma("sp", Lg, ln_g.partition_broadcast(128), wr=["lnc"])
    s.dma("sp", Lb, ln_b.partition_broadcast(128), wr=["lnc"])
    Wr = k.sb("Wr", [128, 8, 8], F32)
    s.dma("sp", Wr, w_router.rearrange("(k p) e -> p k e", p=128), wr=["Wr"])
    brt = k.sb("brt", [128, 8], F32)
    s.dma("sp", brt, b_router.partition_broadcast(128), wr=["brt"])
    Sf = k.sb("Sf", [96, 4, 192], F32)
    BkP = k.sb("BkP", [96, 4, 192], F32)
    s.dma("sp", Sf.rearrange("p h d -> p (h d)"), Fp, rd=list(st_rd), wr=["Sf"])
    s.dma("sp", BkP.rearrange("p h d -> p (h d)"), Bp, rd=list(st_rd), wr=["BkP"])
    if masks is not None:
        mk = k.sb("mk", [96, 2], F32)
        s.dma("sp", mk, masks.partition_broadcast(96), wr=["mk"])
        s.op("dve", lambda e: e.tensor_scalar(Sf.rearrange("p h d -> p (h d)"), Sf.rearrange("p h d -> p (h d)"), mk[:, 0:1], None, OP.mult),
             rd=["Sf", "mk"], wr=["Sf"])
        s.op("dve", lambda e: e.tensor_scalar(BkP.rearrange("p h d -> p (h d)"), BkP.rearrange("p h d -> p (h d)"), mk[:, 1:2], None, OP.mult),
             rd=["BkP", "mk"], wr=["BkP"])
    XX = [k.sb("Xf%d" % i, [128, 1024], F32) for i in range(2)]
    xT = k.sb("xTf", [128, 8, 128], BF16)
    xT32 = k.sb("xT32", [128, 8, 128], F32)
    A, B = c["A"], c["B"]
    qk = k.sb("qk", [128, 768], F32)
    KF = [k.sb("kf1_%d" % i, [128, 4, 96], BF16) for i in range(2)]
    VB = [k.sb("vb1_%d" % i, [128, 768], BF16) for i in range(2)]
    SG = [k.sb("sgl%d" % i, [128, 768], F32) for i in range(2)]
    QT = [k.sb("qkT%d" % i, [96, 8, 128], BF16) for i in range(2)]
    STT = [k.sb("ST%d" % i, [128, 4, 128], BF16) for i in range(2)]
    Sfb = k.sb("Sfb", [96, 4, 192], BF16)
    SbT = k.sb("SbT", [96, 4, 192], BF16)
    o = k.sb("o", [128, 4, 192], F32)
    hst = k.sb("hst", [128, 4, 6], F32)
    hmv = k.sb("hmv", [128, 4, 2], F32)
    hrs = k.sb("hrs", [128, 4], F32)
    mixT = k.sb("mixT1", [128, 6, 128], BF16)
    MQ = [k.sb("mqT1_%d" % i, [128, 2, 128], BF16) for i in range(2)]
    xTo = k.sb("xTo", [128, 8, 128], BF16)
    memoT = k.sb("memoT1", [128, 2, 128], BF16)
    probs = k.sb("probs1", [128, 4, 256], F32)
    probsT = k.sb("probsT1", [128, 4, 2, 128], BF16)
    rmax = k.sb("rmax1", [128, 4], F32)
    rsum = k.sb("rsum1", [128, 4], F32)
    lgt = k.sb("lgt", [128, 8], F32)
    l2 = k.sb("l2", [128, 8], F32)
    eq1 = k.sb("eq1", [128, 8], F32)
    eq2 = k.sb("eq2", [128, 8], F32)
    m12 = k.sb("m12", [128, 4], F32)
    def stA(n):
        p = n % 2
        X, vb, sgl, qkT, ST, kf, mqT = XX[p], VB[p], SG[p], QT[p], STT[p], KF[p], MQ[p]
        rX, rvb, rsgl, rqkT, rST, rkf, rmq = "Xf%d" % p, "vb1_%d" % p, "sgl%d" % p, "qkT%d" % p, "ST%d" % p, "kf1_%d" % p, "mqT1_%d" % p
        s.dma("sp", X, X1[n * 128:(n + 1) * 128, :], rd=["X1_%d" % n], wr=[rX])
        yield
        k.transpose_tok(X, rX, xT, "xTf", 0)
        yield
        for h2 in range(2):
            b = k.bank()
            s.mm(ps[:, b, 0:384], "ps%d" % b, [(xT[:, kk, :], Wb[:, kk, h2 * 384:(h2 + 1) * 384]) for kk in range(8)], ["xTf", "Wb"])
            rope(k, c, n, ps[:, b, 0:384], 4, A[:, 0:384], B[:, 0:384], qk[:, h2 * 384:(h2 + 1) * 384], ["ps%d" % b], "qk")
        yield
        for h2 in range(2):
            b = k.bank()
            s.mm(ps[:, b, 0:384], "ps%d" % b, [(xT[:, kk, :], Wb[:, kk, 768 + h2 * 384:768 + (h2 + 1) * 384]) for kk in range(8)],
                 ["xTf", "Wb"])
            s.op("act", lambda e, b=b, h2=h2: e.copy(vb[:, h2 * 384:(h2 + 1) * 384], ps[:, b, 0:384]), rd=["ps%d" % b], wr=[rvb])
        for h2 in range(2):
            b = k.bank()
            s.mm(ps[:, b, 0:384], "ps%d" % b, [(xT[:, kk, :], Wb[:, kk, 1536 + h2 * 384:1536 + (h2 + 1) * 384]) for kk in range(8)],
                 ["xTf", "Wb"])
            s.op("act", lambda e, b=b, h2=h2: e.activation(sgl[:, h2 * 384:(h2 + 1) * 384], ps[:, b, 0:384], AF.Silu),
                 rd=["ps%d" % b], wr=[rsgl])
        for t in range(2):
            b = k.bank()
            s.mm(ps[:, b, 0:128], "ps%d" % b, [(Wb[:, kk, 2304 + t * 128:2304 + (t + 1) * 128], xT[:, kk, :]) for kk in range(8)],
                 ["xTf", "Wb"])
            k.evac(mqT[:, t, :], ps[:, b, 0:128], rd=["ps%d" % b], wr=[rmq])
        for hh in range(2):
            b = k.bank()
            s.tr([(ps[0:96, b, i * 128:(i + 1) * 128], qk[:, (hh * 4 + i) * 96:(hh * 4 + i + 1) * 96]) for i in range(4)],
                 "ps%d" % b, ["qk"], k.ident)
            k.evac(qkT[:, hh * 4:(hh + 1) * 4, :].rearrange("p h t -> p (h t)"), ps[0:96, b, :], rd=["ps%d" % b], wr=[rqkT])
        yield
        k3 = qk[:, 384:768].rearrange("p (h d) -> p h d", h=4)
        s.op("dve", lambda e: e.tensor_tensor(kf, k3, bc(fac[:, 2, :], [[1, 4], [0, 96]]), OP.mult), rd=["qk", "fac"], wr=[rkf])
        b = k.bank()
        for h in range(4):
            s.mm(ps[:, b, h * 128:(h + 1) * 128], "ps%d" % b, [(qkT[:, 4 + h, :], qkT[:, h, :])], [rqkT])
        s.op("dve", lambda e, b=b: e.tensor_tensor(ST.rearrange("p h i -> p (h i)"), ps[:, b, :], maskT.rearrange("p h i -> p (h i)"), OP.mult),
             rd=["ps%d" % b, "maskT"], wr=[rST])
        s.op("act", lambda e: e.copy(Sfb.rearrange("p h d -> p (h d)"), Sf.rearrange("p h d -> p (h d)")), rd=["Sf"], wr=["Sfb"])
        for h in range(4):
            s.op("dve", lambda e, h=h, n=n: e.scalar_tensor_tensor(SbT[:, h, :], BkP[:, h, :], pw[0:96, 4 + h, n:n + 1],
                                                                  SbAll[:, n, h * 192:(h + 1) * 192], OP.mult, OP.add),
                 rd=["BkP", "pw", "SbAll"], wr=["SbT"])

    def stB(n):
        p = n % 2
        X, vb, sgl, qkT, ST, kf, mqT = XX[p], VB[p], SG[p], QT[p], STT[p], KF[p], MQ[p]
        rX, rvb, rsgl, rqkT, rST, rkf, rmq = "Xf%d" % p, "vb1_%d" % p, "sgl%d" % p, "qkT%d" % p, "ST%d" % p, "kf1_%d" % p, "mqT1_%d" % p
        for hp in range(2):
            bi_, bf_, bb_ = k.bank(), k.bank(), k.bank()
            for h2 in range(2):
                h = hp * 2 + h2
                cs = slice(h2 * 192, (h2 + 1) * 192)
                s.mm(ps[:, bi_, cs], "ps%d" % bi_, [(ST[:, h, :], vb[:, h * 192:(h + 1) * 192])], [rST, rvb])
                s.mm(ps[:, bf_, cs], "ps%d" % bf_, [(qkT[:, h, :], Sfb[:, h, :])], [rqkT, "Sfb"])
                s.mm(ps[:, bb_, cs], "ps%d" % bb_, [(qkT[:, h, :], SbT[:, h, :])], [rqkT, "SbT"])
            s.op("act", lambda e, hp=hp, bi_=bi_: e.copy(o[:, hp * 2:(hp + 1) * 2, :].rearrange("p h d -> p (h d)"), ps[:, bi_, 0:384]),
                 rd=["ps%d" % bi_], wr=["o"])
            for h2 in range(2):
                h = hp * 2 + h2
                cs = slice(h2 * 192, (h2 + 1) * 192)
                s.op("dve", lambda e, h=h, cs=cs, bf_=bf_: e.scalar_tensor_tensor(o[:, h, :], ps[:, bf_, cs], fac[:, 0, h:h + 1], o[:, h, :],
                                                                              OP.mult, OP.add), rd=["ps%d" % bf_, "o", "fac"], wr=["o"])
                s.op("dve", lambda e, h=h, cs=cs, bb_=bb_: e.scalar_tensor_tensor(o[:, h, :], ps[:, bb_, cs], fac[:, 1, h:h + 1], o[:, h, :],
                                                                              OP.mult, OP.add), rd=["ps%d" % bb_, "o", "fac"], wr=["o"])
        yield
        for hp in range(2):
            b = k.bank()
            for h2 in range(2):
                h = hp * 2 + h2
                s.mm(ps[0:96, b, h2 * 192:(h2 + 1) * 192], "ps%d" % b, [(kf[:, h, :], vb[:, h * 192:(h + 1) * 192])], [rkf, rvb])
            for h2 in range(2):
                h = hp * 2 + h2
                s.op("dve", lambda e, b=b, h=h, h2=h2: e.scalar_tensor_tensor(
                    Sf[:, h, :], Sf[:, h, :], g128[0:96, h:h + 1], ps[0:96, b, h2 * 192:(h2 + 1) * 192], OP.mult, OP.add),
                    rd=["ps%d" % b, "Sf", "g128", "Sfb"], wr=["Sf"])
        for h in range(4):
            s.op("dve", lambda e, h=h: e.bn_stats(hst[:, h, :], o[:, h, :]), rd=["o"], wr=["hst"])
        for h in range(4):
            s.op("dve", lambda e, h=h: e.bn_aggr(hmv[:, h, :], hst[:, h, :]), rd=["hst"], wr=["hmv"])
        s.op("act", lambda e: e.activation(hrs, hmv[:, :, 1], AF.Ln, bias=EPS), rd=["hmv"], wr=["hrs"])
        s.op("act", lambda e: e.activation(hrs, hrs, AF.Exp, scale=-0.5), rd=["hrs"], wr=["hrs"])
        for h in range(4):
            s.op("dve", lambda e, h=h: e.tensor_scalar(o[:, h, :], o[:, h, :], hmv[:, h, 0:1], hrs[:, h:h + 1], OP.subtract, OP.mult),
                 rd=["o", "hmv", "hrs"], wr=["o"])
        of = o.rearrange("p h d -> p (h d)")
        s.op("dve", lambda e: e.tensor_tensor(of, of, gng, OP.mult), rd=["o", "lnc"], wr=["o"])
        s.op("dve", lambda e: e.tensor_tensor(of, of, gnb, OP.add), rd=["o", "lnc"], wr=["o"])
        s.op("dve", lambda e: e.tensor_tensor(of, of, sgl, OP.mult), rd=["o", rsgl], wr=["o"])
        yield
        k.transpose_tok(of, "o", mixT, "mixT1", 0, nk=6)
        mem_attn_a(k, mqT, rmq, memkT, vmem, memoT, "memoT1", 0, (probs, probsT, rmax, rsum))
        yield
        mem_attn_b(k, mqT, rmq, memkT, vmem, memoT, "memoT1", 0, (probs, probsT, rmax, rsum))
        yield
        for nh in range(2):
            b = k.bank()
            s.mm(ps[:, b, :], "ps%d" % b,
                 [(mixT[:, g, :], Wo[:, g, nh * 512:(nh + 1) * 512]) for g in range(6)] +
                 [(memoT[:, t, :], Wo[:, 6 + t, nh * 512:(nh + 1) * 512]) for t in range(2)],
                 ["mixT1", "memoT1", "Wo1"])
            s.op("dve", lambda e, b=b, nh=nh: e.scalar_tensor_tensor(
                X[:, nh * 512:(nh + 1) * 512], X[:, nh * 512:(nh + 1) * 512], ALPHA, ps[:, b, :], OP.mult, OP.add),
                rd=["ps%d" % b, rX], wr=[rX])
        k.ln_inplace(X, rX, Lg, Lb)
        s.dma("sp", X1N[n * 128:(n + 1) * 128, :], X, rd=[rX], wr=["X1N_%d" % n])
        yield
        for hh in range(2):
            b = k.bank()
            s.tr([(ps[:, b, i * 128:(i + 1) * 128], X[:, (hh * 4 + i) * 128:(hh * 4 + i + 1) * 128]) for i in range(4)],
                 "ps%d" % b, [rX], k.ident)
            s.op("dve", lambda e, b=b, hh=hh: e.tensor_copy(xT32[:, hh * 4:(hh + 1) * 4, :].rearrange("p k t -> p (k t)"), ps[:, b, :]),
                 rd=["ps%d" % b], wr=["xT32"])
            s.op("act", lambda e, hh=hh: e.copy(xTo[:, hh * 4:(hh + 1) * 4, :].rearrange("p k t -> p (k t)"),
                                               xT32[:, hh * 4:(hh + 1) * 4, :].rearrange("p k t -> p (k t)")),
                 rd=["xT32"], wr=["xTo"])
        s.dma("sp", X1NT[n], xTo.rearrange("p k t -> p (k t)"), rd=["xTo"], wr=["X1NT_%d" % n])
        yield
        b = k.bank()
        s.mm(ps[:, b, 0:8], "ps%d" % b, [(xT32[:, kk, :], Wr[:, kk, :]) for kk in range(8)], ["xT32", "Wr"])
        s.op("dve", lambda e, b=b: e.tensor_tensor(lgt, ps[:, b, 0:8], brt, OP.add), rd=["ps%d" % b, "brt"], wr=["lgt"])
        s.op("dve", lambda e: e.tensor_reduce(m12[:, 0:1], lgt, AX.X, OP.max), rd=["lgt"], wr=["m12"])
        s.op("dve", lambda e: e.tensor_scalar(eq1, lgt, m12[:, 0:1], None, OP.is_equal), rd=["lgt", "m12"], wr=["eq1"])
        s.op("dve", lambda e: e.scalar_tensor_tensor(l2, eq1, -1e30, lgt, OP.mult, OP.add), rd=["eq1", "lgt"], wr=["l2"])
        s.op("dve", lambda e: e.tensor_reduce(m12[:, 1:2], l2, AX.X, OP.max), rd=["l2"], wr=["m12"])
        s.op("dve", lambda e: e.tensor_scalar(eq2, l2, m12[:, 1:2], None, OP.is_equal), rd=["l2", "m12"], wr=["eq2"])
        s.op("dve", lambda e: e.tensor_tensor(m12[:, 2:3], m12[:, 1:2], m12[:, 0:1], OP.subtract), rd=["m12"], wr=["m12"])
        s.op("act", lambda e: e.activation(m12[:, 2:3], m12[:, 2:3], AF.Exp), rd=["m12"], wr=["m12"])
        s.op("dve", lambda e: e.tensor_scalar(m12[:, 3:4], m12[:, 2:3], 1.0, None, OP.add), rd=["m12"], wr=["m12"])
        s.op("dve", lambda e: e.reciprocal(m12[:, 3:4], m12[:, 3:4]), rd=["m12"], wr=["m12"])
        s.op("dve", lambda e: e.tensor_tensor(m12[:, 2:3], m12[:, 2:3], m12[:, 3:4], OP.mult), rd=["m12"], wr=["m12"])
        s.op("dve", lambda e: e.tensor_scalar(eq1, eq1, m12[:, 3:4], None, OP.mult), rd=["eq1", "m12"], wr=["eq1"])
        s.op("dve", lambda e, n=n: e.scalar_tensor_tensor(GT[:, n, :], eq2, m12[:, 2:3], eq1, OP.mult, OP.add),
             rd=["eq1", "eq2", "m12"], wr=["GT"])


    for _ in stA(0):
        pass
    for n in range(nch):
        gens = {"A": stA(n + 1) if n + 1 < nch else None, "B": stB(n)}
        for ch in "ABABABABBABB":
            g = gens[ch]
            if g is not None:
                try:
                    next(g)
                except StopIteration:
                    gens[ch] = None
        for ch in "BA":
            g = gens[ch]
            while g is not None:
                try:
                    next(g)
                except StopIteration:
                    g = None


def phase_moe(k, X1N, X1NT, GT, weg_bf, weu_bf, wed_bf, ln_g, ln_b, out, nexp=8, ecast=None):
    s, ps = k.s, k.ps
    k.banks = [0, 1, 2, 3, 4, 5, 6, 7]
    XNT = k.sb("XNT", [128, 16, 8, 128], BF16)
    facc = k.sb("facc", [128, 16, 1024], F32)
    WG = [k.sb("EG%d" % i, [128, 8, 512], BF16) for i in range(2)]
    WU = [k.sb("EU%d" % i, [128, 8, 512], BF16) for i in range(2)]
    WD = [k.sb("ED%d" % i, [128, 4, 1024], BF16) for i in range(2)]
    sgt = [k.sb("esg%d" % i, [128, 512], BF16) for i in range(2)]
    hT = [k.sb("ehT%d" % i, [128, 4, 512], BF16) for i in range(2)]
    Lg = k.sb("L2g", [128, 1024], F32)
    Lb = k.sb("L2b", [128, 1024], F32)
    s.dma("sp", Lg, ln_g.partition_broadcast(128), wr=["lnc"])
    s.dma("sp", Lb, ln_b.partition_broadcast(128), wr=["lnc"])
    Xr = [k.sb("Xr%d" % i, [128, 1024], F32) for i in range(2)]
    xrc = [0]

    def epilogue(hh, sub):
        gsub = hh * 16 + sub
        xr = Xr[xrc[0] % 2]
        rn = "Xr%d" % (xrc[0] % 2)
        xrc[0] += 1
        s.dma("sp", xr, X1N[gsub * 128:(gsub + 1) * 128, :], rd=["X1N_%d" % gsub], wr=[rn])
        s.op("dve", lambda e: e.scalar_tensor_tensor(xr, xr, ALPHA, facc[:, sub, :], OP.mult, OP.add),
             rd=[rn, "facc%d" % sub], wr=[rn])
        k.ln_inplace(xr, rn, Lg, Lb)
        s.dma("sp", out[gsub * 128:(gsub + 1) * 128, :], xr, rd=[rn], wr=["out_%d" % gsub])

    def down(hh, ex, blk, tt, jb, hb):
        first = (ex == 0 and blk == 0)
        if first and hh == 1:
            for s4 in range(4):
                epilogue(0, tt * 4 + s4)
        for s4 in range(4):
            sub = tt * 4 + s4
            gsub = hh * 16 + sub
            for nh in range(2):
                b = k.bank()
                s.mm(ps[:, b, :], "ps%d" % b,
                     [(hT[hb][:, t4, s4 * 128:(s4 + 1) * 128], WD[jb][:, t4, nh * 512:(nh + 1) * 512]) for t4 in range(4)],
                     ["ehT%d" % hb, "ED%d" % jb])
                if first:
                    s.op("dve", lambda e, b=b, sub=sub, gsub=gsub, nh=nh, ex=ex: e.tensor_scalar(
                        facc[:, sub, nh * 512:(nh + 1) * 512], ps[:, b, :], GT[:, gsub, ex:ex + 1], None, OP.mult),
                        rd=["ps%d" % b, "GT", "facc%d" % sub], wr=["facc%d" % sub])
                else:
                    s.op("dve", lambda e, b=b, sub=sub, gsub=gsub, nh=nh, ex=ex: e.scalar_tensor_tensor(
                        facc[:, sub, nh * 512:(nh + 1) * 512], ps[:, b, :], GT[:, gsub, ex:ex + 1],
                        facc[:, sub, nh * 512:(nh + 1) * 512], OP.mult, OP.add),
                        rd=["ps%d" % b, "GT", "facc%d" % sub], wr=["facc%d" % sub])
        if hh == 1 and ex == nexp - 1 and blk == 6:
            for s4 in range(4):
                epilogue(1, tt * 4 + s4)

    wc = 0
    hc = 0
    pend = None
    for hh in range(2):
        for cc in range(16):
            s.dma("sp", XNT[:, cc, :, :].rearrange("p k t -> p (k t)"), X1NT[hh * 16 + cc],
                  rd=["X1NT_%d" % (hh * 16 + cc)], wr=["XNT%d" % (cc // 4)])
        for ex in range(nexp):
            nxt = list(ecast[ex + 1]) if (ecast is not None and hh == 0 and ex + 1 < nexp) else []
            for blk in range(7):
                jb = wc % 2
                wc += 1
                s.dma("sp", WG[jb], weg_bf[ex * 1024:(ex + 1) * 1024, blk * 512:(blk + 1) * 512].rearrange("(k p) n -> p k n", p=128),
                      rd=[weg_bf.name + "_%d" % (ex * 2048 + i * 512) for i in range(4)], wr=["EG%d" % jb])
                s.dma("sp", WU[jb], weu_bf[ex * 1024:(ex + 1) * 1024, blk * 512:(blk + 1) * 512].rearrange("(k p) n -> p k n", p=128),
                      rd=[weu_bf.name + "_%d" % (ex * 2048 + i * 512) for i in range(4)], wr=["EU%d" % jb])
                s.dma("sp", WD[jb], wed_bf[ex * 3584 + blk * 512:ex * 3584 + (blk + 1) * 512, :].rearrange("(t p) n -> p t n", p=128),
                      rd=[wed_bf.name + "_%d" % (ex * 3584 + blk * 512)], wr=["ED%d" % jb])
                for tt in range(4):
                    hb = hc % 2
                    hc += 1
                    for t4 in range(4):
                        bg, bu = k.bank(), k.bank()
                        xs = lambda kk: XNT[:, tt * 4:(tt + 1) * 4, kk, :]
                        s.mm(ps[:, bg, :].rearrange("p (c t) -> p c t", c=4), "ps%d" % bg,
                             [(WG[jb][:, kk, t4 * 128:(t4 + 1) * 128], xs(kk)) for kk in range(8)], ["EG%d" % jb, "XNT%d" % tt])
                        s.mm(ps[:, bu, :].rearrange("p (c t) -> p c t", c=4), "ps%d" % bu,
                             [(WU[jb][:, kk, t4 * 128:(t4 + 1) * 128], xs(kk)) for kk in range(8)], ["EU%d" % jb, "XNT%d" % tt])
                        s.op("act", lambda e, bg=bg, t4=t4: e.activation(sgt[t4 % 2], ps[:, bg, :], AF.Silu), rd=["ps%d" % bg], wr=["esg%d" % (t4 % 2)])
                        s.op("dve", lambda e, bu=bu, t4=t4, hb=hb: e.tensor_tensor(hT[hb][:, t4, :], sgt[t4 % 2], ps[:, bu, :], OP.mult),
                             rd=["ps%d" % bu, "esg%d" % (t4 % 2)], wr=["ehT%d" % hb])
                    if pend is not None:
                        down(*pend)
                    pend = (hh, ex, blk, tt, jb, hb)
                    if tt == 1:
                        for _ in range((2, 2, 2, 2, 3, 2, 2)[blk]):
                            if nxt:
                                nxt.pop(0)(rd=["ehT%d" % hb])
    down(*pend)


def build_F():
    k = K()
    x = k.din("x", [4096, 1024]); xo = k.din("xo", [4096, 1024])
    mem = k.din("mem", [256, 1024]); wkv = k.din("w_mem_kv", [1024, 512])
    w_in_a = k.din("w_in_a", [1024, 1792]); sg_ln_g = k.din("sg_ln_g", [768]); sg_ln_b = k.din("sg_ln_b", [768])
    sg_w = k.din("sg_w", [8, 128, 128]); sg_b = k.din("sg_b", [8, 128])
    w_out = k.din("w_out", [2, 1024, 1024]); ln_g = k.din("ln_g", [2, 2, 1024]); ln_b = k.din("ln_b", [2, 2, 1024])
    wg = k.din("w_ff_gate", [1024, 2816]); wu = k.din("w_ff_up", [1024, 2816]); wd = k.din("w_ff_down", [2816, 1024])
    w_in_b = k.din("w_in_b", [1024, 2560]); dlf = k.din("decay_logit_f", [4]); dlb = k.din("decay_logit_b", [4])
    gn_g = k.din("ret_gn_g", [768]); gn_b = k.din("ret_gn_b", [768])
    w_router = k.din("w_router", [1024, 8]); b_router = k.din("b_router", [8])
    weg = k.din("w_e_gate", [8, 1024, 3584]); weu = k.din("w_e_up", [8, 1024, 3584]); wed = k.din("w_e_down", [8, 3584, 1024])
    posb = k.din("posb", [1]); posbo = k.din("posbo", [1]); masks = k.din("masks", [2])
    out = k.dout("out", [4096, 1024])
    X1 = k.dscr("X1", [4096, 1024], F32); X1o = k.dscr("X1o", [4096, 1024], F32)
    st = k.dscr("st_oth", [192, 768], F32); st2 = k.dscr("st_own", [192, 768], F32)
    wkv_bf = k.dscr("wkv_bf", [1024, 512], BF16); w_in_bf = k.dscr("w_in_a_bf", [1024, 1792], BF16)
    w_out_bf = k.dscr("w_out_bf", [2048, 1024], BF16)
    wg_bf = k.dscr("wg_bf", [1024, 2816], BF16); wu_bf = k.dscr("wu_bf", [1024, 2816], BF16); wd_bf = k.dscr("wd_bf", [2816, 1024], BF16)
    w_in_b_bf = k.dscr("w_in_b_bf", [1024, 2560], BF16)
    weg_bf = k.dscr("weg_bf", [8 * 1024, 3584], BF16); weu_bf = k.dscr("weu_bf", [8 * 1024, 3584], BF16)
    wed_bf = k.dscr("wed_bf", [8 * 3584, 1024], BF16)
    X1N = k.dscr("X1N", [4096, 1024], F32); X1NT = k.dscr("X1NT", [32, 128, 1024], BF16)
    k.cast_dram(w_in_bf, w_in_a); k.cast_dram(wkv_bf, wkv)
    k.cast_dram(w_out_bf, w_out.rearrange("a k n -> (a k) n"))
    k.cast_dram(wg_bf.rearrange("k (a n) -> (k a) n", a=2), wg.rearrange("k (a n) -> (k a) n", a=2))
    k.cast_dram(wu_bf.rearrange("k (a n) -> (k a) n", a=2), wu.rearrange("k (a n) -> (k a) n", a=2))
    k.cast_dram(wd_bf, wd)
    late = [lambda: k.cast_dram(w_in_b_bf.rearrange("k (a n) -> (k a) n", a=2), w_in_b.rearrange("k (a n) -> (k a) n", a=2))]
    memkT, vmem = setup_mem(k, mem, wkv_bf)
    GT = k.sb("GT", [128, 32, 8], F32)
    bgj = []
    g1 = k.cast_jobs(weg_bf.rearrange("k (a n) -> (k a) n", a=2), weg.rearrange("e k (a n) -> (e k a) n", a=2), rows_per=512)
    g2 = k.cast_jobs(weu_bf.rearrange("k (a n) -> (k a) n", a=2), weu.rearrange("e k (a n) -> (e k a) n", a=2), rows_per=512)
    g3 = k.cast_jobs(wed_bf, wed.rearrange("e k n -> (e k) n"), rows_per=512)
    ecast = []
    for i in range(8):
        pcs = []
        for q in range(4):
            pcs += [g1[i * 4 + q], g2[i * 4 + q]]
        pcs += g3[i * 7:(i + 1) * 7]
        ecast.append(pcs)
    c = setup_l1(k, dlf, dlb, posb)
    setup_rope(k, c, posbo)
    with k.scope():
        phase_l0(k, [(xo, X1o, "X1o"), (x, X1, "X1")], memkT, vmem, w_in_bf, w_out_bf[0:1024, :], wg_bf, wu_bf, wd_bf,
                 sg_w, sg_b, sg_ln_g, sg_ln_b, ln_g[0], ln_b[0], bg=late)
    with k.scope():
        Wb = load_l1_weights(k, w_in_b_bf, None)
        SbAll = k.sb("SbAll", [96, 32, 768], BF16)
        for j in ecast[0]:
            j()
        with k.scope():
            phase_scan(k, c, X1o, Wb, st[0:96, :], st[96:192, :], SbAll, pfx="X1o", u="_o")
        setup_rope(k, c, posb)
        with k.scope():
            phase_scan(k, c, X1, Wb, st2[0:96, :], st2[96:192, :], SbAll, pfx="X1", u="", need_f=False)
        with k.scope():
            phase_l1(k, c, X1, Wb, memkT, vmem, w_out_bf[1024:2048, :], st[0:96, :], st[96:192, :], SbAll, gn_g, gn_b,
                     ln_g[1, 0], ln_b[1, 0], w_router, b_router, X1N, X1NT, GT, masks=masks, st_rd=["Floc_out_o", "Sbend_out_o"])
    with k.scope():
        phase_moe(k, X1N, X1NT, GT, weg_bf, weu_bf, wed_bf, ln_g[1, 1], ln_b[1, 1], out, ecast=ecast)
    k.s.wait_all("sp")
    return k.nc


def kernel(x, mem, w_mem_kv, w_in_a, sg_ln_g, sg_ln_b, sg_w, sg_b, w_in_b, decay_logit_f, decay_logit_b, ret_gn_g, ret_gn_b,
           w_out, ln_g, ln_b, w_ff_gate, w_ff_up, w_ff_down, w_router, b_router, w_e_gate, w_e_up, w_e_down):
    f = lambda a: np.ascontiguousarray(np.asarray(a, dtype=np.float32))
    x = f(x); mem = f(mem)
    n = 8
    common = dict(w_mem_kv=f(w_mem_kv), w_in_a=f(w_in_a)[0], sg_ln_g=f(sg_ln_g)[0], sg_ln_b=f(sg_ln_b)[0], sg_w=f(sg_w)[0],
                  sg_b=f(sg_b)[0], w_out=f(w_out), ln_g=f(ln_g), ln_b=f(ln_b), w_ff_gate=f(w_ff_gate)[0], w_ff_up=f(w_ff_up)[0],
                  w_ff_down=f(w_ff_down)[0], w_in_b=f(w_in_b)[0], decay_logit_f=f(decay_logit_f)[0], decay_logit_b=f(decay_logit_b)[0],
                  ret_gn_g=f(ret_gn_g)[0], ret_gn_b=f(ret_gn_b)[0], w_router=f(w_router)[0], b_router=f(b_router)[0],
                  w_e_gate=f(w_e_gate)[0], w_e_up=f(w_e_up)[0], w_e_down=f(w_e_down)[0])
    in_maps = []
    for c in range(n):
        b, hf = c // 2, c % 2
        in_maps.append(dict(common, x=np.ascontiguousarray(x[b, hf * 4096:(hf + 1) * 4096]),
                            xo=np.ascontiguousarray(x[b, (1 - hf) * 4096:(2 - hf) * 4096]),
                            mem=np.ascontiguousarray(mem[b]), posb=np.full([1], float(hf * 4096), np.float32),
                            posbo=np.full([1], float((1 - hf) * 4096), np.float32),
                            masks=np.array([float(hf == 1), float(hf == 0)], np.float32)))
    nc = build_F()
    res = run_bass_kernel_spmd(nc, in_maps, core_ids=list(range(n))).results
    out = np.zeros([4, 8192, 1024], np.float32)
    for c in range(n):
        out[c // 2, (c % 2) * 4096:(c % 2 + 1) * 4096] = res[c]["out"]
    return out
```

```python
import numpy as np
import concourse.bass as bass
import concourse.mybir as mybir

F32 = mybir.dt.float32
BF16 = mybir.dt.bfloat16
I32 = mybir.dt.int32
AF = mybir.ActivationFunctionType
OP = mybir.AluOpType
AX = mybir.AxisListType

NDS = 40
NPS = 8


class S:
    def __init__(self, nc):
        self.nc = nc
        self.eng = dict(pe=nc.tensor, act=nc.scalar, dve=nc.vector, pool=nc.gpsimd, sp=nc.sync)
        self.sem = {k: nc.alloc_semaphore("s_" + k) for k in ("pe", "act", "dve", "pool")}
        self.cnt = {k: 0 for k in self.sem}
        self.dsem = [nc.alloc_semaphore("d%d" % i) for i in range(NDS + NPS)]
        self.dcnt = [0] * (NDS + NPS)
        self.dnext = 0
        self.pnext = 0
        self.waited = {}
        self.w = {}
        self.r = {}
        self.nins = 0

    def _semof(self, sk):
        return self.sem[sk] if isinstance(sk, str) else self.dsem[sk[1]]

    def _wait(self, e, toks):
        best = {}
        for sk, v in toks:
            if sk == e and e == "pe":
                continue
            if best.get(sk, 0) < v:
                best[sk] = v
        for sk, v in best.items():
            if self.waited.get((e, sk), 0) >= v:
                continue
            self.eng[e].wait_ge(self._semof(sk), v)
            self.waited[(e, sk)] = v
            self.nins += 1

    def _deps(self, rd, wr):
        toks = []
        for res in rd:
            if res in self.w:
                toks.append(self.w[res])
        for res in wr:
            if res in self.w:
                toks.append(self.w[res])
            toks.extend(self.r.get(res, ()))
        return toks

    def _record(self, tok, rd, wr):
        for res in wr:
            self.w[res] = tok
            self.r[res] = []
        for res in rd:
            lst = self.r.setdefault(res, [])
            lst[:] = [t for t in lst if t[0] != tok[0]]
            lst.append(tok)

    def op(self, e, fn, rd=(), wr=()):
        self._wait(e, self._deps(rd, wr))
        ins = fn(self.eng[e])
        self.cnt[e] += 1
        ins.then_inc(self.sem[e], 1)
        self.nins += 1
        tok = (e, self.cnt[e])
        self._record(tok, rd, wr)
        return tok

    def mm(self, out, wr, pairs, rd, start=True, stop=True):
        e = "pe"
        self._wait(e, self._deps(rd, [wr]))
        n = len(pairs)
        ins = None
        for i, (a, b) in enumerate(pairs):
            ins = self.nc.tensor.matmul(out, a, b, start=(start and i == 0), stop=(stop and i == n - 1))
            self.nins += 1
        self.cnt[e] += 1
        ins.then_inc(self.sem[e], 1)
        tok = (e, self.cnt[e])
        self._record(tok, rd, [wr])
        return tok

    def tr(self, outs_ins, wr, rd, ident):
        e = "pe"
        self._wait(e, self._deps(list(rd) + ["ident"], [wr]))
        ins = None
        for o, i in outs_ins:
            ins = self.nc.tensor.transpose(o, i, ident)
            self.nins += 1
        self.cnt[e] += 1
        ins.then_inc(self.sem[e], 1)
        tok = (e, self.cnt[e])
        self._record(tok, list(rd) + ["ident"], [wr])
        return tok

    def dma(self, q, out, in_, rd=(), wr=(), **kw):
        if q == "pool":
            i = NDS + self.pnext
            self.pnext = (self.pnext + 1) % NPS
        else:
            i = self.dnext
            self.dnext = (self.dnext + 1) % NDS
        toks = self._deps(rd, wr)
        if self.dcnt[i] > 0:
            toks.append((("d", i), self.dcnt[i]))
        self._wait(q, toks)
        ins = self.eng[q].dma_start(out, in_, **kw)
        self.dcnt[i] += 16
        ins.then_inc(self.dsem[i], 16)
        self.nins += 1
        tok = (("d", i), self.dcnt[i])
        self._record(tok, rd, wr)
        return tok

    def wait_all(self, e):
        toks = [(k, v) for k, v in self.cnt.items() if v > 0]
        toks += [(("d", i), v) for i, v in enumerate(self.dcnt) if v > 0]
        self._wait(e, toks)


from concourse.bass_utils import run_bass_kernel_spmd

ALPHA = float((2.0 * 2) ** 0.25)
EPS = 1e-5
NTOK = 4096


import os
from contextlib import contextmanager, ExitStack


class Stop(Exception):
    pass


class K:
    def ck(self, name):
        if os.environ.get("STOP") == name:
            raise Stop()

    @contextmanager
    def scope(self):
        st = ExitStack()
        self.stacks.append(st)
        try:
            yield
        finally:
            self.barrier()
            self.stacks.pop()
            st.close()

    def barrier(self):
        for e in ("pe", "act", "dve", "pool", "sp"):
            self.s.wait_all(e)

    def __init__(self):
        nc = bass.Bass("TRN2", target_bir_lowering=False)
        self.nc = nc
        self.stacks = [ExitStack()]
        self.s = S(nc)
        self.ps = nc.alloc_psum_tensor("ps", [128, 8, 512], F32).ap()
        self.banks = [0, 1, 2, 3]
        self.bi = 0
        self.ev = 0
        self.uid = 0
        self.ident = self.sb("ident", [128, 128], F32)
        s = self.s
        s.op("pool", lambda e: e.memset(self.ident, 0.0), wr=["ident"])
        s.op("pool", lambda e: e.affine_select(out=self.ident, in_=self.ident, compare_op=OP.not_equal, fill=1.0,
                                               base=0, pattern=[[-1, 128]], channel_multiplier=1),
             rd=["ident"], wr=["ident"])
        self.small = []
        for i in range(4):
            self.small.append((self.sb("st%d" % i, [128, 2, 6], F32), self.sb("mv%d" % i, [128, 2], F32),
                               self.sb("rs%d" % i, [128, 1], F32), "sm%d" % i))
        self.smi = 0

    def din(self, name, shape, dt=F32):
        return self.nc.dram_tensor(name, list(shape), dt, kind="ExternalInput").ap()

    def dout(self, name, shape, dt=F32):
        return self.nc.dram_tensor(name, list(shape), dt, kind="ExternalOutput").ap()

    def dscr(self, name, shape, dt=F32):
        return self.nc.dram_tensor(name, list(shape), dt).ap()

    def sb(self, name, shape, dt):
        return self.stacks[-1].enter_context(self.nc.sbuf_tensor(name, list(shape), dt)).ap()

    def bank(self):
        b = self.banks[self.bi % len(self.banks)]
        self.bi += 1
        return b

    def evac(self, out, in_, rd, wr, eng=None):
        if eng is None:
            eng = ("dve", "act")[self.ev % 2]
            self.ev += 1
        if eng == "act":
            return self.s.op("act", lambda e: e.copy(out, in_), rd=rd, wr=wr)
        return self.s.op(eng, lambda e: e.tensor_copy(out, in_), rd=rd, wr=wr)

    def cast_dram(self, dst, src, rows_per=4096):
        R = src.shape[0]
        for r0 in range(0, R, rows_per):
            r1 = min(R, r0 + rows_per)
            self.s.dma("pool", dst[r0:r1, :], src[r0:r1, :], rd=[], wr=[dst.name])

    def cast_jobs(self, dst, src, rows_per=2048):
        R = src.shape[0]
        jobs = []
        for r0 in range(0, R, rows_per):
            r1 = min(R, r0 + rows_per)
            jobs.append(lambda rd=(), r0=r0, r1=r1: self.s.dma("pool", dst[r0:r1, :], src[r0:r1, :], rd=list(rd), wr=[dst.name + "_%d" % r0]))
        return jobs

    def transpose_tok(self, src, src_res, dstT, dst_res, col0, nk=8):
        s = self.s
        for h in range(0, nk, 4):
            b = self.bank()
            n = min(4, nk - h)
            s.tr([(self.ps[:, b, i * 128:(i + 1) * 128], src[:, (h + i) * 128:(h + i + 1) * 128]) for i in range(n)],
                 "ps%d" % b, [src_res], self.ident)
            self.evac(dstT[:, h:h + n, col0:col0 + 128],
                      self.ps[:, b, 0:n * 128].rearrange("p (k t) -> p k t", k=n), rd=["ps%d" % b], wr=[dst_res], eng="act")

    def ln_inplace(self, X, res, gt, bt, out=None, out_res=None):
        s = self.s
        st, mv, rs, sm = self.small[self.smi % 4]
        self.smi += 1
        W = X.shape[-1]
        nch = (W + 511) // 512
        cw = W // nch
        for c in range(nch):
            s.op("dve", lambda e, c=c: e.bn_stats(st[:, c, :], X[:, c * cw:(c + 1) * cw]), rd=[res], wr=[sm])
        s.op("dve", lambda e: e.bn_aggr(mv, st[:, 0:nch, :].rearrange("p a b -> p (a b)")), rd=[sm], wr=[sm])
        s.op("act", lambda e: e.activation(rs, mv[:, 1:2], AF.Ln, bias=EPS), rd=[sm], wr=[sm])
        s.op("act", lambda e: e.activation(rs, rs, AF.Exp, scale=-0.5), rd=[sm], wr=[sm])
        if gt is None:
            s.op("dve", lambda e: e.tensor_scalar(X, X, mv[:, 0:1], rs, OP.subtract, OP.mult), rd=[res, sm], wr=[res])
        else:
            s.op("dve", lambda e: e.scalar_tensor_tensor(X, X, mv[:, 0:1], gt, OP.subtract, OP.mult), rd=[res, sm, "lnc"], wr=[res])
            s.op("dve", lambda e: e.scalar_tensor_tensor(out if out is not None else X, X, rs, bt, OP.mult, OP.add),
                 rd=[res, sm, "lnc"], wr=[out_res if out is not None else res])


def setup_mem(k, mem, wkv_bf):
    s, ps = k.s, k.ps
    memkT = k.sb("memkT", [128, 2, 256], BF16)
    vmem = k.sb("vmem", [128, 2, 256], BF16)
    with k.scope():
        _setup_mem(k, mem, wkv_bf, memkT, vmem)
    return memkT, vmem


def _setup_mem(k, mem, wkv_bf, memkT, vmem):
    s, ps = k.s, k.ps
    memX = k.sb("memX", [128, 2, 1024], F32)
    memT = k.sb("memT", [128, 8, 256], BF16)
    Wkv = k.sb("Wkv", [128, 8, 512], BF16)
    s.dma("sp", memX, mem.rearrange("(t p) d -> p t d", p=128), wr=["memX"])
    s.dma("sp", Wkv, wkv_bf.rearrange("(k p) n -> p k n", p=128), rd=[wkv_bf.name], wr=["Wkv"])
    for t in range(2):
        k.transpose_tok(memX[:, t, :], "memX", memT, "memT", t * 128)
    for hp in range(2):
        b = k.bank()
        s.mm(ps[:, b, 0:256], "ps%d" % b, [(Wkv[:, kk, hp * 128:(hp + 1) * 128], memT[:, kk, :]) for kk in range(8)],
             ["Wkv", "memT"])
        s.op("act", lambda e, b=b, hp=hp: e.mul(memkT[:, hp, :], ps[:, b, 0:256], 0.125), rd=["ps%d" % b], wr=["memkT"])
    for mt in range(2):
        b = k.bank()
        s.mm(ps[:, b, 0:256], "ps%d" % b,
             [(memT[:, kk, mt * 128:(mt + 1) * 128], Wkv[:, kk, 256:512]) for kk in range(8)], ["Wkv", "memT"])
        k.evac(vmem[:, mt, :], ps[:, b, 0:256], rd=["ps%d" % b], wr=["vmem"])
    k.ck("mem")
    return memkT, vmem


def mem_attn_a(k, mqT, mq_res, memkT, vmem, memoT, memo_res, s_, bufs):
    s, ps = k.s, k.ps
    probs, probsT, rmax, rsum = bufs
    c0 = s_ * 128
    for h2 in range(2):
        b = k.bank()
        for hp in range(2):
            s.mm(ps[:, b, hp * 256:(hp + 1) * 256], "ps%d" % b,
                 [(mqT[h2 * 64:(h2 + 1) * 64, hp, c0:c0 + 128], memkT[h2 * 64:(h2 + 1) * 64, hp, :])],
                 [mq_res, "memkT"])
        s.op("dve", lambda e, b=b, h2=h2: e.tensor_reduce(rmax[:, h2 * 2:(h2 + 1) * 2],
                                                         ps[:, b, :].rearrange("p (h m) -> p h m", h=2),
                                                         AX.X, OP.max, negate=True),
             rd=["ps%d" % b], wr=["rmax"])
        for hp in range(2):
            h = hp * 2 + h2
            s.op("act", lambda e, b=b, h=h, hp=hp, h2=h2: e.activation(probs[:, h, :], ps[:, b, hp * 256:(hp + 1) * 256], AF.Exp,
                                                               bias=rmax[:, h2 * 2 + hp:h2 * 2 + hp + 1], scale=1.0,
                                                               accum_out=rsum[:, h:h + 1]),
                 rd=["ps%d" % b, "rmax"], wr=["probs", "rsum"])
    s.op("dve", lambda e: e.reciprocal(rsum, rsum), rd=["rsum"], wr=["rsum"])
    for h in range(4):
        s.op("act", lambda e, h=h: e.mul(probs[:, h, :], probs[:, h, :], rsum[:, h:h + 1]),
             rd=["probs", "rsum"], wr=["probs"])


def mem_attn_b(k, mqT, mq_res, memkT, vmem, memoT, memo_res, s_, bufs):
    s, ps = k.s, k.ps
    probs, probsT, rmax, rsum = bufs
    c0 = s_ * 128
    for hp in range(2):
        b = k.bank()
        s.tr([(ps[:, b, (h2 * 2 + mt) * 128:(h2 * 2 + mt + 1) * 128], probs[:, hp * 2 + h2, mt * 128:(mt + 1) * 128])
              for h2 in range(2) for mt in range(2)], "ps%d" % b, ["probs"], k.ident)
        k.evac(probsT[:, hp * 2:(hp + 1) * 2, :, :].rearrange("p h m t -> p (h m t)"), ps[:, b, :],
               rd=["ps%d" % b], wr=["probsT"])
    b = k.bank()
    for h in range(4):
        hp, h2 = h // 2, h % 2
        s.mm(ps[h2 * 64:(h2 + 1) * 64, b, hp * 128:(hp + 1) * 128], "ps%d" % b,
             [(vmem[:, mt, h * 64:(h + 1) * 64], probsT[:, h, mt, :]) for mt in range(2)], ["vmem", "probsT"])
    k.evac(memoT[:, :, c0:c0 + 128], ps[:, b, 0:256].rearrange("p (t c) -> p t c", t=2), rd=["ps%d" % b], wr=[memo_res])


def mem_attn(k, mqT, mq_res, memkT, vmem, memoT, memo_res, s_, bufs):
    mem_attn_a(k, mqT, mq_res, memkT, vmem, memoT, memo_res, s_, bufs)
    mem_attn_b(k, mqT, mq_res, memkT, vmem, memoT, memo_res, s_, bufs)


def phase_l0(k, jobs, memkT, vmem, w_in_bf, w_out_bf, wg_bf, wu_bf, wd_bf, sg_w, sg_b, sg_ln_g, sg_ln_b, ln_g, ln_b, ntt=16, bg=None):
    s, ps = k.s, k.ps
    T = 256
    Win = k.sb("Win", [128, 8, 1792], BF16)
    Wom = k.sb("Wom", [96, 8, 1024], BF16)
    Woq = k.sb("Woq", [128, 2, 1024], BF16)
    s.dma("sp", Win, w_in_bf.rearrange("(k p) n -> p k n", p=128), rd=[w_in_bf.name], wr=["Win"])
    s.dma("sp", Wom, w_out_bf[0:768, :].rearrange("(g p) n -> p g n", p=96), rd=[w_out_bf.name], wr=["Wom"])
    s.dma("sp", Woq, w_out_bf[768:1024, :].rearrange("(t p) n -> p t n", p=128), rd=[w_out_bf.name], wr=["Woq"])
    wsX = k.sb("wsX", [128, 8, 128], F32)
    wsT = k.sb("wsT", [128, 8, 128], BF16)
    s.dma("sp", wsX, sg_w.rearrange("g p q -> p g q"), wr=["wsX"])
    for h in range(2):
        b = k.bank()
        s.tr([(ps[:, b, i * 128:(i + 1) * 128], wsX[:, h * 4 + i, :]) for i in range(4)], "ps%d" % b, ["wsX"], k.ident)
        k.evac(wsT[:, h * 4:(h + 1) * 4, :].rearrange("p g q -> p (g q)"), ps[:, b, :], rd=["ps%d" % b], wr=["wsT"])
    bsb = k.sb("bsb", [96, 8, 128], F32)
    s.dma("sp", bsb.rearrange("p g q -> p (g q)"), sg_b.rearrange("g q -> (g q)").partition_broadcast(96), wr=["bsb"])
    lng = k.sb("lng", [128, 768], F32)
    lnb = k.sb("lnb", [128, 768], F32)
    s.dma("sp", lng, sg_ln_g.partition_broadcast(128), wr=["lnc"])
    s.dma("sp", lnb, sg_ln_b.partition_broadcast(128), wr=["lnc"])
    G = [[k.sb("lg%d%d" % (i, j), [128, 1024], F32) for j in range(2)] for i in range(2)]
    for i in range(2):
        s.dma("sp", G[i][0], ln_g[i].partition_broadcast(128), wr=["lnc"])
        s.dma("sp", G[i][1], ln_b[i].partition_broadcast(128), wr=["lnc"])

    XA = [k.sb("XA%d" % i, [128, 2, 1024], F32) for i in range(3)]
    X0T = k.sb("X0T", [128, 8, T], BF16)
    X1T = [k.sb("X1T%d" % i, [128, 8, T], BF16) for i in range(2)]
    uT = k.sb("uT", [96, 8, T], BF16)
    mqT = k.sb("mqT", [128, 2, T], BF16)
    memoT = k.sb("memoT", [128, 2, T], BF16)
    Vf = [k.sb("Vf%d" % i, [128, 768], F32) for i in range(2)]
    vn = [k.sb("vn%d" % i, [128, 768], BF16) for i in range(2)]
    tmp = k.sb("tmpmix", [96, 4, 128], F32)
    probs = k.sb("probs", [128, 4, 256], F32)
    probsT = k.sb("probsT", [128, 4, 2, 128], BF16)
    rmax = k.sb("rmax", [128, 4], F32)
    rsum = k.sb("rsum", [128, 4], F32)
    NW = 3
    WG = [k.sb("WG%d" % i, [128, 8, 256], BF16) for i in range(NW)]
    WU = [k.sb("WU%d" % i, [128, 8, 256], BF16) for i in range(NW)]
    WD = [k.sb("WD%d" % i, [128, 2, 1024], BF16) for i in range(NW)]
    sg = [k.sb("sg%d" % i, [128, T], BF16) for i in range(2)]
    hT = [k.sb("hT%d" % i, [128, 2, T], BF16) for i in range(2)]
    NT = len(jobs) * ntt
    NBLK = 11

    def wload(g):
        if g >= NT * NBLK:
            return
        j = g % NBLK
        wb = g % NW
        s.dma("sp", WG[wb], wg_bf[:, j * 256:(j + 1) * 256].rearrange("(k p) n -> p k n", p=128), rd=[wg_bf.name + "_%d" % j], wr=["WG%d" % wb])
        s.dma("sp", WU[wb], wu_bf[:, j * 256:(j + 1) * 256].rearrange("(k p) n -> p k n", p=128), rd=[wu_bf.name + "_%d" % j], wr=["WU%d" % wb])
        s.dma("sp", WD[wb], wd_bf[j * 256:(j + 1) * 256, :].rearrange("(t p) n -> p t n", p=128), rd=[wd_bf.name + "_%d" % j], wr=["WD%d" % wb])

    def xload(jt):
        if jt >= NT:
            return
        x, X1, pfx = jobs[jt // ntt]
        r0 = (jt % ntt) * T
        s.dma("sp", XA[jt % 3], x[r0:r0 + T, :].rearrange("(s p) d -> p s d", p=128), wr=["XA%d_%d" % (jt % 3, i) for i in range(2)])

    def mixer(jt):
        p = jt % 2
        x, X1, pfx = jobs[jt // ntt]
        tt = jt % ntt
        r0 = tt * T
        xa = XA[jt % 3]
        xrn = ["XA%d_%d" % (jt % 3, i) for i in range(2)]
        for s_ in range(2):
            k.transpose_tok(xa[:, s_, :], xrn[s_], X0T, "X0T", s_ * 128)
        yield
        for s_ in range(2):
            c0 = s_ * 128
            for h2 in range(2):
                b = k.bank()
                s.mm(ps[:, b, 0:384], "ps%d" % b,
                     [(X0T[:, kk, c0:c0 + 128], Win[:, kk, 768 + h2 * 384:768 + (h2 + 1) * 384]) for kk in range(8)],
                     ["Win", "X0T"])
                s.op("act", lambda e, b=b, h2=h2, s_=s_: e.activation(Vf[s_][:, h2 * 384:(h2 + 1) * 384], ps[:, b, 0:384], AF.Gelu),
                     rd=["ps%d" % b], wr=["Vf%d" % s_])
            k.ln_inplace(Vf[s_], "Vf%d" % s_, lng, lnb, out=vn[s_], out_res="vn%d" % s_)
        yield
        for g in range(8):
            b = k.bank()
            s.mm(ps[0:96, b, 0:T], "ps%d" % b, [(Win[:, kk, g * 96:(g + 1) * 96], X0T[:, kk, :]) for kk in range(8)],
                 ["Win", "X0T"])
            s.op("act", lambda e, b=b, g=g: e.activation(uT[:, g, :], ps[0:96, b, 0:T], AF.Gelu), rd=["ps%d" % b], wr=["uT"])
        for t in range(2):
            b = k.bank()
            s.mm(ps[:, b, 0:T], "ps%d" % b,
                 [(Win[:, kk, 1536 + t * 128:1536 + (t + 1) * 128], X0T[:, kk, :]) for kk in range(8)], ["Win", "X0T"])
            k.evac(mqT[:, t, :], ps[:, b, 0:T], rd=["ps%d" % b], wr=["mqT"])
        yield
        for s_ in range(2):
            c0 = s_ * 128
            for gh in range(2):
                b = k.bank()
                for g4 in range(4):
                    g = gh * 4 + g4
                    s.mm(ps[0:96, b, g4 * 128:(g4 + 1) * 128], "ps%d" % b, [(vn[s_][:, g * 96:(g + 1) * 96], wsT[:, g, :])],
                         ["vn%d" % s_, "wsT"])
                s.op("dve", lambda e, b=b, gh=gh: e.tensor_tensor(tmp, ps[0:96, b, :].rearrange("p (g q) -> p g q", g=4),
                                                                 bsb[:, gh * 4:(gh + 1) * 4, :], OP.add),
                     rd=["ps%d" % b, "bsb"], wr=["tmpmix"])
                s.op("dve", lambda e, gh=gh, c0=c0: e.tensor_tensor(uT[:, gh * 4:(gh + 1) * 4, c0:c0 + 128], tmp,
                                                                   uT[:, gh * 4:(gh + 1) * 4, c0:c0 + 128], OP.mult),
                     rd=["tmpmix", "uT"], wr=["uT"])
        yield
        for s_ in range(2):
            mem_attn_a(k, mqT, "mqT", memkT, vmem, memoT, "memoT", s_, (probs, probsT, rmax, rsum))
            yield
            mem_attn_b(k, mqT, "mqT", memkT, vmem, memoT, "memoT", s_, (probs, probsT, rmax, rsum))
            yield
        for s_ in range(2):
            c0 = s_ * 128
            for nh in range(2):
                b = k.bank()
                s.mm(ps[:, b, :], "ps%d" % b,
                     [(uT[:, g, c0:c0 + 128], Wom[:, g, nh * 512:(nh + 1) * 512]) for g in range(8)] +
                     [(memoT[:, t, c0:c0 + 128], Woq[:, t, nh * 512:(nh + 1) * 512]) for t in range(2)],
                     ["uT", "memoT", "Wom", "Woq"])
                s.op("dve", lambda e, b=b, nh=nh, s_=s_: e.scalar_tensor_tensor(
                    xa[:, s_, nh * 512:(nh + 1) * 512], xa[:, s_, nh * 512:(nh + 1) * 512], ALPHA, ps[:, b, :],
                    OP.mult, OP.add), rd=["ps%d" % b, xrn[s_]], wr=[xrn[s_]])
            k.ln_inplace(xa[:, s_, :], xrn[s_], G[0][0], G[0][1])
            yield
        for s_ in range(2):
            k.transpose_tok(xa[:, s_, :], xrn[s_], X1T[p], "X1T%d" % p, s_ * 128)
        yield

    def ffn(jt):
        p = jt % 2
        x, X1, pfx = jobs[jt // ntt]
        tt = jt % ntt
        r0 = tt * T
        xa = XA[jt % 3]
        xt = X1T[p]
        xtn = "X1T%d" % p
        xrn = ["XA%d_%d" % (jt % 3, i) for i in range(2)]

        def down(j):
            g = jt * NBLK + j
            wb = g % NW
            hb = g % 2
            for s_ in range(2):
                for nh in range(2):
                    ab = 4 + s_ * 2 + nh
                    s.mm(ps[:, ab, :], "ps%d" % ab,
                         [(hT[hb][:, t2, s_ * 128:(s_ + 1) * 128], WD[wb][:, t2, nh * 512:(nh + 1) * 512]) for t2 in range(2)],
                         ["hT%d" % hb, "WD%d" % wb], start=(j == 0), stop=(j == NBLK - 1))

        for j in range(NBLK):
            g = jt * NBLK + j
            wb = g % NW
            hb = g % 2
            if g == 0:
                wload(0)
            wload(g + 1)
            for t2 in range(2):
                bg_ = k.bank()
                bu = k.bank()
                s.mm(ps[:, bg_, 0:T], "ps%d" % bg_, [(WG[wb][:, kk, t2 * 128:(t2 + 1) * 128], xt[:, kk, :]) for kk in range(8)],
                     ["WG%d" % wb, xtn])
                s.mm(ps[:, bu, 0:T], "ps%d" % bu, [(WU[wb][:, kk, t2 * 128:(t2 + 1) * 128], xt[:, kk, :]) for kk in range(8)],
                     ["WU%d" % wb, xtn])
                s.op("act", lambda e, bg_=bg_, t2=t2: e.activation(sg[t2], ps[:, bg_, 0:T], AF.Silu), rd=["ps%d" % bg_],
                     wr=["sg%d" % t2])
                s.op("dve", lambda e, bu=bu, t2=t2, hb=hb: e.tensor_tensor(hT[hb][:, t2, :], sg[t2], ps[:, bu, 0:T], OP.mult),
                     rd=["ps%d" % bu, "sg%d" % t2], wr=["hT%d" % hb])
            if j > 0:
                down(j - 1)
            yield
        down(NBLK - 1)
        for s_ in range(2):
            for nh in range(2):
                ab = 4 + s_ * 2 + nh
                s.op("dve", lambda e, ab=ab, nh=nh, s_=s_: e.scalar_tensor_tensor(
                    xa[:, s_, nh * 512:(nh + 1) * 512], xa[:, s_, nh * 512:(nh + 1) * 512], ALPHA, ps[:, ab, :],
                    OP.mult, OP.add), rd=["ps%d" % ab, xrn[s_]], wr=[xrn[s_]])
            k.ln_inplace(xa[:, s_, :], xrn[s_], G[1][0], G[1][1])
            s.dma("sp", X1[r0 + s_ * 128:r0 + (s_ + 1) * 128, :], xa[:, s_, :], rd=[xrn[s_]], wr=[pfx + "_%d" % (tt * 2 + s_)])
        yield

    bg = list(bg) if bg else []
    xload(0)
    xload(1)
    for _ in mixer(0):
        pass
    sched = [1] * 11 + [0]
    for jt in range(NT):
        m = mixer(jt + 1) if jt + 1 < NT else None
        xload(jt + 2)
        for bi, _ in enumerate(ffn(jt)):
            for _r in range(sched[bi] if bi < len(sched) else 1):
                if m is not None:
                    try:
                        next(m)
                    except StopIteration:
                        m = None
        while m is not None:
            try:
                next(m)
            except StopIteration:
                m = None
        if bg:
            bg.pop(0)()
    while bg:
        bg.pop(0)()


def bc(ap, dims):
    pstep = ap.ap[0]
    return bass.AP(ap.tensor, ap.offset, [list(pstep)] + [list(d) for d in dims])


DK = 96
DV = 192
QS = float(DK ** -0.5)
TWO_PI = float(2 * np.pi)


def setup_l1(k, dlf, dlb, posb):
    c = {}
    c["lg"] = k.sb("lg", [128, 8], F32)
    c["fac"] = k.sb("fac", [128, 4, 4], F32)
    c["g128"] = k.sb("g128", [128, 8], F32)
    c["pw"] = k.sb("pw", [128, 8, 32], F32)
    c["maskT"] = k.sb("maskT", [128, 4, 128], F32)
    c["cos"] = k.sb("cos", [128, 32, 48], F32)
    c["sin"] = k.sb("sin", [128, 32, 48], F32)
    c["A"] = k.sb("ropeA", [128, 768], F32)
    c["B"] = k.sb("ropeB", [128, 768], F32)
    with k.scope():
        _setup_l1(k, c, dlf, dlb, posb)
    return c


def _setup_l1(k, c, dlf, dlb, posb):
    s = k.s
    lg, fac, g128, pw, maskT, cos, sin = c["lg"], c["fac"], c["g128"], c["pw"], c["maskT"], c["cos"], c["sin"]
    s.dma("sp", lg[:, 0:4], dlf.partition_broadcast(128), wr=["lg"])
    s.dma("sp", lg[:, 4:8], dlb.partition_broadcast(128), wr=["lg"])
    s.op("act", lambda e: e.activation(lg, lg, AF.Exp, scale=-1.0), rd=["lg"], wr=["lg"])
    s.op("act", lambda e: e.activation(lg, lg, AF.Ln, bias=1.0), rd=["lg"], wr=["lg"])
    s.op("dve", lambda e: e.tensor_scalar(lg, lg, -1.0, None, OP.mult), rd=["lg"], wr=["lg"])
    pidx_i = k.sb("pidx_i", [128, 1], I32)
    pidx = k.sb("pidx", [128, 1], F32)
    s.op("pool", lambda e: e.iota(pidx_i, [[0, 1]], base=0, channel_multiplier=1), wr=["pidx_i"])
    s.op("dve", lambda e: e.tensor_copy(pidx, pidx_i), rd=["pidx_i"], wr=["pidx"])
    q = k.sb("qtmp", [128, 1], F32)
    specs = [(0, 1.0, 0.0, QS, 0), (1, -1.0, 127.0, QS, 4), (2, -1.0, 128.0, 1.0, 0), (3, 1.0, 1.0, 1.0, 4)]
    for idx, a, b, m, lo in specs:
        s.op("dve", lambda e, a=a, b=b: e.tensor_scalar(q, pidx, a, b, OP.mult, OP.add), rd=["pidx"], wr=["qtmp"])
        s.op("dve", lambda e, idx=idx, lo=lo: e.tensor_scalar(fac[:, idx, :], lg[:, lo:lo + 4], q, None, OP.mult),
             rd=["qtmp", "lg"], wr=["fac"])
        s.op("act", lambda e, idx=idx: e.activation(fac[:, idx, :], fac[:, idx, :], AF.Exp), rd=["fac"], wr=["fac"])
        if m != 1.0:
            s.op("dve", lambda e, idx=idx, m=m: e.tensor_scalar(fac[:, idx, :], fac[:, idx, :], m, None, OP.mult),
                 rd=["fac"], wr=["fac"])
    s.op("act", lambda e: e.activation(g128, lg, AF.Exp, scale=128.0), rd=["lg"], wr=["g128"])
    nidx_i = k.sb("nidx_i", [128, 32], I32)
    nidx = k.sb("nidx", [128, 32], F32)
    s.op("pool", lambda e: e.iota(nidx_i, [[-128, 32]], base=128 * 31, channel_multiplier=0), wr=["nidx_i"])
    s.op("dve", lambda e: e.tensor_copy(nidx, nidx_i), rd=["nidx_i"], wr=["nidx"])
    for j in range(8):
        s.op("dve", lambda e, j=j: e.tensor_scalar(pw[:, j, :], nidx, lg[:, j:j + 1], None, OP.mult), rd=["nidx", "lg"], wr=["pw"])
    s.op("act", lambda e: e.activation(pw, pw, AF.Exp), rd=["pw"], wr=["pw"])
    D_i = k.sb("D_i", [128, 128], I32)
    Dp = k.sb("Dp", [128, 128], F32)
    Dn = k.sb("Dn", [128, 128], F32)
    E1 = k.sb("E1", [128, 128], F32)
    s.op("pool", lambda e: e.iota(D_i, [[1, 128]], base=0, channel_multiplier=-1), wr=["D_i"])
    s.op("dve", lambda e: e.tensor_copy(Dn, D_i), rd=["D_i"], wr=["Dn"])
    s.op("dve", lambda e: e.tensor_scalar(Dp, Dn, 0.0, None, OP.max), rd=["Dn"], wr=["Dp"])
    s.op("dve", lambda e: e.tensor_tensor(Dn, Dp, Dn, OP.subtract), rd=["Dp", "Dn"], wr=["Dn"])
    for h in range(4):
        s.op("dve", lambda e, h=h: e.tensor_scalar(E1, Dp, lg[:, h:h + 1], None, OP.mult), rd=["Dp", "lg"], wr=["E1"])
        s.op("dve", lambda e, h=h: e.scalar_tensor_tensor(E1, Dn, lg[:, 4 + h:5 + h], E1, OP.mult, OP.add),
             rd=["Dn", "lg", "E1"], wr=["E1"])
        s.op("act", lambda e, h=h: e.activation(maskT[:, h, :], E1, AF.Exp), rd=["E1"], wr=["maskT"])
    s.op("dve", lambda e: e.tensor_scalar(maskT, maskT, QS, None, OP.mult), rd=["maskT"], wr=["maskT"])


def setup_rope(k, c, posb):
    with k.scope():
        _setup_rope(k, c, posb)


def _setup_rope(k, c, posb):
    s = k.s
    cos, sin = c["cos"], c["sin"]
    k.uid += 1
    u = "_%d" % k.uid
    pb = k.sb("pb" + u, [128, 1], F32)
    s.dma("sp", pb, posb.partition_broadcast(128), wr=["pb"])
    ii = k.sb("ii" + u, [128, 48], I32)
    inv = k.sb("inv" + u, [128, 48], F32)
    s.op("pool", lambda e: e.iota(ii, [[1, 48]], base=0, channel_multiplier=0), wr=["ii"])
    s.op("dve", lambda e: e.tensor_copy(inv, ii), rd=["ii"], wr=["inv"])
    s.op("act", lambda e: e.activation(inv, inv, AF.Exp, scale=float(-2.0 * np.log(10000.0) / 96.0)), rd=["inv"], wr=["inv"])
    pos_i = k.sb("pos_i" + u, [128, 32], I32)
    pos = k.sb("pos" + u, [128, 32], F32)
    s.op("pool", lambda e: e.iota(pos_i, [[128, 32]], base=0, channel_multiplier=1), wr=["pos_i"])
    s.op("dve", lambda e: e.tensor_copy(pos, pos_i), rd=["pos_i"], wr=["pos"])
    s.op("dve", lambda e: e.tensor_scalar(pos, pos, pb, None, OP.add), rd=["pos", "pb"], wr=["pos"])
    ang = k.sb("ang" + u, [128, 32, 48], F32)
    ri = k.sb("ri" + u, [128, 32, 48], I32)
    rf = k.sb("rf" + u, [128, 32, 48], F32)
    for n in range(32):
        s.op("dve", lambda e, n=n: e.tensor_scalar(ang[:, n, :], inv, pos[:, n:n + 1], None, OP.mult), rd=["inv", "pos"], wr=["ang"])
    for (dst, name, off) in ((sin, "sin", 0.0), (cos, "cos", 0.25)):
        s.op("dve", lambda e, dst=dst, off=off: e.tensor_scalar(dst, ang, float(1.0 / TWO_PI), off, OP.mult, OP.add), rd=["ang"], wr=[name])
        s.op("dve", lambda e, dst=dst: e.tensor_copy(ri, dst), rd=[name], wr=["ri"])
        s.op("dve", lambda e: e.tensor_copy(rf, ri), rd=["ri"], wr=["rf"])
        s.op("dve", lambda e, dst=dst: e.tensor_tensor(dst, dst, rf, OP.subtract), rd=[name, "rf"], wr=[name])
        s.op("dve", lambda e, dst=dst: e.tensor_scalar(rf, dst, 0.5, None, OP.is_gt), rd=[name], wr=["rf"])
        s.op("dve", lambda e, dst=dst: e.tensor_tensor(dst, dst, rf, OP.subtract), rd=[name, "rf"], wr=[name])
        s.op("dve", lambda e, dst=dst: e.tensor_scalar(rf, dst, -0.5, None, OP.is_lt), rd=[name], wr=["rf"])
        s.op("dve", lambda e, dst=dst: e.tensor_tensor(dst, dst, rf, OP.add), rd=[name, "rf"], wr=[name])
        s.op("act", lambda e, dst=dst: e.activation(dst, dst, AF.Sin, scale=TWO_PI), rd=[name], wr=[name])


def rope(k, c, n, src_ps, nh, A, B, dst, rd, wr):
    s = k.s
    cosb = bc(c["cos"][:, n, :], [[0, nh], [0, 2], [1, 48]])
    sinb = bc(c["sin"][:, n, :], [[0, nh], [0, 2], [1, 48]])
    v4 = lambda ap: ap.rearrange("p (h t d) -> p h t d", h=nh, t=2)
    s.op("dve", lambda e: e.tensor_tensor(v4(A), v4(src_ps), cosb, OP.mult), rd=rd + ["cos"], wr=["ropeA"])
    s.op("dve", lambda e: e.tensor_tensor(v4(B), v4(src_ps), sinb, OP.mult), rd=rd + ["sin"], wr=["ropeB"])
    s.op("dve", lambda e: e.tensor_tensor(v4(dst)[:, :, 0, :], v4(A)[:, :, 0, :], v4(B)[:, :, 1, :], OP.subtract),
         rd=["ropeA", "ropeB"], wr=[wr])
    s.op("dve", lambda e: e.tensor_tensor(v4(dst)[:, :, 1, :], v4(A)[:, :, 1, :], v4(B)[:, :, 0, :], OP.add),
         rd=["ropeA", "ropeB"], wr=[wr])


def load_l1_weights(k, w_in_b_bf, w_out1_bf):
    s = k.s
    Wb = k.sb("Wb", [128, 8, 2560], BF16)
    for kk in range(8):
        s.dma("sp", Wb[:, kk, :], w_in_b_bf[kk * 128:(kk + 1) * 128, :], rd=[w_in_b_bf.name], wr=["Wb"])
    return Wb


def phase_scan(k, c, X1, Wb, Floc_out, Sbend_out, SbAll, pfx="X1", u="", need_f=True):
    s, ps = k.s, k.ps
    k.banks = [0, 1, 2, 3, 4, 5, 6, 7]
    X = k.sb("Xs" + u, [128, 1024], F32)
    xT = k.sb("xTs" + u, [128, 8, 128], BF16)
    A, B = c["A"], c["B"]
    kr = k.sb("kr" + u, [128, 384], F32)
    kb = k.sb("kb" + u, [128, 4, 96], BF16)
    kf = k.sb("kf" + u, [128, 4, 96], BF16)
    vb = k.sb("vb" + u, [128, 768], BF16)
    Sb = k.sb("Sb" + u, [96, 4, 192], F32)
    Fl = k.sb("Fl" + u, [96, 4, 192], F32)
    s.op("pool", lambda e: e.memset(Sb, 0.0), wr=["Sb"])
    s.op("pool", lambda e: e.memset(Fl, 0.0), wr=["Fl"])
    fac, g128, pw = c["fac"], c["g128"], c["pw"]
    for n in range(31, -1, -1):
        s.dma("sp", X, X1[n * 128:(n + 1) * 128, :], rd=[pfx + "_%d" % n], wr=["Xs"])
        k.transpose_tok(X, "Xs", xT, "xTs", 0)
        bk = k.bank()
        s.mm(ps[:, bk, 0:384], "ps%d" % bk, [(xT[:, kk, :], Wb[:, kk, 384:768]) for kk in range(8)], ["xTs", "Wb"])
        rope(k, c, n, ps[:, bk, 0:384], 4, A[:, 0:384], B[:, 0:384], kr, ["ps%d" % bk], "kr")
        for h2 in range(2):
            b = k.bank()
            s.mm(ps[:, b, 0:384], "ps%d" % b, [(xT[:, kk, :], Wb[:, kk, 768 + h2 * 384:768 + (h2 + 1) * 384]) for kk in range(8)],
                 ["xTs", "Wb"])
            s.op("act", lambda e, b=b, h2=h2: e.copy(vb[:, h2 * 384:(h2 + 1) * 384], ps[:, b, 0:384]), rd=["ps%d" % b], wr=["vb"])
        kr3 = kr.rearrange("p (h d) -> p h d", h=4)
        s.op("dve", lambda e: e.tensor_tensor(kb, kr3, bc(fac[:, 3, :], [[1, 4], [0, 96]]), OP.mult), rd=["kr", "fac"], wr=["kb"])
        if need_f:
            s.op("dve", lambda e: e.tensor_tensor(kf, kr3, bc(fac[:, 2, :], [[1, 4], [0, 96]]), OP.mult), rd=["kr", "fac"], wr=["kf"])
        s.op("act", lambda e, n=n: e.copy(SbAll[:, n, :], Sb.rearrange("p h d -> p (h d)")), rd=["Sb"], wr=["SbAll"])
        for (kx, kres, dirn) in (((kb, "kb", 1), (kf, "kf", 0)) if need_f else ((kb, "kb", 1),)):
            for hp in range(2):
                b = k.bank()
                for h2 in range(2):
                    h = hp * 2 + h2
                    s.mm(ps[0:96, b, h2 * 192:(h2 + 1) * 192], "ps%d" % b, [(kx[:, h, :], vb[:, h * 192:(h + 1) * 192])], [kres, "vb"])
                for h2 in range(2):
                    h = hp * 2 + h2
                    if dirn == 1:
                        s.op("dve", lambda e, b=b, h=h, h2=h2: e.scalar_tensor_tensor(
                            Sb[:, h, :], Sb[:, h, :], g128[0:96, 4 + h:5 + h], ps[0:96, b, h2 * 192:(h2 + 1) * 192], OP.mult, OP.add),
                            rd=["ps%d" % b, "Sb", "g128"], wr=["Sb"])
                    else:
                        s.op("dve", lambda e, b=b, h=h, h2=h2, n=n: e.scalar_tensor_tensor(
                            Fl[:, h, :], ps[0:96, b, h2 * 192:(h2 + 1) * 192], pw[0:96, h, n:n + 1], Fl[:, h, :], OP.mult, OP.add),
                            rd=["ps%d" % b, "Fl", "pw"], wr=["Fl"])
    if need_f:
        s.dma("sp", Floc_out, Fl.rearrange("p h d -> p (h d)"), rd=["Fl"], wr=["Floc_out" + u])
    s.dma("sp", Sbend_out, Sb.rearrange("p h d -> p (h d)"), rd=["Sb"], wr=["Sbend_out" + u])


def phase_l1(k, c, X1, Wb, memkT, vmem, w_out1_bf, Fp, Bp, SbAll, gn_g, gn_b, ln_g, ln_b, w_router, b_router, X1N, X1NT, GT, nch=32, masks=None, st_rd=()):
    s, ps = k.s, k.ps
    k.banks = [0, 1, 2, 3, 4, 5, 6, 7]
    fac, g128, pw, maskT = c["fac"], c["g128"], c["pw"], c["maskT"]
    Wo = k.sb("Wo1", [128, 8, 1024], BF16)
    s.dma("sp", Wo, w_out1_bf.rearrange("(k p) n -> p k n", p=128), rd=[w_out1_bf.name], wr=["Wo1"])
    gng = k.sb("gng", [128, 768], F32)
    gnb = k.sb("gnb", [128, 768], F32)
    s.dma("sp", gng, gn_g.partition_broadcast(128), wr=["lnc"])
    s.dma("sp", gnb, gn_b.partition_broadcast(128), wr=["lnc"])
    Lg = k.sb("L1g", [128, 1024], F32)
    Lb = k.sb("L1b", [128, 1024], F32)
    s.dma("sp", Lg, ln_g.partition_broadcast(128), wr=["lnc"])
    s.dma("sp", Lb, ln_b.partition_broadcast(128), wr=["lnc"])
    Wr = k.sb("Wr", [128, 8, 8], F32)
    s.dma("sp", Wr, w_router.rearrange("(k p) e -> p k e", p=128), wr=["Wr"])
    brt = k.sb("brt", [128, 8], F32)
    s.dma("sp", brt, b_router.partition_broadcast(128), wr=["brt"])
    Sf = k.sb("Sf", [96, 4, 192], F32)
    BkP = k.sb("BkP", [96, 4, 192], F32)
    s.dma("sp", Sf.rearrange("p h d -> p (h d)"), Fp, rd=list(st_rd), wr=["Sf"])
    s.dma("sp", BkP.rearrange("p h d -> p (h d)"), Bp, rd=list(st_rd), wr=["BkP"])
    if masks is not None:
        mk = k.sb("mk", [96, 2], F32)
        s.dma("sp", mk, masks.partition_broadcast(96), wr=["mk"])
        s.op("dve", lambda e: e.tensor_scalar(Sf.rearrange("p h d -> p (h d)"), Sf.rearrange("p h d -> p (h d)"), mk[:, 0:1], None, OP.mult),
             rd=["Sf", "mk"], wr=["Sf"])
        s.op("dve", lambda e: e.tensor_scalar(BkP.rearrange("p h d -> p (h d)"), BkP.rearrange("p h d -> p (h d)"), mk[:, 1:2], None, OP.mult),
             rd=["BkP", "mk"], wr=["BkP"])
    XX = [k.sb("Xf%d" % i, [128, 1024], F32) for i in range(2)]
    xT = k.sb("xTf", [128, 8, 128], BF16)
    xT32 = k.sb("xT32", [128, 8, 128], F32)
    A, B = c["A"], c["B"]
    qk = k.sb("qk", [128, 768], F32)
    KF = [k.sb("kf1_%d" % i, [128, 4, 96], BF16) for i in range(2)]
    VB = [k.sb("vb1_%d" % i, [128, 768], BF16) for i in range(2)]
    SG = [k.sb("sgl%d" % i, [128, 768], F32) for i in range(2)]
    QT = [k.sb("qkT%d" % i, [96, 8, 128], BF16) for i in range(2)]
    STT = [k.sb("ST%d" % i, [128, 4, 128], BF16) for i in range(2)]
    Sfb = k.sb("Sfb", [96, 4, 192], BF16)
    SbT = k.sb("SbT", [96, 4, 192], BF16)
    o = k.sb("o", [128, 4, 192], F32)
    hst = k.sb("hst", [128, 4, 6], F32)
    hmv = k.sb("hmv", [128, 4, 2], F32)
    hrs = k.sb("hrs", [128, 4], F32)
    mixT = k.sb("mixT1", [128, 6, 128], BF16)
    MQ = [k.sb("mqT1_%d" % i, [128, 2, 128], BF16) for i in range(2)]
    xTo = k.sb("xTo", [128, 8, 128], BF16)
    memoT = k.sb("memoT1", [128, 2, 128], BF16)
    probs = k.sb("probs1", [128, 4, 256], F32)
    probsT = k.sb("probsT1", [128, 4, 2, 128], BF16)
    rmax = k.sb("rmax1", [128, 4], F32)
    rsum = k.sb("rsum1", [128, 4], F32)
    lgt = k.sb("lgt", [128, 8], F32)
    l2 = k.sb("l2", [128, 8], F32)
    eq1 = k.sb("eq1", [128, 8], F32)
    eq2 = k.sb("eq2", [128, 8], F32)
    m12 = k.sb("m12", [128, 4], F32)
    def stA(n):
        p = n % 2
        X, vb, sgl, qkT, ST, kf, mqT = XX[p], VB[p], SG[p], QT[p], STT[p], KF[p], MQ[p]
        rX, rvb, rsgl, rqkT, rST, rkf, rmq = "Xf%d" % p, "vb1_%d" % p, "sgl%d" % p, "qkT%d" % p, "ST%d" % p, "kf1_%d" % p, "mqT1_%d" % p
        s.dma("sp", X, X1[n * 128:(n + 1) * 128, :], rd=["X1_%d" % n], wr=[rX])
        yield
        k.transpose_tok(X, rX, xT, "xTf", 0)
        yield
        for h2 in range(2):
            b = k.bank()
            s.mm(ps[:, b, 0:384], "ps%d" % b, [(xT[:, kk, :], Wb[:, kk, h2 * 384:(h2 + 1) * 384]) for kk in range(8)], ["xTf", "Wb"])
            rope(k, c, n, ps[:, b, 0:384], 4, A[:, 0:384], B[:, 0:384], qk[:, h2 * 384:(h2 + 1) * 384], ["ps%d" % b], "qk")
        yield
        for h2 in range(2):
            b = k.bank()
            s.mm(ps[:, b, 0:384], "ps%d" % b, [(xT[:, kk, :], Wb[:, kk, 768 + h2 * 384:768 + (h2 + 1) * 384]) for kk in range(8)],
                 ["xTf", "Wb"])
            s.op("act", lambda e, b=b, h2=h2: e.copy(vb[:, h2 * 384:(h2 + 1) * 384], ps[:, b, 0:384]), rd=["ps%d" % b], wr=[rvb])
        for h2 in range(2):
            b = k.bank()
            s.mm(ps[:, b, 0:384], "ps%d" % b, [(xT[:, kk, :], Wb[:, kk, 1536 + h2 * 384:1536 + (h2 + 1) * 384]) for kk in range(8)],
                 ["xTf", "Wb"])
            s.op("act", lambda e, b=b, h2=h2: e.activation(sgl[:, h2 * 384:(h2 + 1) * 384], ps[:, b, 0:384], AF.Silu),
                 rd=["ps%d" % b], wr=[rsgl])
        for t in range(2):
            b = k.bank()
            s.mm(ps[:, b, 0:128], "ps%d" % b, [(Wb[:, kk, 2304 + t * 128:2304 + (t + 1) * 128], xT[:, kk, :]) for kk in range(8)],
                 ["xTf", "Wb"])
            k.evac(mqT[:, t, :], ps[:, b, 0:128], rd=["ps%d" % b], wr=[rmq])
        for hh in range(2):
            b = k.bank()
            s.tr([(ps[0:96, b, i * 128:(i + 1) * 128], qk[:, (hh * 4 + i) * 96:(hh * 4 + i + 1) * 96]) for i in range(4)],
                 "ps%d" % b, ["qk"], k.ident)
            k.evac(qkT[:, hh * 4:(hh + 1) * 4, :].rearrange("p h t -> p (h t)"), ps[0:96, b, :], rd=["ps%d" % b], wr=[rqkT])
        yield
        k3 = qk[:, 384:768].rearrange("p (h d) -> p h d", h=4)
        s.op("dve", lambda e: e.tensor_tensor(kf, k3, bc(fac[:, 2, :], [[1, 4], [0, 96]]), OP.mult), rd=["qk", "fac"], wr=[rkf])
        b = k.bank()
        for h in range(4):
            s.mm(ps[:, b, h * 128:(h + 1) * 128], "ps%d" % b, [(qkT[:, 4 + h, :], qkT[:, h, :])], [rqkT])
        s.op("dve", lambda e, b=b: e.tensor_tensor(ST.rearrange("p h i -> p (h i)"), ps[:, b, :], maskT.rearrange("p h i -> p (h i)"), OP.mult),
             rd=["ps%d" % b, "maskT"], wr=[rST])
        s.op("act", lambda e: e.copy(Sfb.rearrange("p h d -> p (h d)"), Sf.rearrange("p h d -> p (h d)")), rd=["Sf"], wr=["Sfb"])
        for h in range(4):
            s.op("dve", lambda e, h=h, n=n: e.scalar_tensor_tensor(SbT[:, h, :], BkP[:, h, :], pw[0:96, 4 + h, n:n + 1],
                                                                  SbAll[:, n, h * 192:(h + 1) * 192], OP.mult, OP.add),
                 rd=["BkP", "pw", "SbAll"], wr=["SbT"])

    def stB(n):
        p = n % 2
        X, vb, sgl, qkT, ST, kf, mqT = XX[p], VB[p], SG[p], QT[p], STT[p], KF[p], MQ[p]
        rX, rvb, rsgl, rqkT, rST, rkf, rmq = "Xf%d" % p, "vb1_%d" % p, "sgl%d" % p, "qkT%d" % p, "ST%d" % p, "kf1_%d" % p, "mqT1_%d" % p
        for hp in range(2):
            bi_, bf_, bb_ = k.bank(), k.bank(), k.bank()
            for h2 in range(2):
                h = hp * 2 + h2
                cs = slice(h2 * 192, (h2 + 1) * 192)
                s.mm(ps[:, bi_, cs], "ps%d" % bi_, [(ST[:, h, :], vb[:, h * 192:(h + 1) * 192])], [rST, rvb])
                s.mm(ps[:, bf_, cs], "ps%d" % bf_, [(qkT[:, h, :], Sfb[:, h, :])], [rqkT, "Sfb"])
                s.mm(ps[:, bb_, cs], "ps%d" % bb_, [(qkT[:, h, :], SbT[:, h, :])], [rqkT, "SbT"])
            s.op("act", lambda e, hp=hp, bi_=bi_: e.copy(o[:, hp * 2:(hp + 1) * 2, :].rearrange("p h d -> p (h d)"), ps[:, bi_, 0:384]),
                 rd=["ps%d" % bi_], wr=["o"])
            for h2 in range(2):
                h = hp * 2 + h2
                cs = slice(h2 * 192, (h2 + 1) * 192)
                s.op("dve", lambda e, h=h, cs=cs, bf_=bf_: e.scalar_tensor_tensor(o[:, h, :], ps[:, bf_, cs], fac[:, 0, h:h + 1], o[:, h, :],
                                                                              OP.mult, OP.add), rd=["ps%d" % bf_, "o", "fac"], wr=["o"])
                s.op("dve", lambda e, h=h, cs=cs, bb_=bb_: e.scalar_tensor_tensor(o[:, h, :], ps[:, bb_, cs], fac[:, 1, h:h + 1], o[:, h, :],
                                                                              OP.mult, OP.add), rd=["ps%d" % bb_, "o", "fac"], wr=["o"])
        yield
        for hp in range(2):
            b = k.bank()
            for h2 in range(2):
                h = hp * 2 + h2
                s.mm(ps[0:96, b, h2 * 192:(h2 + 1) * 192], "ps%d" % b, [(kf[:, h, :], vb[:, h * 192:(h + 1) * 192])], [rkf, rvb])
            for h2 in range(2):
                h = hp * 2 + h2
                s.op("dve", lambda e, b=b, h=h, h2=h2: e.scalar_tensor_tensor(
                    Sf[:, h, :], Sf[:, h, :], g128[0:96, h:h + 1], ps[0:96, b, h2 * 192:(h2 + 1) * 192], OP.mult, OP.add),
                    rd=["ps%d" % b, "Sf", "g128", "Sfb"], wr=["Sf"])
        for h in range(4):
            s.op("dve", lambda e, h=h: e.bn_stats(hst[:, h, :], o[:, h, :]), rd=["o"], wr=["hst"])
        for h in range(4):
            s.op("dve", lambda e, h=h: e.bn_aggr(hmv[:, h, :], hst[:, h, :]), rd=["hst"], wr=["hmv"])
        s.op("act", lambda e: e.activation(hrs, hmv[:, :, 1], AF.Ln, bias=EPS), rd=["hmv"], wr=["hrs"])
        s.op("act", lambda e: e.activation(hrs, hrs, AF.Exp, scale=-0.5), rd=["hrs"], wr=["hrs"])
        for h in range(4):
            s.op("dve", lambda e, h=h: e.tensor_scalar(o[:, h, :], o[:, h, :], hmv[:, h, 0:1], hrs[:, h:h + 1], OP.subtract, OP.mult),
                 rd=["o", "hmv", "hrs"], wr=["o"])
        of = o.rearrange("p h d -> p (h d)")
        s.op("dve", lambda e: e.tensor_tensor(of, of, gng, OP.mult), rd=["o", "lnc"], wr=["o"])
        s.op("dve", lambda e: e.tensor_tensor(of, of, gnb, OP.add), rd=["o", "lnc"], wr=["o"])
        s.op("dve", lambda e: e.tensor_tensor(of, of, sgl, OP.mult), rd=["o", rsgl], wr=["o"])
        yield
        k.transpose_tok(of, "o", mixT, "mixT1", 0, nk=6)
        mem_attn_a(k, mqT, rmq, memkT, vmem, memoT, "memoT1", 0, (probs, probsT, rmax, rsum))
        yield
        mem_attn_b(k, mqT, rmq, memkT, vmem, memoT, "memoT1", 0, (probs, probsT, rmax, rsum))
        yield
        for nh in range(2):
            b = k.bank()
            s.mm(ps[:, b, :], "ps%d" % b,
                 [(mixT[:, g, :], Wo[:, g, nh * 512:(nh + 1) * 512]) for g in range(6)] +
                 [(memoT[:, t, :], Wo[:, 6 + t, nh * 512:(nh + 1) * 512]) for t in range(2)],
                 ["mixT1", "memoT1", "Wo1"])
            s.op("dve", lambda e, b=b, nh=nh: e.scalar_tensor_tensor(
                X[:, nh * 512:(nh + 1) * 512], X[:, nh * 512:(nh + 1) * 512], ALPHA, ps[:, b, :], OP.mult, OP.add),
                rd=["ps%d" % b, rX], wr=[rX])
        k.ln_inplace(X, rX, Lg, Lb)
        s.dma("sp", X1N[n * 128:(n + 1) * 128, :], X, rd=[rX], wr=["X1N_%d" % n])
        yield
        for hh in range(2):
            b = k.bank()
            s.tr([(ps[:, b, i * 128:(i + 1) * 128], X[:, (hh * 4 + i) * 128:(hh * 4 + i + 1) * 128]) for i in range(4)],
                 "ps%d" % b, [rX], k.ident)
            s.op("dve", lambda e, b=b, hh=hh: e.tensor_copy(xT32[:, hh * 4:(hh + 1) * 4, :].rearrange("p k t -> p (k t)"), ps[:, b, :]),
                 rd=["ps%d" % b], wr=["xT32"])
            s.op("act", lambda e, hh=hh: e.copy(xTo[:, hh * 4:(hh + 1) * 4, :].rearrange("p k t -> p (k t)"),
                                               xT32[:, hh * 4:(hh + 1) * 4, :].rearrange("p k t -> p (k t)")),
                 rd=["xT32"], wr=["xTo"])
        s.dma("sp", X1NT[n], xTo.rearrange("p k t -> p (k t)"), rd=["xTo"], wr=["X1NT_%d" % n])
        yield
        b = k.bank()
        s.mm(ps[:, b, 0:8], "ps%d" % b, [(xT32[:, kk, :], Wr[:, kk, :]) for kk in range(8)], ["xT32", "Wr"])
        s.op("dve", lambda e, b=b: e.tensor_tensor(lgt, ps[:, b, 0:8], brt, OP.add), rd=["ps%d" % b, "brt"], wr=["lgt"])
        s.op("dve", lambda e: e.tensor_reduce(m12[:, 0:1], lgt, AX.X, OP.max), rd=["lgt"], wr=["m12"])
        s.op("dve", lambda e: e.tensor_scalar(eq1, lgt, m12[:, 0:1], None, OP.is_equal), rd=["lgt", "m12"], wr=["eq1"])
        s.op("dve", lambda e: e.scalar_tensor_tensor(l2, eq1, -1e30, lgt, OP.mult, OP.add), rd=["eq1", "lgt"], wr=["l2"])
        s.op("dve", lambda e: e.tensor_reduce(m12[:, 1:2], l2, AX.X, OP.max), rd=["l2"], wr=["m12"])
        s.op("dve", lambda e: e.tensor_scalar(eq2, l2, m12[:, 1:2], None, OP.is_equal), rd=["l2", "m12"], wr=["eq2"])
        s.op("dve", lambda e: e.tensor_tensor(m12[:, 2:3], m12[:, 1:2], m12[:, 0:1], OP.subtract), rd=["m12"], wr=["m12"])
        s.op("act", lambda e: e.activation(m12[:, 2:3], m12[:, 2:3], AF.Exp), rd=["m12"], wr=["m12"])
        s.op("dve", lambda e: e.tensor_scalar(m12[:, 3:4], m12[:, 2:3], 1.0, None, OP.add), rd=["m12"], wr=["m12"])
        s.op("dve", lambda e: e.reciprocal(m12[:, 3:4], m12[:, 3:4]), rd=["m12"], wr=["m12"])
        s.op("dve", lambda e: e.tensor_tensor(m12[:, 2:3], m12[:, 2:3], m12[:, 3:4], OP.mult), rd=["m12"], wr=["m12"])
        s.op("dve", lambda e: e.tensor_scalar(eq1, eq1, m12[:, 3:4], None, OP.mult), rd=["eq1", "m12"], wr=["eq1"])
        s.op("dve", lambda e, n=n: e.scalar_tensor_tensor(GT[:, n, :], eq2, m12[:, 2:3], eq1, OP.mult, OP.add),
             rd=["eq1", "eq2", "m12"], wr=["GT"])


    for _ in stA(0):
        pass
    for n in range(nch):
        gens = {"A": stA(n + 1) if n + 1 < nch else None, "B": stB(n)}
        for ch in "ABABABABBABB":
            g = gens[ch]
            if g is not None:
                try:
                    next(g)
                except StopIteration:
                    gens[ch] = None
        for ch in "BA":
            g = gens[ch]
            while g is not None:
                try:
                    next(g)
                except StopIteration:
                    g = None


def phase_moe(k, X1N, X1NT, GT, weg_bf, weu_bf, wed_bf, ln_g, ln_b, out, nexp=8, ecast=None):
    s, ps = k.s, k.ps
    k.banks = [0, 1, 2, 3, 4, 5, 6, 7]
    XNT = k.sb("XNT", [128, 16, 8, 128], BF16)
    facc = k.sb("facc", [128, 16, 1024], F32)
    WG = [k.sb("EG%d" % i, [128, 8, 512], BF16) for i in range(2)]
    WU = [k.sb("EU%d" % i, [128, 8, 512], BF16) for i in range(2)]
    WD = [k.sb("ED%d" % i, [128, 4, 1024], BF16) for i in range(2)]
    sgt = [k.sb("esg%d" % i, [128, 512], BF16) for i in range(2)]
    hT = [k.sb("ehT%d" % i, [128, 4, 512], BF16) for i in range(2)]
    Lg = k.sb("L2g", [128, 1024], F32)
    Lb = k.sb("L2b", [128, 1024], F32)
    s.dma("sp", Lg, ln_g.partition_broadcast(128), wr=["lnc"])
    s.dma("sp", Lb, ln_b.partition_broadcast(128), wr=["lnc"])
    Xr = [k.sb("Xr%d" % i, [128, 1024], F32) for i in range(2)]
    xrc = [0]

    def epilogue(hh, sub):
        gsub = hh * 16 + sub
        xr = Xr[xrc[0] % 2]
        rn = "Xr%d" % (xrc[0] % 2)
        xrc[0] += 1
        s.dma("sp", xr, X1N[gsub * 128:(gsub + 1) * 128, :], rd=["X1N_%d" % gsub], wr=[rn])
        s.op("dve", lambda e: e.scalar_tensor_tensor(xr, xr, ALPHA, facc[:, sub, :], OP.mult, OP.add),
             rd=[rn, "facc%d" % sub], wr=[rn])
        k.ln_inplace(xr, rn, Lg, Lb)
        s.dma("sp", out[gsub * 128:(gsub + 1) * 128, :], xr, rd=[rn], wr=["out_%d" % gsub])

    def down(hh, ex, blk, tt, jb, hb):
        first = (ex == 0 and blk == 0)
        if first and hh == 1:
            for s4 in range(4):
                epilogue(0, tt * 4 + s4)
        for s4 in range(4):
            sub = tt * 4 + s4
            gsub = hh * 16 + sub
            for nh in range(2):
                b = k.bank()
                s.mm(ps[:, b, :], "ps%d" % b,
                     [(hT[hb][:, t4, s4 * 128:(s4 + 1) * 128], WD[jb][:, t4, nh * 512:(nh + 1) * 512]) for t4 in range(4)],
                     ["ehT%d" % hb, "ED%d" % jb])
                if first:
                    s.op("dve", lambda e, b=b, sub=sub, gsub=gsub, nh=nh, ex=ex: e.tensor_scalar(
                        facc[:, sub, nh * 512:(nh + 1) * 512], ps[:, b, :], GT[:, gsub, ex:ex + 1], None, OP.mult),
                        rd=["ps%d" % b, "GT", "facc%d" % sub], wr=["facc%d" % sub])
                else:
                    s.op("dve", lambda e, b=b, sub=sub, gsub=gsub, nh=nh, ex=ex: e.scalar_tensor_tensor(
                        facc[:, sub, nh * 512:(nh + 1) * 512], ps[:, b, :], GT[:, gsub, ex:ex + 1],
                        facc[:, sub, nh * 512:(nh + 1) * 512], OP.mult, OP.add),
                        rd=["ps%d" % b, "GT", "facc%d" % sub], wr=["facc%d" % sub])
        if hh == 1 and ex == nexp - 1 and blk == 6:
            for s4 in range(4):
                epilogue(1, tt * 4 + s4)

    wc = 0
    hc = 0
    pend = None
    for hh in range(2):
        for cc in range(16):
            s.dma("sp", XNT[:, cc, :, :].rearrange("p k t -> p (k t)"), X1NT[hh * 16 + cc],
                  rd=["X1NT_%d" % (hh * 16 + cc)], wr=["XNT%d" % (cc // 4)])
        for ex in range(nexp):
            nxt = list(ecast[ex + 1]) if (ecast is not None and hh == 0 and ex + 1 < nexp) else []
            for blk in range(7):
                jb = wc % 2
                wc += 1
                s.dma("sp", WG[jb], weg_bf[ex * 1024:(ex + 1) * 1024, blk * 512:(blk + 1) * 512].rearrange("(k p) n -> p k n", p=128),
                      rd=[weg_bf.name + "_%d" % (ex * 2048 + i * 512) for i in range(4)], wr=["EG%d" % jb])
                s.dma("sp", WU[jb], weu_bf[ex * 1024:(ex + 1) * 1024, blk * 512:(blk + 1) * 512].rearrange("(k p) n -> p k n", p=128),
                      rd=[weu_bf.name + "_%d" % (ex * 2048 + i * 512) for i in range(4)], wr=["EU%d" % jb])
                s.dma("sp", WD[jb], wed_bf[ex * 3584 + blk * 512:ex * 3584 + (blk + 1) * 512, :].rearrange("(t p) n -> p t n", p=128),
                      rd=[wed_bf.name + "_%d" % (ex * 3584 + blk * 512)], wr=["ED%d" % jb])
                for tt in range(4):
                    hb = hc % 2
                    hc += 1
                    for t4 in range(4):
                        bg, bu = k.bank(), k.bank()
                        xs = lambda kk: XNT[:, tt * 4:(tt + 1) * 4, kk, :]
                        s.mm(ps[:, bg, :].rearrange("p (c t) -> p c t", c=4), "ps%d" % bg,
                             [(WG[jb][:, kk, t4 * 128:(t4 + 1) * 128], xs(kk)) for kk in range(8)], ["EG%d" % jb, "XNT%d" % tt])
                        s.mm(ps[:, bu, :].rearrange("p (c t) -> p c t", c=4), "ps%d" % bu,
                             [(WU[jb][:, kk, t4 * 128:(t4 + 1) * 128], xs(kk)) for kk in range(8)], ["EU%d" % jb, "XNT%d" % tt])
                        s.op("act", lambda e, bg=bg, t4=t4: e.activation(sgt[t4 % 2], ps[:, bg, :], AF.Silu), rd=["ps%d" % bg], wr=["esg%d" % (t4 % 2)])
                        s.op("dve", lambda e, bu=bu, t4=t4, hb=hb: e.tensor_tensor(hT[hb][:, t4, :], sgt[t4 % 2], ps[:, bu, :], OP.mult),
                             rd=["ps%d" % bu, "esg%d" % (t4 % 2)], wr=["ehT%d" % hb])
                    if pend is not None:
                        down(*pend)
                    pend = (hh, ex, blk, tt, jb, hb)
                    if tt == 1:
                        for _ in range((2, 2, 2, 2, 3, 2, 2)[blk]):
                            if nxt:
                                nxt.pop(0)(rd=["ehT%d" % hb])
    down(*pend)


def build_F():
    k = K()
    x = k.din("x", [4096, 1024]); xo = k.din("xo", [4096, 1024])
    mem = k.din("mem", [256, 1024]); wkv = k.din("w_mem_kv", [1024, 512])
    w_in_a = k.din("w_in_a", [1024, 1792]); sg_ln_g = k.din("sg_ln_g", [768]); sg_ln_b = k.din("sg_ln_b", [768])
    sg_w = k.din("sg_w", [8, 128, 128]); sg_b = k.din("sg_b", [8, 128])
    w_out = k.din("w_out", [2, 1024, 1024]); ln_g = k.din("ln_g", [2, 2, 1024]); ln_b = k.din("ln_b", [2, 2, 1024])
    wg = k.din("w_ff_gate", [1024, 2816]); wu = k.din("w_ff_up", [1024, 2816]); wd = k.din("w_ff_down", [2816, 1024])
    w_in_b = k.din("w_in_b", [1024, 2560]); dlf = k.din("decay_logit_f", [4]); dlb = k.din("decay_logit_b", [4])
    gn_g = k.din("ret_gn_g", [768]); gn_b = k.din("ret_gn_b", [768])
    w_router = k.din("w_router", [1024, 8]); b_router = k.din("b_router", [8])
    weg = k.din("w_e_gate", [8, 1024, 3584]); weu = k.din("w_e_up", [8, 1024, 3584]); wed = k.din("w_e_down", [8, 3584, 1024])
    posb = k.din("posb", [1]); posbo = k.din("posbo", [1]); masks = k.din("masks", [2])
    out = k.dout("out", [4096, 1024])
    X1 = k.dscr("X1", [4096, 1024], F32); X1o = k.dscr("X1o", [4096, 1024], F32)
    st = k.dscr("st_oth", [192, 768], F32); st2 = k.dscr("st_own", [192, 768], F32)
    wkv_bf = k.dscr("wkv_bf", [1024, 512], BF16); w_in_bf = k.dscr("w_in_a_bf", [1024, 1792], BF16)
    w_out_bf = k.dscr("w_out_bf", [2048, 1024], BF16)
    wg_bf = k.dscr("wg_bf", [1024, 2816], BF16); wu_bf = k.dscr("wu_bf", [1024, 2816], BF16); wd_bf = k.dscr("wd_bf", [2816, 1024], BF16)
    w_in_b_bf = k.dscr("w_in_b_bf", [1024, 2560], BF16)
    weg_bf = k.dscr("weg_bf", [8 * 1024, 3584], BF16); weu_bf = k.dscr("weu_bf", [8 * 1024, 3584], BF16)
    wed_bf = k.dscr("wed_bf", [8 * 3584, 1024], BF16)
    X1N = k.dscr("X1N", [4096, 1024], F32); X1NT = k.dscr("X1NT", [32, 128, 1024], BF16)
    k.cast_dram(w_in_bf, w_in_a); k.cast_dram(wkv_bf, wkv)
    k.cast_dram(w_out_bf, w_out.rearrange("a k n -> (a k) n"))
    for j in range(11):
        k.s.dma("pool", wg_bf[:, j * 256:(j + 1) * 256], wg[:, j * 256:(j + 1) * 256], wr=[wg_bf.name + "_%d" % j])
        k.s.dma("pool", wu_bf[:, j * 256:(j + 1) * 256], wu[:, j * 256:(j + 1) * 256], wr=[wu_bf.name + "_%d" % j])
        k.s.dma("pool", wd_bf[j * 256:(j + 1) * 256, :], wd[j * 256:(j + 1) * 256, :], wr=[wd_bf.name + "_%d" % j])
    late = [lambda: k.cast_dram(w_in_b_bf.rearrange("k (a n) -> (k a) n", a=2), w_in_b.rearrange("k (a n) -> (k a) n", a=2))]
    memkT, vmem = setup_mem(k, mem, wkv_bf)
    GT = k.sb("GT", [128, 32, 8], F32)
    bgj = []
    g1 = k.cast_jobs(weg_bf.rearrange("k (a n) -> (k a) n", a=2), weg.rearrange("e k (a n) -> (e k a) n", a=2), rows_per=512)
    g2 = k.cast_jobs(weu_bf.rearrange("k (a n) -> (k a) n", a=2), weu.rearrange("e k (a n) -> (e k a) n", a=2), rows_per=512)
    g3 = k.cast_jobs(wed_bf, wed.rearrange("e k n -> (e k) n"), rows_per=512)
    ecast = []
    for i in range(8):
        pcs = []
        for q in range(4):
            pcs += [g1[i * 4 + q], g2[i * 4 + q]]
        pcs += g3[i * 7:(i + 1) * 7]
        ecast.append(pcs)
    c = setup_l1(k, dlf, dlb, posb)
    setup_rope(k, c, posbo)
    with k.scope():
        phase_l0(k, [(xo, X1o, "X1o"), (x, X1, "X1")], memkT, vmem, w_in_bf, w_out_bf[0:1024, :], wg_bf, wu_bf, wd_bf,
                 sg_w, sg_b, sg_ln_g, sg_ln_b, ln_g[0], ln_b[0], bg=late)
    with k.scope():
        Wb = load_l1_weights(k, w_in_b_bf, None)
        SbAll = k.sb("SbAll", [96, 32, 768], BF16)
        for j in ecast[0]:
            j()
        with k.scope():
            phase_scan(k, c, X1o, Wb, st[0:96, :], st[96:192, :], SbAll, pfx="X1o", u="_o")
        setup_rope(k, c, posb)
        with k.scope():
            phase_scan(k, c, X1, Wb, st2[0:96, :], st2[96:192, :], SbAll, pfx="X1", u="", need_f=False)
        with k.scope():
            phase_l1(k, c, X1, Wb, memkT, vmem, w_out_bf[1024:2048, :], st[0:96, :], st[96:192, :], SbAll, gn_g, gn_b,
                     ln_g[1, 0], ln_b[1, 0], w_router, b_router, X1N, X1NT, GT, masks=masks, st_rd=["Floc_out_o", "Sbend_out_o"])
    with k.scope():
        phase_moe(k, X1N, X1NT, GT, weg_bf, weu_bf, wed_bf, ln_g[1, 1], ln_b[1, 1], out, ecast=ecast)
    k.s.wait_all("sp")
    return k.nc


def kernel(x, mem, w_mem_kv, w_in_a, sg_ln_g, sg_ln_b, sg_w, sg_b, w_in_b, decay_logit_f, decay_logit_b, ret_gn_g, ret_gn_b,
           w_out, ln_g, ln_b, w_ff_gate, w_ff_up, w_ff_down, w_router, b_router, w_e_gate, w_e_up, w_e_down):
    f = lambda a: np.ascontiguousarray(np.asarray(a, dtype=np.float32))
    x = f(x); mem = f(mem)
    n = 8
    common = dict(w_mem_kv=f(w_mem_kv), w_in_a=f(w_in_a)[0], sg_ln_g=f(sg_ln_g)[0], sg_ln_b=f(sg_ln_b)[0], sg_w=f(sg_w)[0],
                  sg_b=f(sg_b)[0], w_out=f(w_out), ln_g=f(ln_g), ln_b=f(ln_b), w_ff_gate=f(w_ff_gate)[0], w_ff_up=f(w_ff_up)[0],
                  w_ff_down=f(w_ff_down)[0], w_in_b=f(w_in_b)[0], decay_logit_f=f(decay_logit_f)[0], decay_logit_b=f(decay_logit_b)[0],
                  ret_gn_g=f(ret_gn_g)[0], ret_gn_b=f(ret_gn_b)[0], w_router=f(w_router)[0], b_router=f(b_router)[0],
                  w_e_gate=f(w_e_gate)[0], w_e_up=f(w_e_up)[0], w_e_down=f(w_e_down)[0])
    in_maps = []
    for c in range(n):
        b, hf = c // 2, c % 2
        in_maps.append(dict(common, x=np.ascontiguousarray(x[b, hf * 4096:(hf + 1) * 4096]),
                            xo=np.ascontiguousarray(x[b, (1 - hf) * 4096:(2 - hf) * 4096]),
                            mem=np.ascontiguousarray(mem[b]), posb=np.full([1], float(hf * 4096), np.float32),
                            posbo=np.full([1], float((1 - hf) * 4096), np.float32),
                            masks=np.array([float(hf == 1), float(hf == 0)], np.float32)))
    nc = build_F()
    res = run_bass_kernel_spmd(nc, in_maps, core_ids=list(range(n))).results
    out = np.zeros([4, 8192, 1024], np.float32)
    for c in range(n):
        out[c // 2, (c % 2) * 4096:(c % 2 + 1) * 4096] = res[c]["out"]
    return out
```
